# Optimizing a Trainium2 kernel written in Bass

```python
import jax, jax.numpy as jnp
from jax import lax
import numpy as np

D_MODEL = 1024
BATCH = 1
SEQ = 16384
DEPTH = 4

N_A_LAYERS = DEPTH // 2
N_B_LAYERS = DEPTH - N_A_LAYERS
D_FF = 4 * D_MODEL
NORM_EPS = 1e-6
NEG_INF = -1e30
GATE_FLOOR = 1e-30

HG_EXPAND = 128
HG_HEADS = D_MODEL // HG_EXPAND
HG_DK = HG_EXPAND
HG_DV = D_MODEL // HG_HEADS
HG_CHUNK = 64

NSA_HEADS = 16
NSA_KV_HEADS = 4
NSA_GROUP = NSA_HEADS // NSA_KV_HEADS
NSA_DH = D_MODEL // NSA_HEADS
CMP_BLOCK = 32
CMP_STRIDE = 16
CMP_HIDDEN = 4 * NSA_DH
SLC_BLOCK = 64
SLC_TOPK = 16
WINDOW = 512
Q_BLOCK = 128
FORCE_BONUS = 1e4
N_KV_STREAMS = 6

kernel_name = 'hgrn2_nsa_yoco_hybrid'


def rmsnorm(x, w):
    xf = x.astype(jnp.float32)
    y = xf * lax.rsqrt(jnp.mean(jnp.square(xf), axis=-1, keepdims=True) + NORM_EPS)
    return (y * w.astype(jnp.float32)).astype(x.dtype)


def squared_relu_mlp(h, w_up, w_down):
    return jnp.square(jax.nn.relu(h @ w_up)) @ w_down


def hgrn2_lower_bounds(lb_param):
    p = jax.nn.softmax(lb_param.astype(jnp.float32), axis=0)
    return jnp.cumsum(p, axis=0) - p[0:1]


def alibi_slopes(n):
    return jnp.exp2(-8.0 * jnp.arange(1, n + 1, dtype=jnp.float32) / n)


def masked_softmax(s, mask):
    p = jax.nn.softmax(jnp.where(mask, s, NEG_INF), axis=-1)
    return jnp.where(mask, p, 0.0)


def hgrn2_mixer(h, w_in, gnorm_w, w_out, lb):
    B, T, _ = h.shape
    hk = HG_HEADS * HG_DK
    hv = HG_HEADS * HG_DV
    proj = h @ w_in
    q = proj[..., :hk]
    f_pre = proj[..., hk:2 * hk].astype(jnp.float32)
    v_in = proj[..., 2 * hk:2 * hk + hv]
    g_out = proj[..., 2 * hk + hv:]
    f_gate = lb + (1.0 - lb) * jax.nn.sigmoid(f_pre)
    log_f = jnp.log(jnp.maximum(f_gate, GATE_FLOOR))
    k_in = (1.0 - lb) * jax.nn.sigmoid(-f_pre)
    nc = T // HG_CHUNK

    def chunks(t, d):
        return t.astype(jnp.float32).reshape(B, nc, HG_CHUNK, HG_HEADS, d).transpose(1, 0, 3, 2, 4)

    qc, kc, vc, gc = chunks(q, HG_DK), chunks(k_in, HG_DK), chunks(v_in, HG_DV), chunks(log_f, HG_DK)
    causal = jnp.tril(jnp.ones((HG_CHUNK, HG_CHUNK), dtype=bool))[None, None, :, :, None]

    def step(S, inp):
        qb, kb, vb, gb = inp
        bcum = jnp.cumsum(gb, axis=2)
        diff = bcum[:, :, :, None, :] - bcum[:, :, None, :, :]
        decay = jnp.exp(jnp.where(causal, diff, NEG_INF))
        attn = jnp.einsum('bhtd,bhsd,bhtsd->bhts', qb, kb, decay)
        o = (jnp.einsum('bhts,bhsv->bhtv', attn, vb)
             + jnp.einsum('bhtd,bhdv->bhtv', qb * jnp.exp(bcum), S))
        blast = bcum[:, :, -1:, :]
        S = (jnp.exp(blast[:, :, 0, :])[..., None] * S
             + jnp.einsum('bhsd,bhsv->bhdv', kb * jnp.exp(blast - bcum), vb))
        return S, o

    S0 = jnp.zeros((B, HG_HEADS, HG_DK, HG_DV), jnp.float32)
    _, o = lax.scan(step, S0, (qc, kc, vc, gc))
    o = o.transpose(1, 0, 3, 2, 4).reshape(B, T, HG_HEADS, HG_DV)
    o = rmsnorm(o, gnorm_w).reshape(B, T, hv) * jax.nn.silu(g_out.astype(jnp.float32))
    return o.astype(h.dtype) @ w_out


def compress_blocks(x, pe, w1, w2):
    B, G, T, dh = x.shape
    nc = T // CMP_STRIDE
    xp = jnp.pad(x, ((0, 0), (0, 0), (0, CMP_BLOCK - CMP_STRIDE), (0, 0)))
    parts = [xp[:, :, j * CMP_STRIDE:j * CMP_STRIDE + T].reshape(B, G, nc, CMP_STRIDE, dh)
             for j in range(CMP_BLOCK // CMP_STRIDE)]
    blocks = jnp.concatenate(parts, axis=3) + pe
    hidden = jax.nn.gelu(blocks.reshape(B, G, nc, CMP_BLOCK * dh) @ w1)
    return hidden @ w2


def nsa_shared_kv(h, kv_w, pe_k, w1_k, w2_k, pe_v, w1_v, w2_v):
    B, T, _ = h.shape
    G, dh = NSA_KV_HEADS, NSA_DH
    kv = (h @ kv_w).reshape(B, T, N_KV_STREAMS, G, dh).transpose(2, 0, 3, 1, 4)
    k_cmp = compress_blocks(kv[0], pe_k, w1_k, w2_k)
    v_cmp = compress_blocks(kv[1], pe_v, w1_v, w2_v)
    n_slc = T // SLC_BLOCK
    k_slc = kv[2].reshape(B, G, n_slc, SLC_BLOCK, dh)
    v_slc = kv[3].reshape(B, G, n_slc, SLC_BLOCK, dh)
    pad = ((0, 0), (0, 0), (WINDOW, 0), (0, 0))
    k_win = jnp.pad(kv[4], pad)
    v_win = jnp.pad(kv[5], pad)
    return k_cmp, v_cmp, k_slc, v_slc, k_win, v_win


def nsa_mixer(h, w_in, w_out, k_cmp, v_cmp, k_slc, v_slc, k_win, v_win):
    B, T, _ = h.shape
    G, J, dh = NSA_KV_HEADS, NSA_GROUP, NSA_DH
    nq = T // Q_BLOCK
    n_cmp = k_cmp.shape[2]
    n_slc = k_slc.shape[2]
    top_k = min(SLC_TOPK, n_slc)
    scale = dh ** -0.5
    slopes = alibi_slopes(NSA_HEADS).reshape(1, G, J, 1, 1)
    proj = h @ w_in
    q = proj[..., :NSA_HEADS * dh].reshape(B, nq, Q_BLOCK, G, J, dh).transpose(1, 0, 3, 4, 2, 5)
    gates = jax.nn.sigmoid(proj[..., NSA_HEADS * dh:].astype(jnp.float32))
    gates = gates.reshape(B, nq, Q_BLOCK, G, J, 3).transpose(1, 0, 3, 4, 2, 5)
    cmp_end = jnp.arange(n_cmp) * CMP_STRIDE + (CMP_BLOCK - 1)
    slc_idx = jnp.arange(n_slc)
    tok_in_blk = jnp.arange(SLC_BLOCK)
    win_off = jnp.arange(WINDOW + Q_BLOCK)
    bi = jnp.arange(B)[:, None, None, None]
    gi = jnp.arange(G)[None, :, None, None]
    r = SLC_BLOCK // CMP_STRIDE
    left = CMP_BLOCK // CMP_STRIDE - 1

    def block(args):
        qi, qb, gb = args
        t_pos = qi * Q_BLOCK + jnp.arange(Q_BLOCK)
        dist_c = t_pos[:, None] - cmp_end[None, :]
        s_c = (jnp.einsum('bgjqd,bgcd->bgjqc', qb, k_cmp).astype(jnp.float32) * scale
               - slopes * dist_c.astype(jnp.float32))
        p_cmp = masked_softmax(s_c, dist_c >= 0)
        o_cmp = jnp.einsum('bgjqc,bgcd->bgjqd', p_cmp.astype(v_cmp.dtype), v_cmp)
        imp = jnp.pad(p_cmp.sum(axis=2), ((0, 0), (0, 0), (0, 0), (left, r)))
        imp_slc = imp[..., 0:r * n_slc:r]
        for u in range(1, r + left):
            imp_slc = imp_slc + imp[..., u:u + r * n_slc:r]
        cur = t_pos // SLC_BLOCK
        blk_ok = slc_idx[None, :] <= cur[:, None]
        forced = ((slc_idx[None, :] == 0) | (slc_idx[None, :] == cur[:, None])
                  | (slc_idx[None, :] == cur[:, None] - 1))
        score = jnp.where(blk_ok, imp_slc + FORCE_BONUS * forced, NEG_INF)
        top_s, top_i = lax.top_k(score, top_k)
        k_sel = k_slc[bi, gi, top_i].reshape(B, G, Q_BLOCK, top_k * SLC_BLOCK, dh)
        v_sel = v_slc[bi, gi, top_i].reshape(B, G, Q_BLOCK, top_k * SLC_BLOCK, dh)
        pos5 = top_i[..., None] * SLC_BLOCK + tok_in_blk
        dist5 = t_pos[None, None, :, None, None] - pos5
        ok5 = (top_s > NEG_INF * 0.5)[..., None] & (dist5 >= 0)
        dist_s = dist5.reshape(B, G, Q_BLOCK, top_k * SLC_BLOCK)[:, :, None]
        ok_s = ok5.reshape(B, G, Q_BLOCK, top_k * SLC_BLOCK)[:, :, None]
        s_s = (jnp.einsum('bgjqd,bgqkd->bgjqk', qb, k_sel).astype(jnp.float32) * scale
               - slopes * dist_s.astype(jnp.float32))
        p_s = masked_softmax(s_s, ok_s)
        o_sel = jnp.einsum('bgjqk,bgqkd->bgjqd', p_s.astype(v_sel.dtype), v_sel)
        kw = lax.dynamic_slice_in_dim(k_win, qi * Q_BLOCK, WINDOW + Q_BLOCK, axis=2)
        vw = lax.dynamic_slice_in_dim(v_win, qi * Q_BLOCK, WINDOW + Q_BLOCK, axis=2)
        pos_w = qi * Q_BLOCK - WINDOW + win_off
        dist_w = t_pos[:, None] - pos_w[None, :]
        ok_w = (pos_w[None, :] >= 0) & (dist_w >= 0) & (dist_w < WINDOW)
        s_w = (jnp.einsum('bgjqd,bgkd->bgjqk', qb, kw).astype(jnp.float32) * scale
               - slopes * dist_w.astype(jnp.float32))
        p_w = masked_softmax(s_w, ok_w)
        o_win = jnp.einsum('bgjqk,bgkd->bgjqd', p_w.astype(vw.dtype), vw)
        out = (gb[..., 0:1] * o_cmp.astype(jnp.float32) + gb[..., 1:2] * o_sel.astype(jnp.float32)
               + gb[..., 2:3] * o_win.astype(jnp.float32))
        return out.astype(h.dtype)

    outs = lax.map(block, (jnp.arange(nq), q, gates))
    o = outs.transpose(1, 0, 4, 2, 3, 5).reshape(B, T, NSA_HEADS * dh)
    return o @ w_out


def setup_inputs(seed: int = 0) -> dict:
    key = jax.random.key(seed)
    ks = jax.random.split(key, 24)
    f32 = jnp.float32

    def nrm(k, shape, fan_in):
        return jax.random.normal(k, shape, f32) * fan_in ** -0.5

    def gain(k, shape):
        return 1.0 + 0.01 * jax.random.normal(k, shape, f32)

    hk = HG_HEADS * HG_DK
    hv = HG_HEADS * HG_DV
    a_in_width = 2 * hk + 2 * hv
    b_q_width = NSA_HEADS * NSA_DH
    b_in_width = b_q_width + 3 * NSA_HEADS
    kv_width = N_KV_STREAMS * NSA_KV_HEADS * NSA_DH
    cmp_in = CMP_BLOCK * NSA_DH
    return {
        'x': jax.random.normal(ks[0], (BATCH, SEQ, D_MODEL), f32),
        'a_norm_w': gain(ks[1], (N_A_LAYERS, D_MODEL)),
        'a_w_in': nrm(ks[2], (N_A_LAYERS, D_MODEL, a_in_width), D_MODEL),
        'a_gnorm_w': gain(ks[3], (N_A_LAYERS, HG_DV)),
        'a_w_out': nrm(ks[4], (N_A_LAYERS, hv, D_MODEL), hv),
        'a_lower_bounds': 0.5 * jax.random.normal(ks[5], (N_A_LAYERS, hk), f32),
        'kv_norm_w': gain(ks[6], (D_MODEL,)),
        'kv_w': nrm(ks[7], (D_MODEL, kv_width), D_MODEL),
        'cmp_pe_k': 0.1 * jax.random.normal(ks[8], (CMP_BLOCK, NSA_DH), f32),
        'cmp_w1_k': nrm(ks[9], (cmp_in, CMP_HIDDEN), cmp_in),
        'cmp_w2_k': nrm(ks[10], (CMP_HIDDEN, NSA_DH), CMP_HIDDEN),
        'cmp_pe_v': 0.1 * jax.random.normal(ks[11], (CMP_BLOCK, NSA_DH), f32),
        'cmp_w1_v': nrm(ks[12], (cmp_in, CMP_HIDDEN), cmp_in),
        'cmp_w2_v': nrm(ks[13], (CMP_HIDDEN, NSA_DH), CMP_HIDDEN),
        'b_norm_w': gain(ks[14], (N_B_LAYERS, D_MODEL)),
        'b_w_in': nrm(ks[15], (N_B_LAYERS, D_MODEL, b_in_width), D_MODEL),
        'b_w_out': nrm(ks[16], (N_B_LAYERS, b_q_width, D_MODEL), b_q_width),
        'mlp_norm_w': gain(ks[17], (DEPTH, D_MODEL)),
        'mlp_w_up': nrm(ks[18], (DEPTH, D_MODEL, D_FF), D_MODEL),
        'mlp_w_down': nrm(ks[19], (DEPTH, D_FF, D_MODEL), D_FF),
        'final_norm_w': gain(ks[20], (D_MODEL,)),
    }


def reference(x, a_norm_w, a_w_in, a_gnorm_w, a_w_out, a_lower_bounds, kv_norm_w, kv_w,
              cmp_pe_k, cmp_w1_k, cmp_w2_k, cmp_pe_v, cmp_w1_v, cmp_w2_v,
              b_norm_w, b_w_in, b_w_out, mlp_norm_w, mlp_w_up, mlp_w_down, final_norm_w):
    lbs = hgrn2_lower_bounds(a_lower_bounds)
    shared = None
    for layer in range(DEPTH):
        if layer < N_A_LAYERS:
            x = x + hgrn2_mixer(rmsnorm(x, a_norm_w[layer]), a_w_in[layer], a_gnorm_w[layer],
                                a_w_out[layer], lbs[layer])
        else:
            if shared is None:
                shared = nsa_shared_kv(rmsnorm(x, kv_norm_w), kv_w, cmp_pe_k, cmp_w1_k, cmp_w2_k,
                                       cmp_pe_v, cmp_w1_v, cmp_w2_v)
            b = layer - N_A_LAYERS
            x = x + nsa_mixer(rmsnorm(x, b_norm_w[b]), b_w_in[b], b_w_out[b], *shared)
        x = x + squared_relu_mlp(rmsnorm(x, mlp_norm_w[layer]), mlp_w_up[layer], mlp_w_down[layer])
    return rmsnorm(x, final_norm_w)
```

```python
import numpy as np
import concourse.bass as bass
import concourse.mybir as mybir
from concourse.bass_utils import run_bass_kernel_spmd

F32 = mybir.dt.float32
BF16 = mybir.dt.bfloat16
AF = mybir.ActivationFunctionType
ALU = mybir.AluOpType
AX = mybir.AxisListType


class Res:
    __slots__ = ("name", "lw", "rd", "dsem", "dcnt", "dq")

    def __init__(self, name):
        self.name = name
        self.lw = None
        self.rd = []
        self.dsem = None
        self.dq = None
        self.dcnt = 0


class Prog:
    def __init__(self, nc, stack):
        self.nc = nc
        self.stack = stack
        self.gstack = stack
        self.free_sems = []
        self.phase_res = []
        self.eng = {"pe": nc.tensor, "act": nc.scalar, "dve": nc.vector,
                    "pool": nc.gpsimd, "sp": nc.sync}
        self.sem = {}
        self.cnt = {}
        for k in ("pe", "act", "dve", "pool"):
            self.sem[k] = stack.enter_context(nc.semaphore("s_" + k))
            self.cnt[k] = 0
        self.waited = {k: {} for k in self.eng}
        self.nres = 0
        self.out_tokens = []
        self.ninstr = 0

    def sb(self, name, shape, dt):
        self.nalloc = getattr(self, "nalloc", 0) + 1
        t = self.stack.enter_context(self.nc.sbuf_tensor("s%d_%s" % (self.nalloc, name), list(shape), dt))
        return t

    def ps(self, name, shape, dt=F32):
        self.nalloc = getattr(self, "nalloc", 0) + 1
        t = self.stack.enter_context(self.nc.psum_tensor("p%d_%s" % (self.nalloc, name), list(shape), dt))
        return t

    def res(self, name=None):
        self.nres += 1
        r = Res(name or ("r%d" % self.nres))
        self.phase_res.append(r)
        return r

    def barrier(self):
        toks = [(k, self.cnt[k]) for k in ("pe", "act", "dve", "pool") if self.cnt[k] > 0]
        for r in self.phase_res:
            if r.dsem is not None:
                toks.append((r.dsem, r.dcnt))
        for e in ("sp", "pe", "act", "dve", "pool"):
            self._emit_waits_all(e, toks)

    def _emit_waits_all(self, e, toks):
        wd = self.waited[e]
        for (k, v) in toks:
            if wd.get(k, 0) >= v:
                continue
            self.eng[e].wait_ge(self.sem[k], v)
            wd[k] = v
            self.ninstr += 1

    def begin_phase(self):
        from contextlib import ExitStack as _ES
        self.stack = _ES()
        self.phase_res = []
        return self.stack

    def end_phase(self):
        self.barrier()
        for r in self.phase_res:
            if r.dsem is not None:
                self.free_sems.append((r.dsem, r.dcnt, r.dq))
                r.dsem = None
        self.phase_res = []
        self.stack.close()
        self.stack = self.gstack

    def _deps(self, reads, writes):
        toks = []
        for r in reads:
            if r.lw is not None:
                toks.append(r.lw)
        for w in writes:
            if w.lw is not None:
                toks.append(w.lw)
            toks.extend(w.rd)
        return toks

    def _emit_waits(self, e, toks):
        wd = self.waited[e]
        need = {}
        for (k, v) in toks:
            if e == "pe" and k == "pe":
                continue
            if wd.get(k, 0) >= v:
                continue
            if need.get(k, 0) < v:
                need[k] = v
        for k, v in need.items():
            self.eng[e].wait_ge(self.sem[k], v)
            wd[k] = v
            self.ninstr += 1

    def _commit(self, tok, reads, writes):
        for r in reads:
            r.rd.append(tok)
        for w in writes:
            w.lw = tok
            w.rd = []

    def op(self, e, fn, reads=(), writes=()):
        self._emit_waits(e, self._deps(reads, writes))
        ins = fn(self.eng[e])
        self.cnt[e] += 1
        ins.then_inc(self.sem[e], 1)
        self.ninstr += 1
        tok = (e, self.cnt[e])
        self._commit(tok, reads, writes)
        return tok

    def dma(self, q, out, in_, sres, reads=(), writes=(), is_output=False, **kw):
        self._emit_waits(q, self._deps(reads, writes))
        qt = "sw" if q == "pool" else "hw"
        if sres.dsem is not None:
            assert sres.dq == qt, "mixed SW/HW DMA on one semaphore: " + sres.name
        if sres.dsem is None:
            sres.dq = qt
            fl = [i for i, f in enumerate(self.free_sems) if f[2] == qt]
            if fl:
                key, cnt0, _ = self.free_sems.pop(fl[0])
                sres.dsem = key
                sres.dcnt = cnt0
            else:
                key = "d%d" % self.nres
                self.nres += 1
                self.sem[key] = self.gstack.enter_context(self.nc.semaphore(key))
                sres.dsem = key
        ins = self.eng[q].dma_start(out=out, in_=in_, **kw)
        sres.dcnt += 16
        ins.then_inc(self.sem[sres.dsem], 16)
        self.ninstr += 1
        tok = (sres.dsem, sres.dcnt)
        self._commit(tok, reads, writes)
        if is_output:
            self.out_tokens.append(tok)
        return tok

    def finish(self):
        self._emit_waits("sp", self.out_tokens)


D = 1024
KD = D // 128
DFF = 4096
EPS = 1e-6
TT = 512


def ACT(p, func, out, in_, reads, writes, **kw):
    return p.op("act", lambda g: g.activation(out=out, in_=in_, func=func, **kw), reads, writes)


def MM(p, out, lhsT, rhs, start, stop, reads, writes):
    return p.op("pe", lambda g: g.matmul(out, lhsT, rhs, start=start, stop=stop), reads, writes)


class Common:
    def __init__(self, p):
        self.p = p
        self.ones_bf = p.sb("ones_bf", [128, 128], BF16)
        self.r_ones = p.res("ones")
        p.op("dve", lambda g: g.memset(self.ones_bf[:], 1.0), (), (self.r_ones,))
        self.eps = p.sb("eps_col", [128, 1], F32)
        self.r_eps = p.res("eps")
        p.op("dve", lambda g: g.memset(self.eps[:], EPS), (), (self.r_eps,))
        self.sq = p.sb("sq", [128, KD, TT], BF16)
        self.r_sq = p.res("sq")
        self.rstd = p.sb("rstd", [128, TT], F32)
        self.r_rstd = p.res("rstd")
        self.ps_n = p.ps("ps_n", [128, TT])
        self.r_psn = p.res("psn")


def load_cols(p, q, dst, r_dst, src_vec, n):
    with p.nc.allow_non_contiguous_dma(reason="tiny param vectors"):
        p.dma(q, dst[:, :n], src_vec.rearrange("(k p) -> p k", p=128), r_dst, (), (r_dst,))


def rmsnorm_fm(p, c, x_ap, r_x, w_col, r_w, h_ap, r_h, n):
    for k in range(KD):
        ACT(p, AF.Square, c.sq[:, k, :n], x_ap[:, k, :n], (r_x,), (c.r_sq,))
    for k in range(KD):
        MM(p, c.ps_n[:, :n], c.ones_bf[:], c.sq[:, k, :n], k == 0, k == KD - 1,
           (c.r_sq, c.r_ones), (c.r_psn,))
    ACT(p, AF.Sqrt, c.rstd[:, :n], c.ps_n[:, :n], (c.r_psn, c.r_eps), (c.r_rstd,),
        scale=1.0 / D, bias=c.eps[:, 0:1])
    p.op("dve", lambda g: g.reciprocal(out=c.rstd[:, :n], in_=c.rstd[:, :n]), (c.r_rstd,), (c.r_rstd,))
    for k in range(KD):
        p.op("dve", lambda g, k=k: g.scalar_tensor_tensor(
            out=h_ap[:, k, :n], in0=x_ap[:, k, :n], scalar=w_col[:, k:k + 1],
            in1=c.rstd[:, :n], op0=ALU.mult, op1=ALU.mult), (r_x, r_w, c.r_rstd), (r_h,))


class MLP:
    def __init__(self, p, c):
        self.p, self.c = p, c
        self.NS = 3
        self.wu = [p.sb("wu%d" % i, [128, KD, 512], BF16) for i in range(self.NS)]
        self.r_wu = [p.res("wu%d" % i) for i in range(self.NS)]
        self.wd = [p.sb("wd%d" % i, [128, 8, 512], BF16) for i in range(self.NS)]
        self.r_wd = [p.res("wd%d" % i) for i in range(self.NS)]
        self.h = [p.sb("mh%d" % i, [128, KD, TT], BF16) for i in range(2)]
        self.r_h = [p.res("mh%d" % i) for i in range(2)]
        self.hid = [p.sb("hid%d" % i, [128, 32, TT], BF16) for i in range(2)]
        self.r_hid = [p.res("hid%d" % i) for i in range(2)]
        self.rl = [p.sb("rl%d" % i, [128, TT], BF16) for i in range(2)]
        self.r_rl = [p.res("rl%d" % i) for i in range(2)]
        self.ps_u = [p.ps("ps_u%d" % i, [128, TT]) for i in range(2)]
        self.r_psu = [p.res("psu%d" % i) for i in range(2)]
        self.ps_d = [p.ps("ps_d%d" % i, [128, TT]) for i in range(4)]
        self.r_psd = [p.res("psd%d" % i) for i in range(4)]
        self.wn = p.sb("mlp_wn", [128, KD], F32)
        self.r_wn = p.res("mlp_wn")
        self.iu = 0
        self.id = 0
        self.ih = 0
        self.ipu = 0

    def load_up(self, w_up, fb):
        s = self.iu % self.NS
        self.iu += 1
        self.p.dma("pool", self.wu[s][:], w_up[:, fb * 512:(fb + 1) * 512].rearrange("(k p) f -> p k f", p=128),
                   self.r_wu[s], (), (self.r_wu[s],))
        return s

    def load_down(self, w_down, half, g):
        s = self.id % self.NS
        self.id += 1
        src = w_down[g * 1024:(g + 1) * 1024, half * 512:(half + 1) * 512].rearrange("(k p) d -> p k d", p=128)
        self.p.dma("pool", self.wd[s][:], src, self.r_wd[s], (), (self.r_wd[s],))
        return s

    def run(self, x_in, x_out, ntiles, wn_vec, w_up, w_down, final_w=None):
        p, c = self.p, self.c
        load_cols(p, "sp", self.wn, self.r_wn, wn_vec, KD)
        if final_w is not None:
            self.fw = p.sb("mlp_fw", [128, KD], F32)
            self.r_fw = p.res("mlp_fw")
            load_cols(p, "sp", self.fw, self.r_fw, final_w, KD)
            self.fo = p.sb("mlp_fo", [128, KD, TT], F32)
            self.r_fo = p.res("mlp_fo")
        xs = [p.sb("mlpx%d" % i, [128, KD, TT], F32) for i in range(2)]
        r_xs = [p.res("mlpx%d" % i) for i in range(2)]
        sched = []
        for t in range(ntiles):
            for fb in range(8):
                sched.append(("u", fb))
            for half in range(2):
                for g in range(4):
                    sched.append(("d", half, g))
        slots = {}
        nxt = [0]

        def prefetch(upto):
            while nxt[0] < len(sched) and nxt[0] <= upto:
                it = sched[nxt[0]]
                if it[0] == "u":
                    slots[nxt[0]] = self.load_up(w_up, it[1])
                else:
                    slots[nxt[0]] = self.load_down(w_down, it[1], it[2])
                nxt[0] += 1

        def load_x(t):
            p.dma("sp", xs[t % 2][:], x_in[:, t * TT:(t + 1) * TT].rearrange("(k q) n -> q k n", q=128),
                  r_xs[t % 2], (), (r_xs[t % 2],))

        def norm(t):
            hs = self.ih % 2
            self.ih += 1
            rmsnorm_fm(p, c, xs[t % 2], r_xs[t % 2], self.wn, self.r_wn, self.h[hs], self.r_h[hs], TT)
            return hs

        prefetch(1)
        load_x(0)
        hs = norm(0)
        si = 0
        for t in range(ntiles):
            hb = t % 2
            xt, r_xt = xs[t % 2], r_xs[t % 2]
            for fb in range(8):
                prefetch(si + self.NS - 1)
                s = slots[si]
                si += 1
                for ft in range(4):
                    pu = self.ipu % 2
                    self.ipu += 1
                    for k in range(KD):
                        MM(p, self.ps_u[pu][:], self.wu[s][:, k, ft * 128:(ft + 1) * 128], self.h[hs][:, k, :],
                           k == 0, k == KD - 1, (self.r_wu[s], self.r_h[hs]), (self.r_psu[pu],))
                    ACT(p, AF.Relu, self.rl[pu][:], self.ps_u[pu][:], (self.r_psu[pu],), (self.r_rl[pu],))
                    f = fb * 4 + ft
                    p.op("dve", lambda g, f=f, pu=pu: g.tensor_tensor(
                        out=self.hid[hb][:, f, :], in0=self.rl[pu][:], in1=self.rl[pu][:], op=ALU.mult),
                        (self.r_rl[pu],), (self.r_hid[hb],))
            if t + 1 < ntiles:
                load_x(t + 1)
                hs_next = norm(t + 1)
            else:
                hs_next = None
            for half in range(2):
                for g in range(4):
                    prefetch(si + self.NS - 1)
                    s = slots[si]
                    si += 1
                    for dt_ in range(4):
                        for kk in range(8):
                            fc = g * 8 + kk
                            MM(p, self.ps_d[dt_][:], self.wd[s][:, kk, dt_ * 128:(dt_ + 1) * 128],
                               self.hid[hb][:, fc, :], fc == 0, fc == 31,
                               (self.r_wd[s], self.r_hid[hb]), (self.r_psd[dt_],))
                for dt_ in range(4):
                    k = half * 4 + dt_
                    p.op("dve", lambda g, k=k, dt_=dt_: g.tensor_tensor(
                        out=xt[:, k, :], in0=xt[:, k, :], in1=self.ps_d[dt_][:], op=ALU.add),
                        (self.r_psd[dt_], r_xt), (r_xt,))
            if final_w is None:
                p.dma("sp", x_out[:, t * TT:(t + 1) * TT].rearrange("(k q) n -> q k n", q=128), xt[:], r_xt,
                      (r_xt,), (), is_output=True)
            else:
                for k in range(KD):
                    ACT(p, AF.Square, c.sq[:, k, :], xt[:, k, :], (r_xt,), (c.r_sq,))
                for k in range(KD):
                    MM(p, c.ps_n[:], c.ones_bf[:], c.sq[:, k, :], k == 0, k == KD - 1, (c.r_sq, c.r_ones), (c.r_psn,))
                ACT(p, AF.Sqrt, c.rstd[:], c.ps_n[:], (c.r_psn, c.r_eps), (c.r_rstd,), scale=1.0 / D, bias=c.eps[:, 0:1])
                p.op("dve", lambda g: g.reciprocal(out=c.rstd[:], in_=c.rstd[:]), (c.r_rstd,), (c.r_rstd,))
                for k in range(KD):
                    p.op("dve", lambda g, k=k: g.scalar_tensor_tensor(
                        out=self.fo[:, k, :], in0=xt[:, k, :], scalar=self.fw[:, k:k + 1], in1=c.rstd[:],
                        op0=ALU.mult, op1=ALU.mult), (r_xt, self.r_fw, c.r_rstd), (self.r_fo,))
                p.dma("sp", x_out[:, t * TT:(t + 1) * TT].rearrange("(k q) n -> q k n", q=128), self.fo[:], self.r_fo,
                      (self.r_fo,), (), is_output=True)
            hs = hs_next


class WStream:
    def __init__(self, p, ns=3):
        self.p = p
        self.ns = ns
        self.t = [p.sb("ws%d" % i, [128, KD, 512], BF16) for i in range(ns)]
        self.r = [p.res("ws%d" % i) for i in range(ns)]
        self.sched = []
        self.issued = 0
        self.used = 0

    def plan(self, item):
        self.sched.append(item)

    def plan_cols(self, w, c0, n=512):
        self.plan([(0, n, w[:, c0:c0 + n].rearrange("(k p) f -> p k f", p=128))])

    def plan_rows(self, w, r0, c0, n=512):
        self.plan([(0, n, w[r0:r0 + 1024, c0:c0 + n].rearrange("(k p) f -> p k f", p=128))])

    def _issue(self, upto):
        while self.issued < len(self.sched) and self.issued <= upto:
            s = self.issued % self.ns
            for (c0, n, src) in self.sched[self.issued]:
                self.p.dma("pool", self.t[s][:, :, c0:c0 + n], src, self.r[s], (), (self.r[s],))
            self.issued += 1

    def next(self):
        i = self.used
        self._issue(i + self.ns - 1)
        self.used += 1
        return self.t[i % self.ns], self.r[i % self.ns]


class Banks:
    def __init__(self, p, n=6):
        self.b = [p.ps("bk%d" % i, [128, 512]) for i in range(n)]
        self.r = [p.res("bk%d" % i) for i in range(n)]
        self.tb = p.ps("bkT", [128, 1024], BF16)
        self.r_tb = p.res("bkT")


HG_C = 64


class HG1:
    def __init__(self, p, c, ws, bk, cd):
        self.p, self.c, self.ws, self.bk = p, c, ws, bk
        f32t = lambda n: p.sb(n, [128, TT], F32)
        self.xs = [p.sb("g1x%d" % i, [128, KD, TT], F32) for i in range(2)]
        self.r_xs = [p.res("g1x%d" % i) for i in range(2)]
        self.h = [p.sb("g1h%d" % i, [128, KD, TT], BF16) for i in range(1)]
        self.r_h = [p.res("g1h%d" % i) for i in range(1)]
        self.vtok = p.sb("vtok", [64, 8, 1024], BF16)
        self.r_vtok = p.res("vtok")
        self.qt = p.sb("qt", [128, 8, TT], BF16)
        self.kt = p.sb("kt", [128, 8, TT], BF16)
        self.kh = p.sb("kh", [128, 8, TT], BF16)
        self.r_qt, self.r_kt, self.r_kh = p.res("qt"), p.res("kt"), p.res("kh")
        names = ("sig", "ksig", "lf", "b", "Bg", "eb", "enb", "eB", "d2")
        self.Tm = [{n: f32t(n + str(i)) for n in names} for i in range(2)]
        self.Rt = [{n: p.res(n + str(i)) for n in names} for i in range(2)]
        self.a_all = p.sb("a_all", [128, 8, 8], F32)
        self.r_a = p.res("a_all")
        self.bgl = p.sb("bgl", [128, 8], F32)
        self.r_bgl = p.res("bgl")
        self.S = p.sb("S32", [128, 8, 128], F32)
        self.Sb = p.sb("Sbf", [128, 8, 128], BF16)
        self.r_S, self.r_Sb = p.res("S32"), p.res("Sbf")
        self.attn = p.sb("attn", [64, 8, 64], BF16)
        self.r_attn = p.res("attn")
        self.khat = p.sb("khat", [64, 8, 128], BF16)
        self.r_khat = p.res("khat")
        self.ost = p.sb("ost", [128, 8, TT], F32)
        self.r_ost = p.res("ost")
        self.qd = [p.sb("qdst%d" % i, [128, TT], BF16) for i in range(2)]
        self.r_qd = [p.res("qdst%d" % i) for i in range(2)]
        self.sg = [p.sb("sgst%d" % i, [128, TT], BF16) for i in range(2)]
        self.r_sg = [p.res("sgst%d" % i) for i in range(2)]
        self.wn = p.sb("g1wn", [128, KD], F32)
        self.r_wn = p.res("g1wn")
        self.alb = p.sb("alb", [128, 2, 8], F32)
        self.lbt = p.sb("lbt", [128, 8, 8], F32)
        self.r_lb = p.res("lb")
        self.ones32 = p.sb("ones32", [128, TT], F32)
        self.r_ones32 = p.res("ones32")
        self.cd = cd

    def setup(self, norm_w, alb_dram, layer):
        p = self.p
        load_cols(p, "sp", self.wn, self.r_wn, norm_w, KD)
        with p.nc.allow_non_contiguous_dma(reason="tiny"):
            p.dma("sp", self.alb[:], alb_dram.rearrange("l (h q) -> q l h", q=128), self.r_lb, (), (self.r_lb,))
        L = self.lbt
        R = (self.r_lb,)
        a0, a1 = self.alb[:, 0, :], self.alb[:, 1, :]
        p.op("dve", lambda g: g.tensor_tensor(out=L[:, 0, :], in0=a0, in1=a1, op=ALU.max), R, R)
        p.op("dve", lambda g: g.tensor_tensor(out=L[:, 1, :], in0=a0, in1=L[:, 0, :], op=ALU.subtract), R, R)
        p.op("dve", lambda g: g.tensor_tensor(out=L[:, 2, :], in0=a1, in1=L[:, 0, :], op=ALU.subtract), R, R)
        ACT(p, AF.Exp, L[:, 1:3, :], L[:, 1:3, :], R, R)
        p.op("dve", lambda g: g.tensor_tensor(out=L[:, 3, :], in0=L[:, 1, :], in1=L[:, 2, :], op=ALU.add), R, R)
        p.op("dve", lambda g: g.reciprocal(out=L[:, 3, :], in_=L[:, 3, :]), R, R)
        p.op("dve", lambda g: g.tensor_tensor(out=L[:, 4, :], in0=L[:, 1, :], in1=L[:, 3, :], op=ALU.mult), R, R)
        if layer == 0:
            p.op("dve", lambda g: g.tensor_copy(out=L[:, 5, :], in_=L[:, 4, :]), R, R)
        else:
            p.op("dve", lambda g: g.tensor_tensor(out=L[:, 5, :], in0=L[:, 2, :], in1=L[:, 3, :], op=ALU.mult), R, R)
            p.op("dve", lambda g: g.tensor_tensor(out=L[:, 5, :], in0=L[:, 5, :], in1=L[:, 4, :], op=ALU.add), R, R)
        p.op("dve", lambda g: g.tensor_tensor(out=L[:, 6, :], in0=L[:, 5, :], in1=L[:, 4, :], op=ALU.subtract), R, R)
        p.op("dve", lambda g: g.tensor_scalar(out=L[:, 7, :], in0=L[:, 6, :], scalar1=-1.0, scalar2=1.0,
                                              op0=ALU.mult, op1=ALU.add), R, R)
        p.op("dve", lambda g: g.memset(self.ones32[:], 1.0), (), (self.r_ones32,))
        p.op("dve", lambda g: g.memset(self.S[:], 0.0), (), (self.r_S,))
        p.op("dve", lambda g: g.memset(self.Sb[:], 0.0), (), (self.r_Sb,))
        p.op("dve", lambda g: g.memset(self.bgl[:], 0.0), (), (self.r_bgl,))

    def plan_weights(self, w_in, ntiles):
        for t in range(ntiles):
            for hb in range(4):
                self.ws.plan([(0, 256, w_in[:, hb * 256:(hb + 1) * 256].rearrange("(k p) f -> p k f", p=128)),
                              (256, 256, w_in[:, 1024 + hb * 256:1024 + (hb + 1) * 256].rearrange("(k p) f -> p k f", p=128))])
            for gb in range(2):
                self.ws.plan_cols(w_in, 3072 + gb * 512)
            for vb in range(2):
                self.ws.plan_cols(w_in, 2048 + vb * 512)

    def tile(self, t, xT_dram, o_loc, qdec, sgo):
        p, c, bk = self.p, self.c, self.bk
        T0 = t * TT
        xs, r_xs = self.xs[t % 2], self.r_xs[t % 2]
        p.dma("sp", xs[:], xT_dram[:, T0:T0 + TT].rearrange("(k q) n -> q k n", q=128), r_xs, (), (r_xs,))
        h, r_h = self.h[0], self.r_h[0]
        rmsnorm_fm(p, c, xs, r_xs, self.wn, self.r_wn, h, r_h, TT)
        L = self.lbt
        for hb in range(4):
            w, r_w = self.ws.next()
            for hh in range(2):
                hd = hb * 2 + hh
                pq, r_pq = bk.b[hd % 2], bk.r[hd % 2]
                pf, r_pf = bk.b[2 + hd % 2], bk.r[2 + hd % 2]
                for k in range(KD):
                    MM(p, pq[:], w[:, k, hh * 128:(hh + 1) * 128], h[:, k, :], k == 0, k == KD - 1, (r_w, r_h), (r_pq,))
                for k in range(KD):
                    MM(p, pf[:], w[:, k, 256 + hh * 128:256 + (hh + 1) * 128], h[:, k, :], k == 0, k == KD - 1,
                       (r_w, r_h), (r_pf,))
                lb_c, oml_c = L[:, 6, hd:hd + 1], L[:, 7, hd:hd + 1]
                Tm, rt = self.Tm[hd % 2], self.Rt[hd % 2]
                ACT(p, AF.Sigmoid, Tm["sig"][:], pf[:], (r_pf,), (rt["sig"],))
                ACT(p, AF.Sigmoid, Tm["ksig"][:], pf[:], (r_pf,), (rt["ksig"],), scale=-1.0)
                p.op("dve", lambda g: g.tensor_scalar(out=Tm["lf"][:], in0=Tm["sig"][:], scalar1=oml_c, scalar2=lb_c,
                                                      op0=ALU.mult, op1=ALU.add), (rt["sig"], self.r_lb), (rt["lf"],))
                p.op("pool", lambda g: g.tensor_scalar(out=Tm["lf"][:], in0=Tm["lf"][:], scalar1=1e-30, scalar2=None, op0=ALU.max),
                     (rt["lf"],), (rt["lf"],))
                ACT(p, AF.Ln, Tm["lf"][:], Tm["lf"][:], (rt["lf"],), (rt["lf"],))
                p.op("pool", lambda g: g.tensor_scalar(out=Tm["ksig"][:], in0=Tm["ksig"][:], scalar1=oml_c, scalar2=None, op0=ALU.mult),
                     (rt["ksig"], self.r_lb), (rt["ksig"],))
                p.op("dve", lambda g: g.tensor_tensor_scan(out=Tm["b"][:], data0=self.cd["reset"][:], data1=Tm["lf"][:],
                                                           initial=0.0, op0=ALU.mult, op1=ALU.add),
                     (rt["lf"], self.cd["r"]), (rt["b"],))
                p.op("dve", lambda g: g.tensor_tensor_scan(out=Tm["Bg"][:], data0=self.ones32[:], data1=Tm["lf"][:],
                                                           initial=self.bgl[:, hd:hd + 1], op0=ALU.mult, op1=ALU.add),
                     (rt["lf"], self.r_ones32, self.r_bgl), (rt["Bg"],))
                p.op("dve", lambda g: g.tensor_copy(out=self.bgl[:, hd:hd + 1], in_=Tm["Bg"][:, TT - 1:TT]),
                     (rt["Bg"],), (self.r_bgl,))
                ACT(p, AF.Exp, Tm["eb"][:], Tm["b"][:], (rt["b"],), (rt["eb"],))
                ACT(p, AF.Exp, Tm["enb"][:], Tm["b"][:], (rt["b"],), (rt["enb"],), scale=-1.0)
                ACT(p, AF.Exp, Tm["eB"][:], Tm["Bg"][:], (rt["Bg"],), (rt["eB"],))
                b3 = Tm["b"][:].rearrange("q (c s) -> q c s", s=HG_C)
                p.op("pool", lambda g: g.tensor_tensor(out=Tm["d2"][:].rearrange("q (c s) -> q c s", s=HG_C),
                                                       in0=b3[:, :, HG_C - 1:HG_C].to_broadcast([128, 8, HG_C]),
                                                       in1=b3, op=ALU.subtract), (rt["b"],), (rt["d2"],))
                ACT(p, AF.Exp, Tm["d2"][:], Tm["d2"][:], (rt["d2"],), (rt["d2"],))
                p.op("dve", lambda g: g.tensor_copy(out=self.a_all[:, hd, :], in_=Tm["eb"][:, HG_C - 1::HG_C]),
                     (rt["eb"],), (self.r_a,))
                p.op("dve", lambda g: g.tensor_tensor(out=self.qt[:, hd, :], in0=pq[:], in1=Tm["eb"][:], op=ALU.mult),
                     (r_pq, rt["eb"]), (self.r_qt,))
                qd, r_qd = self.qd[hd % 2], self.r_qd[hd % 2]
                p.op("dve", lambda g: g.tensor_tensor(out=qd[:], in0=pq[:], in1=Tm["eB"][:], op=ALU.mult),
                     (r_pq, rt["eB"]), (r_qd,))
                p.dma("sp", qdec[hd * 128:(hd + 1) * 128, T0:T0 + TT], qd[:], r_qd, (r_qd,), (), is_output=True)
                p.op("dve", lambda g: g.tensor_tensor(out=self.kt[:, hd, :], in0=Tm["ksig"][:], in1=Tm["enb"][:], op=ALU.mult),
                     (rt["ksig"], rt["enb"]), (self.r_kt,))
                p.op("dve", lambda g: g.tensor_tensor(out=self.kh[:, hd, :], in0=Tm["ksig"][:], in1=Tm["d2"][:], op=ALU.mult),
                     (rt["ksig"], rt["d2"]), (self.r_kh,))
        for gb in range(2):
            w, r_w = self.ws.next()
            for hh in range(4):
                hd = gb * 4 + hh
                pg, r_pg = bk.b[4 + hd % 2], bk.r[4 + hd % 2]
                for k in range(KD):
                    MM(p, pg[:], w[:, k, hh * 128:(hh + 1) * 128], h[:, k, :], k == 0, k == KD - 1, (r_w, r_h), (r_pg,))
                sg, r_sg = self.sg[hd % 2], self.r_sg[hd % 2]
                ACT(p, AF.Silu, sg[:], pg[:], (r_pg,), (r_sg,))
                p.dma("sp", sgo[hd * 128:(hd + 1) * 128, T0:T0 + TT], sg[:], r_sg, (r_sg,), (), is_output=True)
        for vb in range(2):
            w, r_w = self.ws.next()
            for ci in range(8):
                pv, r_pv = bk.b[4 + ci % 2], bk.r[4 + ci % 2]
                for k in range(KD):
                    MM(p, pv[0:64, :], h[:, k, ci * HG_C:(ci + 1) * HG_C], w[:, k, :], k == 0, k == KD - 1,
                       (r_w, r_h), (r_pv,))
                ACT(p, AF.Copy, self.vtok[:, ci, vb * 512:(vb + 1) * 512], pv[0:64, :], (r_pv,), (self.r_vtok,))
        pa, r_pa = bk.b[4], bk.r[4]
        po, r_po = bk.b[5], bk.r[5]
        pU, r_pU = (bk.b[0], bk.b[1]), (bk.r[0], bk.r[1])
        for ci in range(8):
            cs = slice(ci * HG_C, (ci + 1) * HG_C)
            for hd in range(8):
                MM(p, pa[0:64, hd * 64:(hd + 1) * 64], self.kt[:, hd, cs], self.qt[:, hd, cs], True, True,
                   (self.r_kt, self.r_qt), (r_pa,))
            p.op("dve", lambda g: g.tensor_tensor(out=self.attn[:], in0=pa[0:64, :].rearrange("q (h s) -> q h s", s=64),
                                                  in1=self.cd["tri"][:].unsqueeze(1).to_broadcast([64, 8, 64]),
                                                  op=ALU.mult), (r_pa, self.cd["r"]), (self.r_attn,))
            for hd in range(8):
                p.op("pe", lambda g, hd=hd: g.transpose(bk.tb[0:64, hd * 128:(hd + 1) * 128], self.kh[:, hd, cs],
                                                        self.cd["ident"][:]),
                     (self.r_kh, self.cd["r"]), (bk.r_tb,))
            ACT(p, AF.Copy, self.khat[:].rearrange("q h d -> q (h d)"), bk.tb[0:64, :], (bk.r_tb,), (self.r_khat,))
            for hd in range(8):
                MM(p, pU[hd // 4][:, (hd % 4) * 128:(hd % 4 + 1) * 128], self.khat[:, hd, :],
                   self.vtok[:, ci, hd * 128:(hd + 1) * 128], True, True,
                   (self.r_khat, self.r_vtok), (r_pU[hd // 4],))
            for hd in range(8):
                MM(p, po[:, hd * 64:(hd + 1) * 64], self.vtok[:, ci, hd * 128:(hd + 1) * 128], self.attn[:, hd, :],
                   True, False, (self.r_vtok, self.r_attn), (r_po,))
                MM(p, po[:, hd * 64:(hd + 1) * 64], self.Sb[:, hd, :], self.qt[:, hd, cs],
                   False, True, (self.r_Sb, self.r_qt), (r_po,))
            ACT(p, AF.Copy, self.ost[:, :, cs], po[:].rearrange("q (h s) -> q h s", s=64), (r_po,), (self.r_ost,))
            for hd in range(8):
                p.op("dve", lambda g, hd=hd: g.scalar_tensor_tensor(
                    out=self.S[:, hd, :], in0=self.S[:, hd, :], scalar=self.a_all[:, hd, ci:ci + 1],
                    in1=pU[hd // 4][:, (hd % 4) * 128:(hd % 4 + 1) * 128], op0=ALU.mult, op1=ALU.add),
                    (self.r_S, self.r_a, r_pU[hd // 4]), (self.r_S,))
            ACT(p, AF.Copy, self.Sb[:], self.S[:], (self.r_S,), (self.r_Sb,))
        p.dma("sp", o_loc[:, T0:T0 + TT].rearrange("(h q) n -> q h n", q=128), self.ost[:], self.r_ost,
              (self.r_ost,), (), is_output=True)

    def finish(self, s_fin, dec):
        p = self.p
        p.dma("sp", s_fin, self.S[:], self.r_S, (self.r_S,), (), is_output=True)
        ACT(p, AF.Exp, self.lbt[:, 0, :], self.bgl[:], (self.r_bgl,), (self.r_lb,))
        p.dma("sp", dec, self.lbt[:, 0, :], self.r_lb, (self.r_lb,), (), is_output=True)


def load_consts_hg(p, reset_d, tri_d, ident_d):
    cd = {"r": p.res("cd")}
    cd["reset"] = p.sb("c_reset", [128, TT], F32)
    cd["tri"] = p.sb("c_tri", [64, 64], BF16)
    cd["ident"] = p.sb("c_ident", [128, 128], BF16)
    p.dma("sp", cd["reset"][:], reset_d, cd["r"], (), (cd["r"],))
    p.dma("sp", cd["tri"][:], tri_d, cd["r"], (), (cd["r"],))
    p.dma("sp", cd["ident"][:], ident_d, cd["r"], (), (cd["r"],))
    return cd


class HG2:
    def __init__(self, p, c, ws, bk):
        self.p, self.c, self.ws, self.bk = p, c, ws, bk
        self.Sin = p.sb("Sin", [128, 8, 128], F32)
        self.SinB = p.sb("SinB", [128, 8, 128], BF16)
        self.r_Sin, self.r_SinB = p.res("Sin"), p.res("SinB")
        self.Sf = [p.sb("Sf%d" % i, [128, 8, 128], F32) for i in range(2)]
        self.r_Sf = [p.res("Sf%d" % i) for i in range(2)]
        self.dcl = p.sb("dcl", [128, 8, 8], F32)
        self.selv = p.sb("selv", [128, 8], F32)
        self.al = p.sb("al", [128, 8], F32)
        self.r_sm = p.res("hg2small")
        self.gw = p.sb("gw", [128, 1], F32)
        self.r_gw = p.res("gw")
        self.xs = [p.sb("g2x%d" % i, [128, KD, TT], F32) for i in range(2)]
        self.r_xs = [p.res("g2x%d" % i) for i in range(2)]
        self.ot = p.sb("g2o", [128, 8, TT], F32)
        self.qd = p.sb("g2qd", [128, 8, TT], BF16)
        self.sgt = p.sb("g2sg", [128, 8, TT], BF16)
        self.on = p.sb("g2on", [128, 8, TT], BF16)
        self.tmp = p.sb("g2tmp", [128, TT], F32)
        self.r_ot, self.r_qd, self.r_sgt, self.r_on, self.r_tmp = (p.res("g2o"), p.res("g2qd"), p.res("g2sg"),
                                                                   p.res("g2on"), p.res("g2tmp"))
        self.eps128 = c.eps

    def prefix(self, S_all, dec_all, selv_d, gnorm_w):
        p = self.p
        R = (self.r_sm,)
        with p.nc.allow_non_contiguous_dma(reason="tiny"):
            p.dma("sp", self.dcl[:], dec_all.rearrange("c q h -> q c h"), self.r_sm, (), R)
            p.dma("sp", self.selv[:], selv_d, self.r_sm, (), R)
            p.dma("sp", self.gw[:], gnorm_w.rearrange("(q o) -> q o", o=1), self.r_gw, (), (self.r_gw,))
        p.op("dve", lambda g: g.memset(self.Sin[:], 0.0), (), (self.r_Sin,))
        for cp in range(8):
            sf, r_sf = self.Sf[cp % 2], self.r_Sf[cp % 2]
            p.dma("sp", sf[:], S_all[cp], r_sf, (), (r_sf,))
            sel_c = self.selv[:, cp:cp + 1]
            p.op("dve", lambda g: g.tensor_scalar(out=self.al[:], in0=self.dcl[:, cp, :], scalar1=-1.0, scalar2=sel_c,
                                                  op0=ALU.add, op1=ALU.mult), R, R)
            p.op("dve", lambda g: g.tensor_scalar(out=self.al[:], in0=self.al[:], scalar1=1.0, scalar2=None,
                                                  op0=ALU.add), R, R)
            p.op("dve", lambda g: g.tensor_scalar(out=sf[:], in0=sf[:], scalar1=sel_c, scalar2=None, op0=ALU.mult),
                 (r_sf, self.r_sm), (r_sf,))
            for hd in range(8):
                p.op("dve", lambda g, hd=hd: g.scalar_tensor_tensor(
                    out=self.Sin[:, hd, :], in0=self.Sin[:, hd, :], scalar=self.al[:, hd:hd + 1], in1=sf[:, hd, :],
                    op0=ALU.mult, op1=ALU.add), (self.r_Sin, self.r_sm, r_sf), (self.r_Sin,))
        ACT(p, AF.Copy, self.SinB[:], self.Sin[:], (self.r_Sin,), (self.r_SinB,))

    def plan_weights(self, w_out, ntiles):
        for t in range(ntiles):
            for blk in range(2):
                self.ws.plan_cols(w_out, blk * 512)

    def tile(self, t, x_in, x_out, o_loc, qdec, sgo):
        p, c, bk = self.p, self.c, self.bk
        T0 = t * TT
        xs, r_xs = self.xs[t % 2], self.r_xs[t % 2]
        p.dma("sp", xs[:], x_in[:, T0:T0 + TT].rearrange("(k q) n -> q k n", q=128), r_xs, (), (r_xs,))
        p.dma("sp", self.ot[:], o_loc[:, T0:T0 + TT].rearrange("(h q) n -> q h n", q=128), self.r_ot, (), (self.r_ot,))
        p.dma("sp", self.qd[:], qdec[:, T0:T0 + TT].rearrange("(h q) n -> q h n", q=128), self.r_qd, (), (self.r_qd,))
        p.dma("sp", self.sgt[:], sgo[:, T0:T0 + TT].rearrange("(h q) n -> q h n", q=128), self.r_sgt, (), (self.r_sgt,))
        for hd in range(8):
            pc, r_pc = bk.b[hd % 2], bk.r[hd % 2]
            MM(p, pc[:], self.SinB[:, hd, :], self.qd[:, hd, :], True, True, (self.r_SinB, self.r_qd), (r_pc,))
            p.op("dve", lambda g: g.tensor_tensor(out=self.ot[:, hd, :], in0=self.ot[:, hd, :], in1=pc[:], op=ALU.add),
                 (self.r_ot, r_pc), (self.r_ot,))
            ACT(p, AF.Square, c.sq[:, hd, :], self.ot[:, hd, :], (self.r_ot,), (c.r_sq,))
            pn, r_pn = bk.b[2 + hd % 2], bk.r[2 + hd % 2]
            MM(p, pn[:], c.ones_bf[:], c.sq[:, hd, :], True, True, (c.r_sq, c.r_ones), (r_pn,))
            ACT(p, AF.Sqrt, c.rstd[:], pn[:], (r_pn, c.r_eps), (c.r_rstd,), scale=1.0 / 128, bias=c.eps[:, 0:1])
            p.op("dve", lambda g: g.reciprocal(out=c.rstd[:], in_=c.rstd[:]), (c.r_rstd,), (c.r_rstd,))
            p.op("dve", lambda g: g.scalar_tensor_tensor(out=self.tmp[:], in0=self.ot[:, hd, :], scalar=self.gw[:, 0:1],
                                                         in1=c.rstd[:], op0=ALU.mult, op1=ALU.mult),
                 (self.r_ot, self.r_gw, c.r_rstd), (self.r_tmp,))
            p.op("pool", lambda g: g.tensor_tensor(out=self.on[:, hd, :], in0=self.tmp[:], in1=self.sgt[:, hd, :],
                                                   op=ALU.mult), (self.r_tmp, self.r_sgt), (self.r_on,))
        for blk in range(2):
            w, r_w = self.ws.next()
            for dt_ in range(4):
                po, r_po = bk.b[4 + dt_ % 2], bk.r[4 + dt_ % 2]
                for k in range(KD):
                    MM(p, po[:], w[:, k, dt_ * 128:(dt_ + 1) * 128], self.on[:, k, :], k == 0, k == KD - 1,
                       (r_w, self.r_on), (r_po,))
                kk = blk * 4 + dt_
                p.op("dve", lambda g: g.tensor_tensor(out=xs[:, kk, :], in0=xs[:, kk, :], in1=po[:], op=ALU.add),
                     (r_xs, r_po), (r_xs,))
        p.dma("sp", x_out[:, T0:T0 + TT].rearrange("(k q) n -> q k n", q=128), xs[:], r_xs, (r_xs,), (), is_output=True)


def host_consts_hg():
    import ml_dtypes
    reset = np.ones((128, TT), np.float32)
    reset[:, ::HG_C] = 0.0
    tri = (np.arange(64)[:, None] <= np.arange(64)[None, :]).astype(ml_dtypes.bfloat16)
    ident = np.eye(128).astype(ml_dtypes.bfloat16)
    return {"c_reset": reset, "c_tri": tri, "c_ident": ident}


NSA_SLOPES = [2.0 ** (-(h + 1) / 2.0) for h in range(16)]
QK_SCALE = 0.125
NAUG = 7
KROWS = 96


class KVPhase:
    def __init__(self, p, c, ws, bk):
        self.p, self.c, self.ws, self.bk = p, c, ws, bk
        self.xs = [p.sb("kvx%d" % i, [128, KD, TT], F32) for i in range(2)]
        self.r_xs = [p.res("kvx%d" % i) for i in range(2)]
        self.h = p.sb("kvh", [128, KD, TT], BF16)
        self.r_h = p.res("kvh")
        self.wn = p.sb("kvwn", [128, KD], F32)
        self.r_wn = p.res("kvwn")
        self.st = [p.sb("kvst%d" % i, [128, TT], BF16) for i in range(2)]
        self.r_st = [p.res("kvst%d" % i) for i in range(2)]
        self.sq32 = p.sb("kvsq", [128, TT], BF16)
        self.r_sq32 = p.res("kvsq")
        self.bd = p.sb("kvbd", [128, 128], BF16)
        self.r_bd = p.res("kvbd")
        self.kmx = p.sb("kvkmx", [128, 4], F32)
        self.mtmp = p.sb("kvmt", [128, 1], F32)
        self.r_kmx = p.res("kvkmx")
        self.vst = [p.sb("kvvst%d" % i, [128, 8, 65], BF16) for i in range(2)]
        self.r_vst = [p.res("kvvst%d" % i) for i in range(2)]

    def setup(self, norm_w):
        p = self.p
        load_cols(p, "sp", self.wn, self.r_wn, norm_w, KD)
        p.op("dve", lambda g: g.memset(self.bd[:], 0.0), (), (self.r_bd,))
        p.op("dve", lambda g: g.memset(self.bd[0:64, 0:64], 1.0), (), (self.r_bd,))
        p.op("dve", lambda g: g.memset(self.bd[64:128, 64:128], 1.0), (), (self.r_bd,))
        p.op("dve", lambda g: g.memset(self.kmx[:], 0.0), (), (self.r_kmx,))
        for i in range(2):
            p.op("dve", lambda g, i=i: g.memset(self.vst[i][:], 1.0), (), (self.r_vst[i],))

    def plan_weights(self, kv_w, ntiles):
        re = lambda a: a.rearrange("(k q) f -> q k f", q=128)
        for t in range(ntiles):
            self.ws.plan([(0, 512, re(kv_w[:, 0:512]))])
            self.ws.plan([(0, 256, re(kv_w[:, 512:768])), (256, 256, re(kv_w[:, 1024:1280]))])
            self.ws.plan([(0, 256, re(kv_w[:, 768:1024])), (256, 256, re(kv_w[:, 1280:1536]))])

    def tile(self, t, x_in, kT01, kT24, vaug):
        p, c, bk = self.p, self.c, self.bk
        T0 = t * TT
        xs, r_xs = self.xs[t % 2], self.r_xs[t % 2]
        p.dma("sp", xs[:], x_in[:, T0:T0 + TT].rearrange("(k q) n -> q k n", q=128), r_xs, (), (r_xs,))
        rmsnorm_fm(p, c, xs, r_xs, self.wn, self.r_wn, self.h, self.r_h, TT)
        h, r_h = self.h, self.r_h
        n = 0
        for blk, dst in ((0, kT01), (1, kT24)):
            w, r_w = self.ws.next()
            for ct in range(4):
                pp, r_pp = bk.b[n % 2], bk.r[n % 2]
                st, r_st = self.st[n % 2], self.r_st[n % 2]
                n += 1
                for k in range(KD):
                    MM(p, pp[:], w[:, k, ct * 128:(ct + 1) * 128], h[:, k, :], k == 0, k == KD - 1, (r_w, r_h), (r_pp,))
                ACT(p, AF.Copy, st[:], pp[:], (r_pp,), (r_st,))
                p.dma("sp", dst[ct * 128:(ct + 1) * 128, T0:T0 + TT], st[:], r_st, (r_st,), (), is_output=True)
                if blk == 1:
                    ACT(p, AF.Square, self.sq32[:], pp[:], (r_pp,), (self.r_sq32,))
                    pm, r_pm = bk.b[2], bk.r[2]
                    MM(p, pm[:], self.bd[:], self.sq32[:], True, True, (self.r_bd, self.r_sq32), (r_pm,))
                    p.op("dve", lambda g: g.reduce_max(out=self.mtmp[:], in_=pm[:], axis=AX.X), (r_pm,), (self.r_kmx,))
                    p.op("dve", lambda g, ct=ct: g.tensor_tensor(out=self.kmx[:, ct:ct + 1], in0=self.kmx[:, ct:ct + 1],
                                                                 in1=self.mtmp[:], op=ALU.max), (self.r_kmx,), (self.r_kmx,))
        w, r_w = self.ws.next()
        for sub in range(TT // 128):
            pp, r_pp = bk.b[3 + sub % 2], bk.r[3 + sub % 2]
            vst, r_vst = self.vst[sub % 2], self.r_vst[sub % 2]
            for k in range(KD):
                MM(p, pp[:], h[:, k, sub * 128:(sub + 1) * 128], w[:, k, :], k == 0, k == KD - 1, (r_w, r_h), (r_pp,))
            ACT(p, AF.Copy, vst[:, :, 0:64], pp[:].rearrange("q (s d) -> q s d", d=64), (r_pp,), (r_vst,))
            ch = t * (TT // 128) + sub
            p.dma("sp", vaug[ch], vst[:], r_vst, (r_vst,), (), is_output=True)

    def finish(self, kmx_out):
        p = self.p
        p.dma("sp", kmx_out, self.kmx[:], self.r_kmx, (self.r_kmx,), (), is_output=True)


class CMPPhase:
    def __init__(self, p, c, bk, NB):
        self.p, self.c, self.bk = p, c, bk
        self.NB = NB
        self.kin = p.sb("cmkin", [64, 4, 16 * NB + 16], BF16)
        self.r_kin = p.res("cmkin")
        self.w1 = p.sb("cmw1", [64, 32, 256], BF16)
        self.r_w1 = p.res("cmw1")
        self.w2 = p.sb("cmw2", [128, 2, 64], BF16)
        self.r_w2 = p.res("cmw2")
        self.peT = p.sb("cmpe", [64, 32], BF16)
        self.r_pe = p.res("cmpe")
        self.c1 = p.sb("cmc1", [128, 2], F32)
        self.r_c1 = p.res("cmc1")
        self.z = p.sb("cmz", [128, NB], F32)
        self.z2 = p.sb("cmz2", [128, NB], F32)
        self.r_z, self.r_z2 = p.res("cmz"), p.res("cmz2")
        self.gl = p.sb("cmgl", [128, 2, NB], BF16)
        self.r_gl = p.res("cmgl")
        self.ko = p.sb("cmko", [64, 4, NB], BF16)
        self.r_ko = p.res("cmko")
        self.vo = p.sb("cmvo", [NB, 4, 65], BF16)
        self.r_vo = p.res("cmvo")
        self.sq = p.sb("cmsq", [64, NB], BF16)
        self.r_sq = p.res("cmsq")
        self.kmx = p.sb("cmkmx", [64, 4], F32)
        self.r_kmx = p.res("cmkmx")

    def run(self, kin_d, vin_d, pe_k, w1_k, w2_k, pe_v, w1_v, w2_v, kc_out, vc_out, kmx_out):
        p, c, bk = self.p, self.c, self.bk
        NB = self.NB
        p.op("dve", lambda g: g.memset(self.vo[:], 1.0), (), (self.r_vo,))
        for si, (src, pe, w1, w2) in enumerate(((kin_d, pe_k, w1_k, w2_k), (vin_d, pe_v, w1_v, w2_v))):
            p.dma("sp", self.kin[:], src.rearrange("g d n -> d g n"), self.r_kin, (), (self.r_kin,))
            p.dma("pool", self.w1[:], w1.rearrange("(j d) m -> d j m", d=64), self.r_w1, (), (self.r_w1,))
            p.dma("pool", self.w2[:], w2.rearrange("(k q) d -> q k d", q=128), self.r_w2, (), (self.r_w2,))
            with p.nc.allow_non_contiguous_dma(reason="tiny pe"):
                p.dma("pool", self.peT[:], pe.rearrange("j d -> d j"), self.r_pe, (), (self.r_pe,))
            for mt in range(2):
                pb, r_pb = bk.b[0], bk.r[0]
                for j in range(32):
                    MM(p, pb[:, 0:1], self.w1[:, j, mt * 128:(mt + 1) * 128], self.peT[:, j:j + 1], j == 0, j == 31,
                       (self.r_w1, self.r_pe), (r_pb,))
                p.op("dve", lambda g, mt=mt: g.tensor_copy(out=self.c1[:, mt:mt + 1], in_=pb[:, 0:1]), (r_pb,), (self.r_c1,))
            for gi in range(4):
                for mt in range(2):
                    ph, r_ph = bk.b[1 + mt], bk.r[1 + mt]
                    for j in range(32):
                        MM(p, ph[:, 0:NB], self.w1[:, j, mt * 128:(mt + 1) * 128], self.kin[:, gi, j:j + 16 * (NB - 1) + 1:16],
                           j == 0, j == 31, (self.r_w1, self.r_kin), (r_ph,))
                    ACT(p, AF.Identity, self.z[:], ph[:, 0:NB], (r_ph, self.r_c1), (self.r_z,), bias=self.c1[:, mt:mt + 1])
                    p.op("dve", lambda g: g.tensor_tensor(out=self.z2[:], in0=self.z[:], in1=self.z[:], op=ALU.mult),
                         (self.r_z,), (self.r_z2,))
                    p.op("dve", lambda g: g.tensor_scalar(out=self.z2[:], in0=self.z2[:], scalar1=0.044715, scalar2=1.0,
                                                          op0=ALU.mult, op1=ALU.add), (self.r_z2,), (self.r_z2,))
                    p.op("dve", lambda g: g.tensor_tensor(out=self.z2[:], in0=self.z2[:], in1=self.z[:], op=ALU.mult),
                         (self.r_z2, self.r_z), (self.r_z2,))
                    ACT(p, AF.Sigmoid, self.z2[:], self.z2[:], (self.r_z2,), (self.r_z2,), scale=1.5957691216057308)
                    p.op("dve", lambda g, mt=mt: g.tensor_tensor(out=self.gl[:, mt, :], in0=self.z2[:], in1=self.z[:],
                                                                 op=ALU.mult), (self.r_z2, self.r_z), (self.r_gl,))
                if si == 0:
                    po, r_po = bk.b[3], bk.r[3]
                    for mt in range(2):
                        MM(p, po[0:64, 0:NB], self.w2[:, mt, :], self.gl[:, mt, :], mt == 0, mt == 1,
                           (self.r_w2, self.r_gl), (r_po,))
                    ACT(p, AF.Copy, self.ko[:, gi, :], po[0:64, 0:NB], (r_po,), (self.r_ko,))
                    ACT(p, AF.Square, self.sq[:], po[0:64, 0:NB], (r_po,), (self.r_sq,))
                    pm, r_pm = bk.b[4], bk.r[4]
                    MM(p, pm[0:64, 0:NB], c.ones_bf[0:64, 0:64], self.sq[:], True, True, (c.r_ones, self.r_sq), (r_pm,))
                    p.op("dve", lambda g, gi=gi: g.reduce_max(out=self.kmx[:, gi:gi + 1], in_=pm[0:64, 0:NB], axis=AX.X),
                         (r_pm,), (self.r_kmx,))
                else:
                    po, r_po = bk.b[3], bk.r[3]
                    for mt in range(2):
                        MM(p, po[0:NB, 0:64], self.gl[:, mt, :], self.w2[:, mt, :], mt == 0, mt == 1,
                           (self.r_w2, self.r_gl), (r_po,))
                    ACT(p, AF.Copy, self.vo[:, gi, 0:64], po[0:NB, 0:64], (r_po,), (self.r_vo,))
        p.dma("sp", kc_out.rearrange("g d n -> d g n"), self.ko[:], self.r_ko, (self.r_ko,), (), is_output=True)
        p.dma("sp", vc_out, self.vo[:], self.r_vo, (self.r_vo,), (), is_output=True)
        p.dma("sp", kmx_out, self.kmx[:], self.r_kmx, (self.r_kmx,), (), is_output=True)


class ATTPhase:
    PC = 16

    def __init__(self, p, c, bk, NS, T):
        self.p, self.c, self.bk, self.NS, self.T = p, c, bk, NS, T
        self.NSB = T // 64
        self.NCc = max(1, (T // 16) // 128)
        NSB, NCc, PC = self.NSB, self.NCc, self.PC
        sb, res = p.sb, p.res
        self.win = sb("at_win", [128, KD, 1072], BF16)
        self.r_win = res("at_win")
        self.wn = sb("at_wn", [128, KD], F32)
        self.r_wn = res("at_wn")
        self.x4 = sb("at_x4", [128, KD, TT], F32)
        self.r_x4 = res("at_x4")
        self.h4 = sb("at_h4", [128, KD, TT], BF16)
        self.r_h4 = res("at_h4")
        self.Qa = [sb("at_Qa%d" % i, [128, 16, KROWS], BF16) for i in range(2)]
        self.r_Qa = [res("at_Qa%d" % i) for i in range(2)]
        self.QT = [sb("at_QT%d" % i, [KROWS, 4, 512], BF16) for i in range(2)]
        self.r_QT = [res("at_QT%d" % i) for i in range(2)]
        self.gates = [sb("at_gt%d" % i, [128, 48], F32) for i in range(2)]
        self.r_gates = [res("at_gt%d" % i) for i in range(2)]
        self.sqt = sb("at_sqt", [128, 512], F32)
        self.r_sqt = res("at_sqt")
        self.sm = sb("at_sm", [128, 8, 16], F32)
        self.r_sm = res("at_sm")
        self.KMs = sb("at_KMs", [128, 16], F32)
        self.kmall = sb("at_kmall", [128, 4, 24], F32)
        self.r_KM = res("at_KM")
        self.cst = {}
        self.r_cst = res("at_cst")
        for nm, shp, dt in (("ident", [128, 128], BF16), ("ident32", [128, 128], F32), ("iota1", [128, 128], F32), ("iota2", [128, 128], F32),
                            ("negL", [128, 128], F32), ("negU", [128, 128], F32), ("apool", [128, NCc, NSB], BF16),
                            ("thr1", [128, NS * NCc], F32), ("thr2", [128, 8], F32), ("negst", [128, NS, 16], F32),
                            ("bonus", [128, NS, NSB], F32)):
            self.cst[nm] = sb("at_c_" + nm, shp, dt)
        self.KTc = sb("at_KTc", [KROWS, 4, NCc * 128], BF16)
        self.Vc = sb("at_Vc", [128, 4, NCc, 65], BF16)
        self.r_kvc = res("at_kvc")
        self.KTp = [sb("at_KTp%d" % i, [KROWS, PC * 128], BF16) for i in range(3)]
        self.Vp = [sb("at_Vp%d" % i, [128, PC, 65], BF16) for i in range(3)]
        self.r_kvp = [res("at_kvp%d" % i) for i in range(3)]
        self.ikv = 0
        self.KTw = [sb("at_KTw%d" % i, [KROWS, 640], BF16) for i in range(2)]
        self.Vw = [sb("at_Vw%d" % i, [128, 5, 65], BF16) for i in range(2)]
        self.r_kvw = [res("at_kvw%d" % i) for i in range(2)]
        self.iw = 0
        self.e = [sb("at_e%d" % i, [128, 512], BF16) for i in range(3)]
        self.r_e = [res("at_e%d" % i) for i in range(3)]
        self.pp = [sb("at_p%d" % i, [128, 512], BF16) for i in range(3)]
        self.r_pp = [res("at_p%d" % i) for i in range(3)]
        self.ie = 0
        self.zt = [sb("at_zt%d" % i, [128, 512], F32) for i in range(2)]
        self.r_zt = [res("at_zt%d" % i) for i in range(2)]
        self.m2 = [sb("at_m2%d" % i, [128, 128], F32) for i in range(3)]
        self.r_m2 = [res("at_m2%d" % i) for i in range(3)]
        self.im2 = 0
        self.sc = sb("at_sc", [128, NSB], F32)
        self.sc2 = sb("at_sc2", [128, NSB], F32)
        self.m8 = sb("at_m8", [128, 16], F32)
        self.sel = sb("at_sel", [128, NSB], BF16)
        self.selx = [sb("at_selx%d" % i, [128, 1024], BF16) for i in range(2)]
        self.r_selx = [res("at_selx%d" % i) for i in range(2)]
        self.r_sc, self.r_sel = res("at_sc"), res("at_sel")
        self.rd = sb("at_rd", [128, 8], F32)
        self.r_rd = res("at_rd")
        self.oacc = sb("at_oacc", [128, 16, 64], F32)
        self.r_oacc = res("at_oacc")
        self.obf = [sb("at_obf%d" % i, [128, 1024], BF16) for i in range(2)]
        self.r_obf = [res("at_obf%d" % i) for i in range(2)]
        self.obT = [sb("at_obT%d" % i, [128, 8, 128], BF16) for i in range(2)]
        self.r_obT = [res("at_obT%d" % i) for i in range(2)]
        self.ps_s = (bk.b[0], bk.b[1])
        self.r_ps_s = (bk.r[0], bk.r[1])
        self.ps_o = (bk.b[3], bk.b[3])
        self.r_ps_o = (bk.r[3], bk.r[3])
        self.poT, self.r_poT = bk.b[2], bk.r[2]
        self.oTs = sb("at_oTs", [65, 512], F32)
        self.r_oTs = res("at_oTs")
        self.ps_r = (bk.b[4], bk.b[5])
        self.r_ps_r = (bk.r[4], bk.r[5])
        self.iss = 0
        self.ipo = 0
        self.r_tbh = (bk.r_tb, bk.r_tb)

    def setup(self, d):
        p = self.p
        load_cols(p, "sp", self.wn, self.r_wn, d["norm_w"], KD)
        re = lambda a: a.rearrange("(k q) f -> q k f", q=128)
        p.dma("pool", self.win[:, :, 0:512], re(d["w_in"][:, 0:512]), self.r_win, (), (self.r_win,))
        p.dma("pool", self.win[:, :, 512:1024], re(d["w_in"][:, 512:1024]), self.r_win, (), (self.r_win,))
        with p.nc.allow_non_contiguous_dma(reason="small gate cols"):
            p.dma("pool", self.win[:, :, 1024:1072], re(d["w_in"][:, 1024:1072]), self.r_win, (), (self.r_win,))
        for nm in self.cst:
            p.dma("sp", self.cst[nm][:], d["c_" + nm], self.r_cst, (), (self.r_cst,))
        for i in range(2):
            p.op("dve", lambda g, i=i: g.memset(self.Qa[i][:], 0.0), (), (self.r_Qa[i],))
            with p.nc.allow_non_contiguous_dma(reason="small const cols"):
                p.dma("sp", self.Qa[i][:, :, 67:71], d["c_qaug"], self.r_Qa[i], (), (self.r_Qa[i],))
        p.dma("sp", self.KTc[:], d["KTc"].rearrange("g r n -> r g n"), self.r_kvc, (), (self.r_kvc,))
        p.dma("sp", self.Vc[:], d["Vc"].rearrange("g q c e -> q g c e"), self.r_kvc, (), (self.r_kvc,))
        p.dma("sp", self.kmall[:], d["kmall"], self.r_KM, (), (self.r_KM,))
        R = (self.r_KM,)
        for g_ in range(4):
            p.op("dve", lambda g, g_=g_: g.reduce_max(out=self.KMs[:, g_ * 4:g_ * 4 + 1], in_=self.kmall[:, g_, :], axis=AX.X), R, R)
        ACT(p, AF.Sqrt, self.KMs[:, 0::4], self.KMs[:, 0::4], R, R)
        p.op("dve", lambda g: g.tensor_scalar(out=self.KMs[:, 0::4], in0=self.KMs[:, 0::4], scalar1=1.02, scalar2=None,
                                              op0=ALU.mult), R, R)
        for j in range(1, 4):
            p.op("dve", lambda g, j=j: g.tensor_copy(out=self.KMs[:, j::4], in_=self.KMs[:, 0::4]), R, R)

    def prep4(self, s4, d):
        p, c = self.p, self.c
        n = min(4, self.NS - s4) * 128
        p.dma("sp", self.x4[:, :, :n], d["xT"][:, s4 * 128:s4 * 128 + n].rearrange("(k q) n -> q k n", q=128),
              self.r_x4, (), (self.r_x4,))
        rmsnorm_fm(p, c, self.x4, self.r_x4, self.wn, self.r_wn, self.h4, self.r_h4, n)

    def prep_slot(self, s):
        p, bk = self.p, self.bk
        sub = s % 4
        tok = slice(sub * 128, (sub + 1) * 128)
        Qa, r_Qa = self.Qa[s % 2], self.r_Qa[s % 2]
        QT, r_QT = self.QT[s % 2], self.r_QT[s % 2]
        gates, r_gates = self.gates[s % 2], self.r_gates[s % 2]
        sm, R = self.sm, (self.r_sm,)
        for half in range(2):
            pq, r_pq = self.ps_r[half], self.r_ps_r[half]
            for k in range(KD):
                MM(p, pq[:], self.h4[:, k, tok], self.win[:, k, half * 512:(half + 1) * 512], k == 0, k == KD - 1,
                   (self.r_h4, self.r_win), (r_pq,))
            ACT(p, AF.Copy, Qa[:, half * 8:(half + 1) * 8, 0:64], pq[:].rearrange("q (h d) -> q h d", d=64),
                (r_pq,), (r_Qa,), scale=QK_SCALE)
            ACT(p, AF.Square, self.sqt[:], pq[:], (r_pq,), (self.r_sqt,), scale=QK_SCALE)
            p.op("dve", lambda g, half=half: g.tensor_reduce(out=sm[:, 0, half * 8:(half + 1) * 8],
                                                             in_=self.sqt[:].rearrange("q (h d) -> q h d", d=64),
                                                             axis=AX.X, op=ALU.add), (self.r_sqt,), R)
        pg, r_pg = self.ps_r[0], self.r_ps_r[0]
        for k in range(KD):
            MM(p, pg[:, 0:48], self.h4[:, k, tok], self.win[:, k, 1024:1072], k == 0, k == KD - 1,
               (self.r_h4, self.r_win), (r_pg,))
        ACT(p, AF.Sigmoid, gates[:], pg[:, 0:48], (r_pg,), (r_gates,))
        ACT(p, AF.Sqrt, sm[:, 1, :], sm[:, 0, :], R, R)
        p.op("dve", lambda g: g.tensor_tensor(out=sm[:, 1, :], in0=sm[:, 1, :], in1=self.KMs[:], op=ALU.mult),
             (self.r_sm, self.r_KM), R)
        p.op("dve", lambda g: g.tensor_tensor(out=sm[:, 2, :], in0=self.cst["negst"][:, s, :], in1=sm[:, 1, :],
                                              op=ALU.subtract), (self.r_sm, self.r_cst), R)
        p.op("dve", lambda g: g.tensor_copy(out=Qa[:, :, 64], in_=sm[:, 2, :]), R, (r_Qa,))
        p.op("dve", lambda g: g.tensor_tensor(out=sm[:, 3, :], in0=sm[:, 2, :], in1=Qa[:, :, 64], op=ALU.subtract),
             (self.r_sm, r_Qa), R)
        p.op("dve", lambda g: g.tensor_copy(out=Qa[:, :, 65], in_=sm[:, 3, :]), R, (r_Qa,))
        p.op("dve", lambda g: g.tensor_tensor(out=sm[:, 4, :], in0=sm[:, 3, :], in1=Qa[:, :, 65], op=ALU.subtract),
             (self.r_sm, r_Qa), R)
        p.op("dve", lambda g: g.tensor_copy(out=Qa[:, :, 66], in_=sm[:, 4, :]), R, (r_Qa,))
        for g_ in range(4):
            hf = g_ % 2
            tb = bk.tb[0:KROWS, hf * 512:(hf + 1) * 512]
            for j in range(4):
                p.op("pe", lambda g, j=j: g.transpose(tb[:, j * 128:(j + 1) * 128], Qa[:, g_ * 4 + j, :],
                                                      self.cst["ident"][:]), (r_Qa, self.r_cst), (self.r_tbh[hf],))
            ACT(p, AF.Copy, QT[:, g_, :], tb, (self.r_tbh[hf],), (r_QT,))

    def _score_exp(self, KT_ap, r_k, QT_ap, r_q, neg=None, r_neg=()):
        p = self.p
        i = self.iss % 2
        self.iss += 1
        ps, r_ps = self.ps_s[i], self.r_ps_s[i]
        MM(p, ps[:], KT_ap, QT_ap, True, True, (r_k, r_q), (r_ps,))
        ie = self.ie % 3
        self.ie += 1
        if neg is None:
            ACT(p, AF.Exp, self.e[ie][:], ps[:], (r_ps,), (self.r_e[ie],))
        else:
            zt, r_zt = self.zt[i], self.r_zt[i]
            p.op("dve", lambda g: g.tensor_tensor(out=zt[:].rearrange("k (j q) -> k j q", q=128),
                                                  in0=ps[:].rearrange("k (j q) -> k j q", q=128),
                                                  in1=neg.unsqueeze(1).to_broadcast([128, 4, 128]), op=ALU.add),
                 (r_ps,) + tuple(r_neg), (r_zt,))
            ACT(p, AF.Exp, self.e[ie][:], zt[:], (r_zt,), (self.r_e[ie],))
        return ie

    def _pv(self, po, r_po, pt, r_pt, V_ap, r_v, first, last):
        p = self.p
        p.op("pe", lambda g: g.matmul(self.poT[0:65, :], V_ap, pt[:], start=first, stop=last),
             (r_pt, r_v), (self.r_poT,))
        if last:
            ACT(p, AF.Copy, self.oTs[:], self.poT[0:65, :], (self.r_poT,), (self.r_oTs,))
            for j in range(4):
                p.op("pe", lambda g, j=j: g.transpose(po[:, j * 65:(j + 1) * 65], self.oTs[:, j * 128:(j + 1) * 128],
                                                      self.cst["ident32"][0:65, 0:65]),
                     (self.r_oTs, self.r_cst), (r_po,))

    def _mask_mul(self, ie, mask_ap, r_mask):
        p = self.p
        pp, r_pp = self.pp[ie], self.r_pp[ie]
        p.op("dve", lambda g: g.tensor_tensor(out=pp[:].rearrange("k (j q) -> k j q", q=128),
                                              in0=self.e[ie][:].rearrange("k (j q) -> k j q", q=128),
                                              in1=mask_ap.unsqueeze(1).to_broadcast([128, 4, 128]), op=ALU.mult),
             (self.r_e[ie],) + tuple(r_mask), (r_pp,))
        return pp, r_pp

    def _finish_branch(self, po, r_po, b, g_, gates, r_gates, first_branch):
        p = self.p
        R = (self.r_rd,)
        den = po[:, 64:260:65]
        p.op("dve", lambda g: g.tensor_scalar(out=self.rd[:, 0:4], in0=den, scalar1=1e-30, scalar2=None, op0=ALU.max),
             (r_po,), R)
        p.op("dve", lambda g: g.reciprocal(out=self.rd[:, 0:4], in_=self.rd[:, 0:4]), R, R)
        gv = gates[:, g_ * 12 + b:g_ * 12 + 12:3]
        p.op("dve", lambda g: g.tensor_tensor(out=self.rd[:, 4:8], in0=self.rd[:, 0:4], in1=gv, op=ALU.mult),
             (self.r_rd, r_gates), R)
        for j in range(4):
            h = g_ * 4 + j
            if first_branch:
                p.op("dve", lambda g, j=j, h=h: g.tensor_scalar(out=self.oacc[:, h, :], in0=po[:, j * 65:j * 65 + 64],
                                                                scalar1=self.rd[:, 4 + j:5 + j], scalar2=None, op0=ALU.mult),
                     (r_po, self.r_rd), (self.r_oacc,))
            else:
                p.op("dve", lambda g, j=j, h=h: g.scalar_tensor_tensor(
                    out=self.oacc[:, h, :], in0=po[:, j * 65:j * 65 + 64], scalar=self.rd[:, 4 + j:5 + j],
                    in1=self.oacc[:, h, :], op0=ALU.mult, op1=ALU.add), (r_po, self.r_rd, self.r_oacc), (self.r_oacc,))

    def _next_po(self):
        i = self.ipo % 2
        self.ipo += 1
        return self.ps_o[i], self.r_ps_o[i]

    def _run_chunks(self, chunks, qt, r_QT, po, r_po, after_pv=None):
        n = len(chunks)
        ies = [None] * n

        def S(i):
            ch = chunks[i]
            if ch.get("pre") is not None:
                ch["pre"]()
            neg = ch.get("neg")
            if neg is None:
                ies[i] = self._score_exp(ch["KT"], ch["r_k"], qt, r_QT)
            else:
                ies[i] = self._score_exp(ch["KT"], ch["r_k"], qt, r_QT, neg[0], neg[1])

        S(0)
        for i in range(n):
            if i + 1 < n:
                S(i + 1)
            ch = chunks[i]
            ie = ies[i]
            mul = ch.get("mul")
            if mul is not None:
                pt, r_pt = self._mask_mul(ie, mul[0], mul[1])
            else:
                pt, r_pt = self.e[ie], self.r_e[ie]
            self._pv(po, r_po, pt, r_pt, ch["V"], ch["r_v"], i == 0, i == n - 1)
            if after_pv is not None:
                after_pv(i, pt, r_pt)

    def _new_m2(self, iota, thr_col):
        p = self.p
        m2, r_m2 = self.m2[self.im2 % 3], self.r_m2[self.im2 % 3]
        self.im2 += 1
        p.op("dve", lambda g: g.tensor_scalar(out=m2[:], in0=iota, scalar1=thr_col, scalar2=-30000.0,
                                              op0=ALU.is_lt, op1=ALU.mult), (self.r_cst,), (r_m2,))
        return m2, r_m2

    def slot_group(self, s, g_, d):
        p, bk, c = self.p, self.bk, self.c
        NSB, NCc, PC = self.NSB, self.NCc, self.PC
        QT, r_QT = self.QT[s % 2], self.r_QT[s % 2]
        gates, r_gates = self.gates[s % 2], self.r_gates[s % 2]
        qt = QT[:, g_, :]
        cst, r_cst = self.cst, self.r_cst
        ncmp = min(NCc, (8 * s + 7) // 16 + 1)
        nfull = (8 * s - 17) // 16 + 1 if 8 * s >= 17 else 0
        po, r_po = self._next_po()
        chunks = []
        for jc in range(ncmp):
            ch = dict(KT=self.KTc[:, g_, jc * 128:(jc + 1) * 128], r_k=self.r_kvc, V=self.Vc[:, g_, jc, :], r_v=self.r_kvc)
            if jc >= nfull:
                m2, r_m2 = self._new_m2(cst["iota1"][:], cst["thr1"][:, s * NCc + jc:s * NCc + jc + 1])
                ch["neg"] = (m2[:], (r_m2,))
            chunks.append(ch)

        def imp_mm(jc, pt, r_pt):
            for j in range(4):
                pr, r_pr = self.ps_r[j // 2], self.r_ps_r[j // 2]
                p.op("pe", lambda g, j=j: g.matmul(pr[:, (j % 2) * 256:(j % 2) * 256 + NSB], pt[:, j * 128:(j + 1) * 128],
                                                   cst["apool"][:, jc, :], start=(jc == 0 and j % 2 == 0),
                                                   stop=(jc == ncmp - 1), skip_group_check=True),
                     (r_pt, r_cst), (r_pr,))

        self._run_chunks(chunks, qt, r_QT, po, r_po, after_pv=imp_mm)
        self._finish_branch(po, r_po, 0, g_, gates, r_gates, True)
        RS = (self.r_sc,)
        for j in range(4):
            pr, r_pr = self.ps_r[j // 2], self.r_ps_r[j // 2]
            src = pr[:, (j % 2) * 256:(j % 2) * 256 + NSB]
            if j == 0:
                p.op("dve", lambda g: g.tensor_scalar(out=self.sc[:], in0=src, scalar1=self.rd[:, 0:1], scalar2=None,
                                                      op0=ALU.mult), (r_pr, self.r_rd), RS)
            else:
                p.op("dve", lambda g, j=j: g.scalar_tensor_tensor(out=self.sc[:], in0=src, scalar=self.rd[:, j:j + 1],
                                                                  in1=self.sc[:], op0=ALU.mult, op1=ALU.add),
                     (r_pr, self.r_rd, self.r_sc), RS)
        p.op("dve", lambda g: g.tensor_tensor(out=self.sc[:], in0=self.sc[:], in1=cst["bonus"][:, s, :], op=ALU.add),
             (self.r_sc, r_cst), RS)
        p.op("dve", lambda g: g.max(out=self.m8[:, 0:8], in_=self.sc[:]), RS, RS)
        p.op("dve", lambda g: g.match_replace(out=self.sc2[:], in_to_replace=self.m8[:, 0:8], in_values=self.sc[:],
                                              imm_value=-3.0e38), RS, RS)
        p.op("dve", lambda g: g.max(out=self.m8[:, 8:16], in_=self.sc2[:]), RS, RS)
        p.op("dve", lambda g: g.tensor_scalar(out=self.m8[:, 15:16], in0=self.m8[:, 15:16], scalar1=-1.0e29, scalar2=None,
                                              op0=ALU.max), RS, RS)
        p.op("dve", lambda g: g.tensor_scalar(out=self.sel[:], in0=self.sc[:], scalar1=self.m8[:, 15:16], scalar2=None,
                                              op0=ALU.is_ge), RS, (self.r_sel,))
        nk = 8 * s + 8
        po, r_po = self._next_po()
        mbanks = ((bk.tb, bk.r_tb), (c.ps_n[:].bitcast(BF16), c.r_psn))
        chunks = []
        for kc in range(nk):
            cl = kc % PC
            pi = kc // PC
            ch = {}

            def pre(kc=kc, cl=cl, pi=pi, ch=ch):
                if cl == 0:
                    ncz = min(PC, nk - kc)
                    ib = self.ikv % 3
                    self.ikv += 1
                    self.cur_kv = (self.KTp[ib], self.Vp[ib], self.r_kvp[ib])
                    KTp, Vp, r_kv = self.cur_kv
                    p.dma("sp", KTp[:, 0:ncz * 128], d["KTs"][g_][:, kc * 128:(kc + ncz) * 128], r_kv, (), (r_kv,))
                    p.dma("sp", Vp[:, 0:ncz, :], d["Vs"][g_][:, kc:kc + ncz, :], r_kv, (), (r_kv,))
                KTp, Vp, r_kv = self.cur_kv
                ch["KT"], ch["r_k"], ch["V"], ch["r_v"] = KTp[:, cl * 128:(cl + 1) * 128], r_kv, Vp[:, cl, :], r_kv
                if kc % 8 == 0:
                    mb, r_mb = mbanks[(kc // 8) % 2]
                    nm_ = min(8, nk - kc)
                    sx, r_sx = self.selx[(kc // 8) % 2], self.r_selx[(kc // 8) % 2]
                    p.op("pool", lambda g: g.tensor_copy(
                        out=sx[:, 0:nm_ * 128].rearrange("q (b k) -> q b k", k=64),
                        in_=self.sel[:, 2 * kc:2 * kc + 2 * nm_].unsqueeze(2).to_broadcast([128, 2 * nm_, 64])),
                        (self.r_sel,), (r_sx,))
                    for m in range(nm_):
                        p.op("pe", lambda g, m=m: g.transpose(mb[:, m * 128:(m + 1) * 128], sx[:, m * 128:(m + 1) * 128],
                                                              cst["ident"][:]), (r_sx, r_cst), (r_mb,))
                mb, r_mb = mbanks[(kc // 8) % 2]
                ch["mul"] = (mb[:, (kc % 8) * 128:(kc % 8 + 1) * 128], (r_mb,))
                if kc >= 8 * s:
                    r = kc - 8 * s
                    m2, r_m2 = self._new_m2(cst["iota2"][:], cst["thr2"][:, r:r + 1])
                    ch["neg"] = (m2[:], (r_m2,))

            ch["pre"] = pre
            chunks.append(ch)
        self._run_chunks(chunks, qt, r_QT, po, r_po)
        self._finish_branch(po, r_po, 1, g_, gates, r_gates, False)
        iw = self.iw % 2
        self.iw += 1
        KTw, Vw, r_kw = self.KTw[iw], self.Vw[iw], self.r_kvw[iw]
        p.dma("sp", KTw[:], d["KTw"][g_][:, s * 640:(s + 1) * 640], r_kw, (), (r_kw,))
        p.dma("sp", Vw[:], d["Vw"][g_][:, s * 5:(s + 1) * 5, :], r_kw, (), (r_kw,))
        po, r_po = self._next_po()
        chunks = []
        for r in range(5):
            ch = dict(KT=KTw[:, r * 128:(r + 1) * 128], r_k=r_kw, V=Vw[:, r, :], r_v=r_kw)
            if r == 0:
                ch["neg"] = (cst["negU"][:], (r_cst,))
            elif r == 4:
                ch["neg"] = (cst["negL"][:], (r_cst,))
            chunks.append(ch)
        self._run_chunks(chunks, qt, r_QT, po, r_po)
        self._finish_branch(po, r_po, 2, g_, gates, r_gates, False)

    def run(self, d):
        p = self.p
        self.setup(d)
        for s in range(self.NS):
            if s % 4 == 0:
                self.prep4(s, d)
            self.prep_slot(s)
            for g_ in range(4):
                self.slot_group(s, g_, d)
            ob, r_ob = self.obf[s % 2], self.r_obf[s % 2]
            ACT(p, AF.Copy, ob[:], self.oacc[:].rearrange("q h d -> q (h d)"), (self.r_oacc,), (r_ob,))
            ot, r_ot = self.obT[s % 2], self.r_obT[s % 2]
            for hf in range(2):
                for kk in range(4):
                    k8 = hf * 4 + kk
                    p.op("pe", lambda g, k8=k8, kk=kk, hf=hf: g.transpose(
                        self.bk.tb[:, hf * 512 + kk * 128:hf * 512 + (kk + 1) * 128], ob[:, k8 * 128:(k8 + 1) * 128],
                        self.cst["ident"][:]), (r_ob, self.r_cst), (self.r_tbh[hf],))
                ACT(p, AF.Copy, ot[:, hf * 4:(hf + 1) * 4, :],
                    self.bk.tb[:, hf * 512:(hf + 1) * 512].rearrange("f (k q) -> f k q", q=128), (self.r_tbh[hf],), (r_ot,))
            with p.nc.allow_non_contiguous_dma(reason="256B runs"):
                p.dma("sp", d["oT"][:, s * 128:(s + 1) * 128].rearrange("(k f) n -> f k n", f=128), ot[:], r_ot,
                      (r_ot,), (), is_output=True)


class OPPhase:
    def __init__(self, p, c, ws, bk):
        self.p, self.c, self.ws, self.bk = p, c, ws, bk
        self.xs = [p.sb("opx%d" % i, [128, KD, TT], F32) for i in range(2)]
        self.r_xs = [p.res("opx%d" % i) for i in range(2)]
        self.on = [p.sb("opo%d" % i, [128, KD, TT], BF16) for i in range(2)]
        self.r_on = [p.res("opo%d" % i) for i in range(2)]

    def run(self, x_in, x_out, oT, w_out, ntiles):
        p, bk = self.p, self.bk
        for t in range(ntiles):
            for blk in range(2):
                self.ws.plan_cols(w_out, blk * 512)
        for t in range(ntiles):
            T0 = t * TT
            xs, r_xs = self.xs[t % 2], self.r_xs[t % 2]
            on, r_on = self.on[t % 2], self.r_on[t % 2]
            p.dma("sp", xs[:], x_in[:, T0:T0 + TT].rearrange("(k q) n -> q k n", q=128), r_xs, (), (r_xs,))
            p.dma("sp", on[:], oT[:, T0:T0 + TT].rearrange("(k q) n -> q k n", q=128), r_on, (), (r_on,))
            for blk in range(2):
                w, r_w = self.ws.next()
                for dt_ in range(4):
                    po, r_po = bk.b[dt_ % 2], bk.r[dt_ % 2]
                    for k in range(KD):
                        MM(p, po[:], w[:, k, dt_ * 128:(dt_ + 1) * 128], on[:, k, :], k == 0, k == KD - 1,
                           (r_w, r_on), (r_po,))
                    kk = blk * 4 + dt_
                    p.op("dve", lambda g: g.tensor_tensor(out=xs[:, kk, :], in0=xs[:, kk, :], in1=po[:], op=ALU.add),
                         (r_xs, r_po), (r_xs,))
            p.dma("sp", x_out[:, T0:T0 + TT].rearrange("(k q) n -> q k n", q=128), xs[:], r_xs, (r_xs,), (),
                  is_output=True)


from contextlib import ExitStack
import ml_dtypes

NPBF = ml_dtypes.bfloat16
NCORES = 8


class _IO:
    def __init__(self, nc):
        self.nc = nc

    def i(self, n, s, d=F32):
        return self.nc.dram_tensor(n, list(s), d, kind="ExternalInput").ap()

    def o(self, n, s, d=F32):
        return self.nc.dram_tensor(n, list(s), d, kind="ExternalOutput").ap()

    def t(self, n, s, d=F32):
        return self.nc.dram_tensor(n, list(s), d, kind="Internal").ap()


def _hg1_io(io, NT, sfx, out=True):
    mk = io.o if out else io.i
    return dict(o_loc=mk("o_loc" + sfx, [D, NT]), qdec=mk("qdec" + sfx, [D, NT], BF16), sg=mk("sg" + sfx, [D, NT], BF16),
                s_fin=mk("s_fin" + sfx, [128, 8, 128]) if out else None, dec=mk("dec" + sfx, [128, 8]) if out else None)


def _phase_hg1(p, c, io, x_ap, layer, NT, outs, cd_in):
    p.begin_phase()
    ws = WStream(p)
    bk = Banks(p)
    cd = load_consts_hg(p, cd_in["reset"], cd_in["tri"], cd_in["ident"])
    g1 = HG1(p, c, ws, bk, cd)
    g1.setup(cd_in["a_norm_w"], cd_in["alb"], layer)
    g1.plan_weights(cd_in["a_w_in"], NT // TT)
    for t in range(NT // TT):
        g1.tile(t, x_ap, outs["o_loc"], outs["qdec"], outs["sg"])
    g1.finish(outs["s_fin"], outs["dec"])
    p.end_phase()


def _phase_hg2(p, c, x_in, x_out, ins, NT):
    p.begin_phase()
    ws = WStream(p)
    bk = Banks(p)
    g2 = HG2(p, c, ws, bk)
    g2.prefix(ins["S_all"], ins["dec_all"], ins["selv"], ins["gnorm_w"])
    g2.plan_weights(ins["a_w_out"], NT // TT)
    for t in range(NT // TT):
        g2.tile(t, x_in, x_out, ins["o_loc"], ins["qdec"], ins["sg"])
    p.end_phase()


def _phase_mlp(p, c, x_in, x_out, wn, wu, wd, NT, final_w=None):
    p.begin_phase()
    m = MLP(p, c)
    m.run(x_in, x_out, NT // TT, wn, wu, wd, final_w=final_w)
    p.end_phase()


def build_L1(NT):
    nc = bass.Bass("TRN2", target_bir_lowering=False)
    io = _IO(nc)
    x = io.i("xT", [D, NT])
    cd_in = dict(reset=io.i("c_reset", [128, TT]), tri=io.i("c_tri", [64, 64], BF16), ident=io.i("c_ident", [128, 128], BF16),
                 a_norm_w=io.i("a_norm_w", [D]), alb=io.i("alb", [2, D]), a_w_in=io.i("a_w_in", [D, 4096]))
    outs = _hg1_io(io, NT, "")
    with ExitStack() as st:
        p = Prog(nc, st)
        c = Common(p)
        _phase_hg1(p, c, io, x, 0, NT, outs, cd_in)
        p.finish()
        nc._ninstr = p.ninstr
    return nc


def build_L2(NT):
    nc = bass.Bass("TRN2", target_bir_lowering=False)
    io = _IO(nc)
    x = io.i("xT", [D, NT])
    ins = dict(S_all=io.i("S_all", [8, 128, 8, 128]), dec_all=io.i("dec_all", [8, 128, 8]), selv=io.i("selv", [128, 8]),
               gnorm_w=io.i("gnorm_w", [128]), a_w_out=io.i("a_w_out", [D, D]),
               o_loc=io.i("o_loc_in", [D, NT]), qdec=io.i("qdec_in", [D, NT], BF16), sg=io.i("sg_in", [D, NT], BF16))
    wn, wu, wd = io.i("mlp_norm_w", [D]), io.i("mlp_w_up", [D, DFF]), io.i("mlp_w_down", [DFF, D])
    cd_in = dict(reset=io.i("c_reset", [128, TT]), tri=io.i("c_tri", [64, 64], BF16), ident=io.i("c_ident", [128, 128], BF16),
                 a_norm_w=io.i("a_norm_w", [D]), alb=io.i("alb", [2, D]), a_w_in=io.i("a_w_in", [D, 4096]))
    x1 = io.t("x1", [D, NT])
    x2 = io.o("x2", [D, NT])
    outs = _hg1_io(io, NT, "")
    with ExitStack() as st:
        p = Prog(nc, st)
        c = Common(p)
        _phase_hg2(p, c, x, x1, ins, NT)
        _phase_mlp(p, c, x1, x2, wn, wu, wd, NT)
        _phase_hg1(p, c, io, x2, 1, NT, outs, cd_in)
        p.finish()
        nc._ninstr = p.ninstr
    return nc


def build_L3(NT):
    nc = bass.Bass("TRN2", target_bir_lowering=False)
    io = _IO(nc)
    x = io.i("xT", [D, NT])
    ins = dict(S_all=io.i("S_all", [8, 128, 8, 128]), dec_all=io.i("dec_all", [8, 128, 8]), selv=io.i("selv", [128, 8]),
               gnorm_w=io.i("gnorm_w", [128]), a_w_out=io.i("a_w_out", [D, D]),
               o_loc=io.i("o_loc_in", [D, NT]), qdec=io.i("qdec_in", [D, NT], BF16), sg=io.i("sg_in", [D, NT], BF16))
    wn, wu, wd = io.i("mlp_norm_w", [D]), io.i("mlp_w_up", [D, DFF]), io.i("mlp_w_down", [DFF, D])
    kvn, kvw = io.i("kv_norm_w", [D]), io.i("kv_w", [D, 1536])
    x1 = io.t("x1", [D, NT])
    x2 = io.o("x2", [D, NT])
    kT01, kT24 = io.o("kT01", [512, NT], BF16), io.o("kT24", [512, NT], BF16)
    vaug = io.o("vaug", [NT // 128, 128, 8, 65], BF16)
    kmx = io.o("kmx", [128, 4])
    with ExitStack() as st:
        p = Prog(nc, st)
        c = Common(p)
        _phase_hg2(p, c, x, x1, ins, NT)
        _phase_mlp(p, c, x1, x2, wn, wu, wd, NT)
        p.begin_phase()
        ws = WStream(p)
        bk = Banks(p)
        kv = KVPhase(p, c, ws, bk)
        kv.setup(kvn)
        kv.plan_weights(kvw, NT // TT)
        for t in range(NT // TT):
            kv.tile(t, x2, kT01, kT24, vaug)
        kv.finish(kmx)
        p.end_phase()
        p.finish()
        nc._ninstr = p.ninstr
    return nc


def build_L4(NB):
    nc = bass.Bass("TRN2", target_bir_lowering=False)
    io = _IO(nc)
    kin, vin = io.i("kin", [4, 64, 16 * NB + 16], BF16), io.i("vin", [4, 64, 16 * NB + 16], BF16)
    pk, w1k, w2k = io.i("cmp_pe_k", [32, 64]), io.i("cmp_w1_k", [2048, 256]), io.i("cmp_w2_k", [256, 64])
    pv, w1v, w2v = io.i("cmp_pe_v", [32, 64]), io.i("cmp_w1_v", [2048, 256]), io.i("cmp_w2_v", [256, 64])
    kc, vc, kmxc = io.o("kc", [4, 64, NB], BF16), io.o("vc", [NB, 4, 65], BF16), io.o("kmxc", [64, 4])
    with ExitStack() as st:
        p = Prog(nc, st)
        c = Common(p)
        p.begin_phase()
        bk = Banks(p)
        cm = CMPPhase(p, c, bk, NB)
        cm.run(kin, vin, pk, w1k, w2k, pv, w1v, w2v, kc, vc, kmxc)
        p.end_phase()
        p.finish()
        nc._ninstr = p.ninstr
    return nc


def build_L5(NT, T, nlayers=2):
    nc = bass.Bass("TRN2", target_bir_lowering=False)
    io = _IO(nc)
    NS = NT // 128
    NSB = T // 64
    NCc = max(1, (T // 16) // 128)
    x = io.i("xT", [D, NT])
    shared = dict(KTs=io.i("KTs", [4, KROWS, T], BF16), Vs=io.i("Vs", [4, 128, T // 128, 65], BF16),
                  KTw=io.i("KTw", [4, KROWS, NS * 640], BF16), Vw=io.i("Vw", [4, 128, NS * 5, 65], BF16),
                  KTc=io.i("KTc", [4, KROWS, NCc * 128], BF16), Vc=io.i("Vc", [4, 128, NCc, 65], BF16),
                  kmall=io.i("kmall", [128, 4, 24]),
                  c_ident=io.i("c_ident", [128, 128], BF16), c_ident32=io.i("c_ident32", [128, 128]), c_iota1=io.i("c_iota1", [128, 128]),
                  c_iota2=io.i("c_iota2", [128, 128]), c_negL=io.i("c_negL", [128, 128]), c_negU=io.i("c_negU", [128, 128]),
                  c_apool=io.i("c_apool", [128, NCc, NSB], BF16), c_thr1=io.i("c_thr1", [128, NS * NCc]),
                  c_thr2=io.i("c_thr2", [128, 8]), c_negst=io.i("c_negst", [128, NS, 16]),
                  c_bonus=io.i("c_bonus", [128, NS, NSB]), c_qaug=io.i("c_qaug", [128, 16, 4], BF16))
    lw = []
    for b in range(nlayers):
        lw.append(dict(norm_w=io.i("b_norm_w%d" % b, [D]), w_in=io.i("b_w_in%d" % b, [D, 1072]),
                       w_out=io.i("b_w_out%d" % b, [D, D]), wn=io.i("mlp_norm_w%d" % b, [D]),
                       wu=io.i("mlp_w_up%d" % b, [D, DFF]), wd=io.i("mlp_w_down%d" % b, [DFF, D])))
    fw = io.i("final_norm_w", [D])
    out = io.o("outT", [D, NT])
    with ExitStack() as st:
        p = Prog(nc, st)
        c = Common(p)
        xcur = x
        for b in range(nlayers):
            oT = io.t("oT%d" % b, [D, NT], BF16)
            xa = io.t("xa%d" % b, [D, NT])
            xb = out if b == nlayers - 1 else io.t("xb%d" % b, [D, NT])
            p.begin_phase()
            bk = Banks(p)
            at = ATTPhase(p, c, bk, NS, T)
            d = dict(shared)
            d.update(xT=xcur, norm_w=lw[b]["norm_w"], w_in=lw[b]["w_in"], oT=oT)
            at.run(d)
            p.end_phase()
            p.begin_phase()
            ws = WStream(p)
            bk = Banks(p)
            op = OPPhase(p, c, ws, bk)
            op.run(xcur, xa, oT, lw[b]["w_out"], NT // TT)
            p.end_phase()
            _phase_mlp(p, c, xa, xb, lw[b]["wn"], lw[b]["wu"], lw[b]["wd"], NT,
                       final_w=fw if b == nlayers - 1 else None)
            xcur = xb
        p.finish()
        nc._ninstr = p.ninstr
    return nc


def _bf16_split(v):
    hi = np.asarray(v, np.float64).astype(NPBF)
    lo = (np.asarray(v, np.float64) - hi.astype(np.float64)).astype(NPBF)
    return hi, lo


def _aug_rows(u):
    u = np.asarray(u, np.int64)
    a = (u // 128).astype(np.float32)
    b = (u % 128).astype(np.float32)
    one = np.ones_like(a)
    return np.stack([one, one, one, a, a, b, b], 0).astype(NPBF)


def att_consts(core, NS, T):
    NSB = T // 64
    NCc = max(1, (T // 16) // 128)
    pp = np.arange(128)[:, None]
    qq = np.arange(128)[None, :]
    cst = {}
    cst["c_ident"] = np.eye(128).astype(NPBF)
    cst["c_ident32"] = np.eye(128).astype(np.float32)
    cst["c_iota1"] = (qq - 16 * pp).astype(np.float32)
    cst["c_iota2"] = (qq - pp).astype(np.float32) * np.ones((128, 128), np.float32)
    cst["c_negL"] = np.where(pp > qq, -30000.0, 0.0).astype(np.float32)
    cst["c_negU"] = np.where(qq >= pp, -30000.0, 0.0).astype(np.float32)
    cg = (np.arange(NCc)[None, :, None] * 128 + np.arange(128)[:, None, None])
    nn = np.arange(NSB)[None, None, :]
    cst["c_apool"] = ((cg >= 4 * nn - 1) & (cg <= 4 * nn + 3)).astype(NPBF)
    qb = 8 * np.arange(NS) + core
    thr1 = (2048 * np.arange(NCc)[None, :] + 31 - 128 * qb[:, None]).reshape(-1).astype(np.float32)
    cst["c_thr1"] = np.broadcast_to(thr1[None, :], (128, NS * NCc)).copy()
    thr2 = (128 * (np.arange(8) - core)).astype(np.float32)
    cst["c_thr2"] = np.broadcast_to(thr2[None, :], (128, 8)).copy()
    bhi, blo = _bf16_split(NSA_SLOPES)
    seff = bhi.astype(np.float64) + blo.astype(np.float64)
    tq = (128 * qb[None, :] + np.arange(128)[:, None]).astype(np.float64)
    cst["c_negst"] = (-tq[:, :, None] * seff[None, None, :]).astype(np.float32)
    cur = (tq // 64).astype(np.int64)[:, :, None]
    nb = np.arange(NSB)[None, None, :]
    forced = (nb == 0) | (nb == cur) | (nb == cur - 1)
    cst["c_bonus"] = np.where(nb <= cur, 1.0e4 * forced, -1.0e30).astype(np.float32)
    qa = np.stack([(128.0 * bhi.astype(np.float64)).astype(NPBF), (128.0 * blo.astype(np.float64)).astype(NPBF), bhi, blo], -1)
    cst["c_qaug"] = np.broadcast_to(qa[None], (128, 16, 4)).copy()
    return cst


_PROGS = {}


def _prog(key, fn):
    if key not in _PROGS:
        _PROGS[key] = fn()
    return _PROGS[key]


def _run(nc, in_maps):
    return run_bass_kernel_spmd(nc, in_maps, core_ids=list(range(NCORES))).results


def kernel(x, a_norm_w, a_w_in, a_gnorm_w, a_w_out, a_lower_bounds, kv_norm_w, kv_w,
           cmp_pe_k, cmp_w1_k, cmp_w2_k, cmp_pe_v, cmp_w1_v, cmp_w2_v,
           b_norm_w, b_w_in, b_w_out, mlp_norm_w, mlp_w_up, mlp_w_down, final_norm_w, _debug=None):
    f32 = lambda a: np.ascontiguousarray(np.asarray(a, dtype=np.float32))
    x = f32(x)
    T = x.shape[1]
    NT = T // NCORES
    NS = NT // 128
    xs = x[0]
    hc = host_consts_hg()
    a_norm_w, a_w_in, a_gnorm_w, a_w_out, alb = map(f32, (a_norm_w, a_w_in, a_gnorm_w, a_w_out, a_lower_bounds))
    mlp_norm_w, mlp_w_up, mlp_w_down = map(f32, (mlp_norm_w, mlp_w_up, mlp_w_down))
    b_norm_w, b_w_in, b_w_out = map(f32, (b_norm_w, b_w_in, b_w_out))
    xT = [np.ascontiguousarray(xs[c * NT:(c + 1) * NT].T) for c in range(NCORES)]

    def selv(c):
        s = np.zeros((128, 8), np.float32)
        s[:, :c] = 1.0
        return s

    r1 = _run(_prog(("L1", NT), lambda: build_L1(NT)),
              [dict(xT=xT[c], a_norm_w=a_norm_w[0], alb=alb, a_w_in=a_w_in[0], **hc) for c in range(NCORES)])
    S_all = np.stack([r["s_fin"] for r in r1])
    dec_all = np.stack([r["dec"] for r in r1])
    r2 = _run(_prog(("L2", NT), lambda: build_L2(NT)),
              [dict(xT=xT[c], S_all=S_all, dec_all=dec_all, selv=selv(c), gnorm_w=a_gnorm_w[0], a_w_out=a_w_out[0],
                    o_loc_in=r1[c]["o_loc"], qdec_in=r1[c]["qdec"], sg_in=r1[c]["sg"],
                    mlp_norm_w=mlp_norm_w[0], mlp_w_up=mlp_w_up[0], mlp_w_down=mlp_w_down[0],
                    a_norm_w=a_norm_w[1], alb=alb, a_w_in=a_w_in[1], **hc) for c in range(NCORES)])
    S_all = np.stack([r["s_fin"] for r in r2])
    dec_all = np.stack([r["dec"] for r in r2])
    r3 = _run(_prog(("L3", NT), lambda: build_L3(NT)),
              [dict(xT=r2[c]["x2"], S_all=S_all, dec_all=dec_all, selv=selv(c), gnorm_w=a_gnorm_w[1], a_w_out=a_w_out[1],
                    o_loc_in=r2[c]["o_loc"], qdec_in=r2[c]["qdec"], sg_in=r2[c]["sg"],
                    mlp_norm_w=mlp_norm_w[1], mlp_w_up=mlp_w_up[1], mlp_w_down=mlp_w_down[1],
                    kv_norm_w=f32(kv_norm_w), kv_w=f32(kv_w)) for c in range(NCORES)])
    if _debug is not None:
        _debug["x_l1"] = np.concatenate([r["x2"].T for r in r3], 0)
    kT01 = np.concatenate([r["kT01"] for r in r3], 1)
    kT24 = np.concatenate([r["kT24"] for r in r3], 1)
    vaug = np.concatenate([r["vaug"] for r in r3], 0)
    n_cmp = T // 16
    NB = n_cmp // NCORES
    pad = np.zeros((512, 16), NPBF)
    kpad = np.concatenate([kT01, pad], 1)
    in4 = []
    for c in range(NCORES):
        sl = kpad[:, 16 * NB * c:16 * NB * (c + 1) + 16]
        in4.append(dict(kin=np.ascontiguousarray(sl[0:256].reshape(4, 64, -1)),
                        vin=np.ascontiguousarray(sl[256:512].reshape(4, 64, -1)),
                        cmp_pe_k=f32(cmp_pe_k), cmp_w1_k=f32(cmp_w1_k), cmp_w2_k=f32(cmp_w2_k),
                        cmp_pe_v=f32(cmp_pe_v), cmp_w1_v=f32(cmp_w1_v), cmp_w2_v=f32(cmp_w2_v)))
    r4 = _run(_prog(("L4", NB), lambda: build_L4(NB)), in4)
    NCc = max(1, n_cmp // 128)
    aug_tok = _aug_rows(np.arange(T))
    zpad = np.zeros((KROWS - 64 - NAUG, T), NPBF)
    KTs = np.stack([np.concatenate([kT24[g * 64:(g + 1) * 64], aug_tok, zpad], 0) for g in range(4)])
    KTwin_full = np.stack([np.concatenate([kT24[256 + g * 64:256 + (g + 1) * 64], aug_tok, zpad], 0) for g in range(4)])
    Vs = np.ascontiguousarray(np.transpose(vaug[:, :, 0:4, :], (2, 1, 0, 3)))
    Vwin_full = np.transpose(vaug[:, :, 4:8, :], (2, 1, 0, 3))
    kc_all = np.concatenate([r["kc"] for r in r4], 2)
    ncp = NCc * 128
    KTc = np.zeros((4, KROWS, ncp), NPBF)
    KTc[:, 0:64, :n_cmp] = kc_all
    KTc[:, 64:64 + NAUG, :n_cmp] = _aug_rows(16 * np.arange(n_cmp) + 31)[None]
    vc_all = np.concatenate([r["vc"] for r in r4], 0)
    vcp = np.zeros((ncp, 4, 65), NPBF)
    vcp[:n_cmp] = vc_all
    Vc = np.ascontiguousarray(np.transpose(vcp.reshape(NCc, 128, 4, 65), (2, 1, 0, 3)))
    km = np.zeros((4, 24), np.float32)
    for g in range(4):
        row = (g % 2) * 64
        vals = [r3[c]["kmx"][row, g // 2] for c in range(NCORES)] + [r3[c]["kmx"][row, 2 + g // 2] for c in range(NCORES)] \
            + [r4[c]["kmxc"][0, g] for c in range(NCORES)]
        km[g] = np.asarray(vals, np.float32)
    kmall = np.broadcast_to(km[None], (128, 4, 24)).copy()
    x_l1 = np.concatenate([r["x2"] for r in r3], 1)
    in5 = []
    for c in range(NCORES):
        qbs = [8 * s + c for s in range(NS)]
        xc = np.concatenate([x_l1[:, 128 * qb:128 * qb + 128] for qb in qbs], 1)
        KTw = np.zeros((4, KROWS, NS * 640), NPBF)
        Vw = np.zeros((4, 128, NS * 5, 65), NPBF)
        for s, qb in enumerate(qbs):
            for r in range(5):
                ch = qb - 4 + r
                if ch >= 0:
                    KTw[:, :, s * 640 + r * 128:s * 640 + (r + 1) * 128] = KTwin_full[:, :, ch * 128:(ch + 1) * 128]
                    Vw[:, :, s * 5 + r, :] = Vwin_full[:, :, ch, :]
        dd = dict(xT=np.ascontiguousarray(xc), KTs=KTs, Vs=Vs, KTw=KTw, Vw=Vw, KTc=KTc, Vc=Vc, kmall=kmall,
                  final_norm_w=f32(final_norm_w))
        dd.update(att_consts(c, NS, T))
        for b in range(2):
            dd.update({"b_norm_w%d" % b: b_norm_w[b], "b_w_in%d" % b: b_w_in[b], "b_w_out%d" % b: b_w_out[b],
                       "mlp_norm_w%d" % b: mlp_norm_w[2 + b], "mlp_w_up%d" % b: mlp_w_up[2 + b],
                       "mlp_w_down%d" % b: mlp_w_down[2 + b]})
        in5.append(dd)
    r5 = _run(_prog(("L5", NT, T), lambda: build_L5(NT, T)), in5)
    out = np.zeros((T, D), np.float32)
    for c in range(NCORES):
        oc = r5[c]["outT"]
        for s in range(NS):
            qb = 8 * s + c
            out[128 * qb:128 * qb + 128] = oc[:, s * 128:(s + 1) * 128].T
    return out[None]
```

```python
import numpy as np
import concourse.bass as bass
import concourse.mybir as mybir
from concourse.bass_utils import run_bass_kernel_spmd

F32 = mybir.dt.float32
BF16 = mybir.dt.bfloat16
AF = mybir.ActivationFunctionType
ALU = mybir.AluOpType
AX = mybir.AxisListType


class Res:
    __slots__ = ("name", "lw", "rd", "dsem", "dcnt", "dq")

    def __init__(self, name):
        self.name = name
        self.lw = None
        self.rd = []
        self.dsem = None
        self.dq = None
        self.dcnt = 0


class Prog:
    def __init__(self, nc, stack):
        self.nc = nc
        self.stack = stack
        self.gstack = stack
        self.free_sems = []
        self.phase_res = []
        self.eng = {"pe": nc.tensor, "act": nc.scalar, "dve": nc.vector,
                    "pool": nc.gpsimd, "sp": nc.sync}
        self.sem = {}
        self.cnt = {}
        for k in ("pe", "act", "dve", "pool"):
            self.sem[k] = stack.enter_context(nc.semaphore("s_" + k))
            self.cnt[k] = 0
        self.waited = {k: {} for k in self.eng}
        self.nres = 0
        self.out_tokens = []
        self.ninstr = 0

    def sb(self, name, shape, dt):
        self.nalloc = getattr(self, "nalloc", 0) + 1
        t = self.stack.enter_context(self.nc.sbuf_tensor("s%d_%s" % (self.nalloc, name), list(shape), dt))
        return t

    def ps(self, name, shape, dt=F32):
        self.nalloc = getattr(self, "nalloc", 0) + 1
        t = self.stack.enter_context(self.nc.psum_tensor("p%d_%s" % (self.nalloc, name), list(shape), dt))
        return t

    def res(self, name=None):
        self.nres += 1
        r = Res(name or ("r%d" % self.nres))
        self.phase_res.append(r)
        return r

    def barrier(self):
        toks = [(k, self.cnt[k]) for k in ("pe", "act", "dve", "pool") if self.cnt[k] > 0]
        for r in self.phase_res:
            if r.dsem is not None:
                toks.append((r.dsem, r.dcnt))
        for e in ("sp", "pe", "act", "dve", "pool"):
            self._emit_waits_all(e, toks)

    def _emit_waits_all(self, e, toks):
        wd = self.waited[e]
        for (k, v) in toks:
            if wd.get(k, 0) >= v:
                continue
            self.eng[e].wait_ge(self.sem[k], v)
            wd[k] = v
            self.ninstr += 1

    def begin_phase(self):
        from contextlib import ExitStack as _ES
        self.stack = _ES()
        self.phase_res = []
        return self.stack

    def end_phase(self):
        self.barrier()
        for r in self.phase_res:
            if r.dsem is not None:
                self.free_sems.append((r.dsem, r.dcnt, r.dq))
                r.dsem = None
        self.phase_res = []
        self.stack.close()
        self.stack = self.gstack

    def _deps(self, reads, writes):
        toks = []
        for r in reads:
            if r.lw is not None:
                toks.append(r.lw)
        for w in writes:
            if w.lw is not None:
                toks.append(w.lw)
            toks.extend(w.rd)
        return toks

    def _emit_waits(self, e, toks):
        wd = self.waited[e]
        need = {}
        for (k, v) in toks:
            if e == "pe" and k == "pe":
                continue
            if k == e and v <= self.cnt[e] - 2:
                continue
            if wd.get(k, 0) >= v:
                continue
            if need.get(k, 0) < v:
                need[k] = v
        for k, v in need.items():
            self.eng[e].wait_ge(self.sem[k], v)
            wd[k] = v
            self.ninstr += 1

    def _commit(self, tok, reads, writes):
        for r in reads:
            r.rd.append(tok)
        for w in writes:
            w.lw = tok
            w.rd = []

    def op(self, e, fn, reads=(), writes=()):
        self._emit_waits(e, self._deps(reads, writes))
        ins = fn(self.eng[e])
        self.cnt[e] += 1
        ins.then_inc(self.sem[e], 1)
        self.ninstr += 1
        tok = (e, self.cnt[e])
        self._commit(tok, reads, writes)
        return tok

    def dma(self, q, out, in_, sres, reads=(), writes=(), is_output=False, **kw):
        self._emit_waits(q, self._deps(reads, writes))
        qt = "sw" if q == "pool" else "hw"
        if sres.dsem is not None:
            assert sres.dq == qt, "mixed SW/HW DMA on one semaphore: " + sres.name
        if sres.dsem is None:
            sres.dq = qt
            fl = [i for i, f in enumerate(self.free_sems) if f[2] == qt]
            if fl:
                key, cnt0, _ = self.free_sems.pop(fl[0])
                sres.dsem = key
                sres.dcnt = cnt0
            else:
                key = "d%d" % self.nres
                self.nres += 1
                self.sem[key] = self.gstack.enter_context(self.nc.semaphore(key))
                sres.dsem = key
        ins = self.eng[q].dma_start(out=out, in_=in_, **kw)
        sres.dcnt += 16
        ins.then_inc(self.sem[sres.dsem], 16)
        self.ninstr += 1
        tok = (sres.dsem, sres.dcnt)
        self._commit(tok, reads, writes)
        if is_output:
            self.out_tokens.append(tok)
        return tok

    def finish(self):
        self._emit_waits("sp", self.out_tokens)


D = 1024
KD = D // 128
DFF = 4096
EPS = 1e-6
TT = 512


def ACT(p, func, out, in_, reads, writes, **kw):
    return p.op("act", lambda g: g.activation(out=out, in_=in_, func=func, **kw), reads, writes)


def MM(p, out, lhsT, rhs, start, stop, reads, writes):
    return p.op("pe", lambda g: g.matmul(out, lhsT, rhs, start=start, stop=stop), reads, writes)


class Common:
    def __init__(self, p):
        self.p = p
        self.ones_bf = p.sb("ones_bf", [128, 128], BF16)
        self.r_ones = p.res("ones")
        p.op("dve", lambda g: g.memset(self.ones_bf[:], 1.0), (), (self.r_ones,))
        self.eps = p.sb("eps_col", [128, 1], F32)
        self.r_eps = p.res("eps")
        p.op("dve", lambda g: g.memset(self.eps[:], EPS), (), (self.r_eps,))
        self.sq = p.sb("sq", [128, KD, TT], BF16)
        self.r_sq = p.res("sq")
        self.rstd = p.sb("rstd", [128, TT], F32)
        self.r_rstd = p.res("rstd")
        self.ps_n = p.ps("ps_n", [128, TT])
        self.r_psn = p.res("psn")


def load_cols(p, q, dst, r_dst, src_vec, n):
    with p.nc.allow_non_contiguous_dma(reason="tiny param vectors"):
        p.dma(q, dst[:, :n], src_vec.rearrange("(k p) -> p k", p=128), r_dst, (), (r_dst,))


def rmsnorm_fm(p, c, x_ap, r_x, w_col, r_w, h_ap, r_h, n):
    for k in range(KD):
        ACT(p, AF.Square, c.sq[:, k, :n], x_ap[:, k, :n], (r_x,), (c.r_sq,))
    for k in range(KD):
        MM(p, c.ps_n[:, :n], c.ones_bf[:], c.sq[:, k, :n], k == 0, k == KD - 1,
           (c.r_sq, c.r_ones), (c.r_psn,))
    ACT(p, AF.Sqrt, c.rstd[:, :n], c.ps_n[:, :n], (c.r_psn, c.r_eps), (c.r_rstd,),
        scale=1.0 / D, bias=c.eps[:, 0:1])
    p.op("dve", lambda g: g.reciprocal(out=c.rstd[:, :n], in_=c.rstd[:, :n]), (c.r_rstd,), (c.r_rstd,))
    for k in range(KD):
        p.op("dve", lambda g, k=k: g.scalar_tensor_tensor(
            out=h_ap[:, k, :n], in0=x_ap[:, k, :n], scalar=w_col[:, k:k + 1],
            in1=c.rstd[:, :n], op0=ALU.mult, op1=ALU.mult), (r_x, r_w, c.r_rstd), (r_h,))


class MLP:
    def __init__(self, p, c):
        self.p, self.c = p, c
        self.NS = 3
        self.wu = [p.sb("wu%d" % i, [128, KD, 512], BF16) for i in range(self.NS)]
        self.r_wu = [p.res("wu%d" % i) for i in range(self.NS)]
        self.wd = [p.sb("wd%d" % i, [128, 8, 512], BF16) for i in range(self.NS)]
        self.r_wd = [p.res("wd%d" % i) for i in range(self.NS)]
        self.h = [p.sb("mh%d" % i, [128, KD, TT], BF16) for i in range(2)]
        self.r_h = [p.res("mh%d" % i) for i in range(2)]
        self.hid = [p.sb("hid%d" % i, [128, 32, TT], BF16) for i in range(2)]
        self.r_hid = [p.res("hid%d" % i) for i in range(2)]
        self.rl = [p.sb("rl%d" % i, [128, TT], BF16) for i in range(2)]
        self.r_rl = [p.res("rl%d" % i) for i in range(2)]
        self.ps_u = [p.ps("ps_u%d" % i, [128, TT]) for i in range(2)]
        self.r_psu = [p.res("psu%d" % i) for i in range(2)]
        self.ps_d = [p.ps("ps_d%d" % i, [128, TT]) for i in range(4)]
        self.r_psd = [p.res("psd%d" % i) for i in range(4)]
        self.wn = p.sb("mlp_wn", [128, KD], F32)
        self.r_wn = p.res("mlp_wn")
        self.iu = 0
        self.id = 0
        self.ih = 0
        self.ipu = 0

    def load_up(self, w_up, fb):
        s = self.iu % self.NS
        self.iu += 1
        self.p.dma("pool", self.wu[s][:], w_up[:, fb * 512:(fb + 1) * 512].rearrange("(k p) f -> p k f", p=128),
                   self.r_wu[s], (), (self.r_wu[s],))
        return s

    def load_down(self, w_down, half, g):
        s = self.id % self.NS
        self.id += 1
        src = w_down[g * 1024:(g + 1) * 1024, half * 512:(half + 1) * 512].rearrange("(k p) d -> p k d", p=128)
        self.p.dma("pool", self.wd[s][:], src, self.r_wd[s], (), (self.r_wd[s],))
        return s

    def run(self, x_in, x_out, ntiles, wn_vec, w_up, w_down, final_w=None):
        p, c = self.p, self.c
        load_cols(p, "sp", self.wn, self.r_wn, wn_vec, KD)
        if final_w is not None:
            self.fw = p.sb("mlp_fw", [128, KD], F32)
            self.r_fw = p.res("mlp_fw")
            load_cols(p, "sp", self.fw, self.r_fw, final_w, KD)
            self.fo = p.sb("mlp_fo", [128, KD, TT], F32)
            self.r_fo = p.res("mlp_fo")
        xs = [p.sb("mlpx%d" % i, [128, KD, TT], F32) for i in range(2)]
        r_xs = [p.res("mlpx%d" % i) for i in range(2)]
        sched = []
        for t in range(ntiles):
            for fb in range(8):
                sched.append(("u", fb))
            for half in range(2):
                for g in range(4):
                    sched.append(("d", half, g))
        slots = {}
        nxt = [0]

        def prefetch(upto):
            while nxt[0] < len(sched) and nxt[0] <= upto:
                it = sched[nxt[0]]
                if it[0] == "u":
                    slots[nxt[0]] = self.load_up(w_up, it[1])
                else:
                    slots[nxt[0]] = self.load_down(w_down, it[1], it[2])
                nxt[0] += 1

        def load_x(t):
            p.dma("sp", xs[t % 2][:], x_in[:, t * TT:(t + 1) * TT].rearrange("(k q) n -> q k n", q=128),
                  r_xs[t % 2], (), (r_xs[t % 2],))

        def norm(t):
            hs = self.ih % 2
            self.ih += 1
            rmsnorm_fm(p, c, xs[t % 2], r_xs[t % 2], self.wn, self.r_wn, self.h[hs], self.r_h[hs], TT)
            return hs

        prefetch(1)
        load_x(0)
        hs = norm(0)
        si = 0
        for t in range(ntiles):
            hb = t % 2
            xt, r_xt = xs[t % 2], r_xs[t % 2]
            for fb in range(8):
                prefetch(si + self.NS - 1)
                s = slots[si]
                si += 1
                for ft in range(4):
                    pu = self.ipu % 2
                    self.ipu += 1
                    for k in range(KD):
                        MM(p, self.ps_u[pu][:], self.wu[s][:, k, ft * 128:(ft + 1) * 128], self.h[hs][:, k, :],
                           k == 0, k == KD - 1, (self.r_wu[s], self.r_h[hs]), (self.r_psu[pu],))
                    ACT(p, AF.Relu, self.rl[pu][:], self.ps_u[pu][:], (self.r_psu[pu],), (self.r_rl[pu],))
                    f = fb * 4 + ft
                    p.op("dve", lambda g, f=f, pu=pu: g.tensor_tensor(
                        out=self.hid[hb][:, f, :], in0=self.rl[pu][:], in1=self.rl[pu][:], op=ALU.mult),
                        (self.r_rl[pu],), (self.r_hid[hb],))
            if t + 1 < ntiles:
                load_x(t + 1)
                hs_next = norm(t + 1)
            else:
                hs_next = None
            for half in range(2):
                for g in range(4):
                    prefetch(si + self.NS - 1)
                    s = slots[si]
                    si += 1
                    for dt_ in range(4):
                        for kk in range(8):
                            fc = g * 8 + kk
                            MM(p, self.ps_d[dt_][:], self.wd[s][:, kk, dt_ * 128:(dt_ + 1) * 128],
                               self.hid[hb][:, fc, :], fc == 0, fc == 31,
                               (self.r_wd[s], self.r_hid[hb]), (self.r_psd[dt_],))
                for dt_ in range(4):
                    k = half * 4 + dt_
                    p.op("dve", lambda g, k=k, dt_=dt_: g.tensor_tensor(
                        out=xt[:, k, :], in0=xt[:, k, :], in1=self.ps_d[dt_][:], op=ALU.add),
                        (self.r_psd[dt_], r_xt), (r_xt,))
            if final_w is None:
                p.dma("sp", x_out[:, t * TT:(t + 1) * TT].rearrange("(k q) n -> q k n", q=128), xt[:], r_xt,
                      (r_xt,), (), is_output=True)
            else:
                for k in range(KD):
                    ACT(p, AF.Square, c.sq[:, k, :], xt[:, k, :], (r_xt,), (c.r_sq,))
                for k in range(KD):
                    MM(p, c.ps_n[:], c.ones_bf[:], c.sq[:, k, :], k == 0, k == KD - 1, (c.r_sq, c.r_ones), (c.r_psn,))
                ACT(p, AF.Sqrt, c.rstd[:], c.ps_n[:], (c.r_psn, c.r_eps), (c.r_rstd,), scale=1.0 / D, bias=c.eps[:, 0:1])
                p.op("dve", lambda g: g.reciprocal(out=c.rstd[:], in_=c.rstd[:]), (c.r_rstd,), (c.r_rstd,))
                for k in range(KD):
                    p.op("dve", lambda g, k=k: g.scalar_tensor_tensor(
                        out=self.fo[:, k, :], in0=xt[:, k, :], scalar=self.fw[:, k:k + 1], in1=c.rstd[:],
                        op0=ALU.mult, op1=ALU.mult), (r_xt, self.r_fw, c.r_rstd), (self.r_fo,))
                p.dma("sp", x_out[:, t * TT:(t + 1) * TT].rearrange("(k q) n -> q k n", q=128), self.fo[:], self.r_fo,
                      (self.r_fo,), (), is_output=True)
            hs = hs_next


class WStream:
    def __init__(self, p, ns=3):
        self.p = p
        self.ns = ns
        self.t = [p.sb("ws%d" % i, [128, KD, 512], BF16) for i in range(ns)]
        self.r = [p.res("ws%d" % i) for i in range(ns)]
        self.sched = []
        self.issued = 0
        self.used = 0

    def plan(self, item):
        self.sched.append(item)

    def plan_cols(self, w, c0, n=512):
        self.plan([(0, n, w[:, c0:c0 + n].rearrange("(k p) f -> p k f", p=128))])

    def plan_rows(self, w, r0, c0, n=512):
        self.plan([(0, n, w[r0:r0 + 1024, c0:c0 + n].rearrange("(k p) f -> p k f", p=128))])

    def _issue(self, upto):
        while self.issued < len(self.sched) and self.issued <= upto:
            s = self.issued % self.ns
            for (c0, n, src) in self.sched[self.issued]:
                self.p.dma("pool", self.t[s][:, :, c0:c0 + n], src, self.r[s], (), (self.r[s],))
            self.issued += 1

    def next(self):
        i = self.used
        self._issue(i + self.ns - 1)
        self.used += 1
        return self.t[i % self.ns], self.r[i % self.ns]


class Banks:
    def __init__(self, p, n=6):
        self.b = [p.ps("bk%d" % i, [128, 512]) for i in range(n)]
        self.r = [p.res("bk%d" % i) for i in range(n)]
        self.tb = p.ps("bkT", [128, 1024], BF16)
        self.r_tb = p.res("bkT")


HG_C = 64


class HG1:
    def __init__(self, p, c, ws, bk, cd):
        self.p, self.c, self.ws, self.bk = p, c, ws, bk
        f32t = lambda n: p.sb(n, [128, TT], F32)
        self.xs = [p.sb("g1x%d" % i, [128, KD, TT], F32) for i in range(2)]
        self.r_xs = [p.res("g1x%d" % i) for i in range(2)]
        self.h = [p.sb("g1h%d" % i, [128, KD, TT], BF16) for i in range(1)]
        self.r_h = [p.res("g1h%d" % i) for i in range(1)]
        self.vtok = p.sb("vtok", [64, 8, 1024], BF16)
        self.r_vtok = p.res("vtok")
        self.qt = p.sb("qt", [128, 8, TT], BF16)
        self.kt = p.sb("kt", [128, 8, TT], BF16)
        self.kh = p.sb("kh", [128, 8, TT], BF16)
        self.r_qt, self.r_kt, self.r_kh = p.res("qt"), p.res("kt"), p.res("kh")
        names = ("sig", "ksig", "lf", "b", "Bg", "eb", "enb", "eB", "d2")
        self.Tm = [{n: f32t(n + str(i)) for n in names} for i in range(2)]
        self.Rt = [{n: p.res(n + str(i)) for n in names} for i in range(2)]
        self.a_all = p.sb("a_all", [128, 8, 8], F32)
        self.r_a = p.res("a_all")
        self.bgl = p.sb("bgl", [128, 8], F32)
        self.r_bgl = p.res("bgl")
        self.S = p.sb("S32", [128, 8, 128], F32)
        self.Sb = p.sb("Sbf", [128, 8, 128], BF16)
        self.r_S, self.r_Sb = p.res("S32"), p.res("Sbf")
        self.attn = p.sb("attn", [64, 8, 64], BF16)
        self.r_attn = p.res("attn")
        self.khat = p.sb("khat", [64, 8, 128], BF16)
        self.r_khat = p.res("khat")
        self.ost = p.sb("ost", [128, 8, TT], F32)
        self.r_ost = p.res("ost")
        self.qd = [p.sb("qdst%d" % i, [128, TT], BF16) for i in range(2)]
        self.r_qd = [p.res("qdst%d" % i) for i in range(2)]
        self.sg = [p.sb("sgst%d" % i, [128, TT], BF16) for i in range(2)]
        self.r_sg = [p.res("sgst%d" % i) for i in range(2)]
        self.wn = p.sb("g1wn", [128, KD], F32)
        self.r_wn = p.res("g1wn")
        self.alb = p.sb("alb", [128, 2, 8], F32)
        self.lbt = p.sb("lbt", [128, 8, 8], F32)
        self.r_lb = p.res("lb")
        self.ones32 = p.sb("ones32", [128, TT], F32)
        self.r_ones32 = p.res("ones32")
        self.cd = cd

    def setup(self, norm_w, alb_dram, layer):
        p = self.p
        load_cols(p, "sp", self.wn, self.r_wn, norm_w, KD)
        with p.nc.allow_non_contiguous_dma(reason="tiny"):
            p.dma("sp", self.alb[:], alb_dram.rearrange("l (h q) -> q l h", q=128), self.r_lb, (), (self.r_lb,))
        L = self.lbt
        R = (self.r_lb,)
        a0, a1 = self.alb[:, 0, :], self.alb[:, 1, :]
        p.op("dve", lambda g: g.tensor_tensor(out=L[:, 0, :], in0=a0, in1=a1, op=ALU.max), R, R)
        p.op("dve", lambda g: g.tensor_tensor(out=L[:, 1, :], in0=a0, in1=L[:, 0, :], op=ALU.subtract), R, R)
        p.op("dve", lambda g: g.tensor_tensor(out=L[:, 2, :], in0=a1, in1=L[:, 0, :], op=ALU.subtract), R, R)
        ACT(p, AF.Exp, L[:, 1:3, :], L[:, 1:3, :], R, R)
        p.op("dve", lambda g: g.tensor_tensor(out=L[:, 3, :], in0=L[:, 1, :], in1=L[:, 2, :], op=ALU.add), R, R)
        p.op("dve", lambda g: g.reciprocal(out=L[:, 3, :], in_=L[:, 3, :]), R, R)
        p.op("dve", lambda g: g.tensor_tensor(out=L[:, 4, :], in0=L[:, 1, :], in1=L[:, 3, :], op=ALU.mult), R, R)
        if layer == 0:
            p.op("dve", lambda g: g.tensor_copy(out=L[:, 5, :], in_=L[:, 4, :]), R, R)
        else:
            p.op("dve", lambda g: g.tensor_tensor(out=L[:, 5, :], in0=L[:, 2, :], in1=L[:, 3, :], op=ALU.mult), R, R)
            p.op("dve", lambda g: g.tensor_tensor(out=L[:, 5, :], in0=L[:, 5, :], in1=L[:, 4, :], op=ALU.add), R, R)
        p.op("dve", lambda g: g.tensor_tensor(out=L[:, 6, :], in0=L[:, 5, :], in1=L[:, 4, :], op=ALU.subtract), R, R)
        p.op("dve", lambda g: g.tensor_scalar(out=L[:, 7, :], in0=L[:, 6, :], scalar1=-1.0, scalar2=1.0,
                                              op0=ALU.mult, op1=ALU.add), R, R)
        p.op("dve", lambda g: g.memset(self.ones32[:], 1.0), (), (self.r_ones32,))
        p.op("dve", lambda g: g.memset(self.S[:], 0.0), (), (self.r_S,))
        p.op("dve", lambda g: g.memset(self.Sb[:], 0.0), (), (self.r_Sb,))
        p.op("dve", lambda g: g.memset(self.bgl[:], 0.0), (), (self.r_bgl,))

    def plan_weights(self, w_in, ntiles):
        for t in range(ntiles):
            for hb in range(4):
                self.ws.plan([(0, 256, w_in[:, hb * 256:(hb + 1) * 256].rearrange("(k p) f -> p k f", p=128)),
                              (256, 256, w_in[:, 1024 + hb * 256:1024 + (hb + 1) * 256].rearrange("(k p) f -> p k f", p=128))])
            for gb in range(2):
                self.ws.plan_cols(w_in, 3072 + gb * 512)
            for vb in range(2):
                self.ws.plan_cols(w_in, 2048 + vb * 512)

    def tile(self, t, xT_dram, o_loc, qdec, sgo):
        p, c, bk = self.p, self.c, self.bk
        T0 = t * TT
        xs, r_xs = self.xs[t % 2], self.r_xs[t % 2]
        p.dma("sp", xs[:], xT_dram[:, T0:T0 + TT].rearrange("(k q) n -> q k n", q=128), r_xs, (), (r_xs,))
        h, r_h = self.h[0], self.r_h[0]
        rmsnorm_fm(p, c, xs, r_xs, self.wn, self.r_wn, h, r_h, TT)
        L = self.lbt
        for hb in range(4):
            w, r_w = self.ws.next()
            for hh in range(2):
                hd = hb * 2 + hh
                pq, r_pq = bk.b[hd % 2], bk.r[hd % 2]
                pf, r_pf = bk.b[2 + hd % 2], bk.r[2 + hd % 2]
                for k in range(KD):
                    MM(p, pq[:], w[:, k, hh * 128:(hh + 1) * 128], h[:, k, :], k == 0, k == KD - 1, (r_w, r_h), (r_pq,))
                for k in range(KD):
                    MM(p, pf[:], w[:, k, 256 + hh * 128:256 + (hh + 1) * 128], h[:, k, :], k == 0, k == KD - 1,
                       (r_w, r_h), (r_pf,))
                lb_c, oml_c = L[:, 6, hd:hd + 1], L[:, 7, hd:hd + 1]
                Tm, rt = self.Tm[hd % 2], self.Rt[hd % 2]
                ACT(p, AF.Sigmoid, Tm["sig"][:], pf[:], (r_pf,), (rt["sig"],))
                ACT(p, AF.Sigmoid, Tm["ksig"][:], pf[:], (r_pf,), (rt["ksig"],), scale=-1.0)
                p.op("dve", lambda g: g.tensor_scalar(out=Tm["lf"][:], in0=Tm["sig"][:], scalar1=oml_c, scalar2=lb_c,
                                                      op0=ALU.mult, op1=ALU.add), (rt["sig"], self.r_lb), (rt["lf"],))
                p.op("pool", lambda g: g.tensor_scalar(out=Tm["lf"][:], in0=Tm["lf"][:], scalar1=1e-30, scalar2=None, op0=ALU.max),
                     (rt["lf"],), (rt["lf"],))
                ACT(p, AF.Ln, Tm["lf"][:], Tm["lf"][:], (rt["lf"],), (rt["lf"],))
                p.op("pool", lambda g: g.tensor_scalar(out=Tm["ksig"][:], in0=Tm["ksig"][:], scalar1=oml_c, scalar2=None, op0=ALU.mult),
                     (rt["ksig"], self.r_lb), (rt["ksig"],))
                p.op("dve", lambda g: g.tensor_tensor_scan(out=Tm["b"][:], data0=self.cd["reset"][:], data1=Tm["lf"][:],
                                                           initial=0.0, op0=ALU.mult, op1=ALU.add),
                     (rt["lf"], self.cd["r"]), (rt["b"],))
                p.op("dve", lambda g: g.tensor_tensor_scan(out=Tm["Bg"][:], data0=self.ones32[:], data1=Tm["lf"][:],
                                                           initial=self.bgl[:, hd:hd + 1], op0=ALU.mult, op1=ALU.add),
                     (rt["lf"], self.r_ones32, self.r_bgl), (rt["Bg"],))
                p.op("dve", lambda g: g.tensor_copy(out=self.bgl[:, hd:hd + 1], in_=Tm["Bg"][:, TT - 1:TT]),
                     (rt["Bg"],), (self.r_bgl,))
                ACT(p, AF.Exp, Tm["eb"][:], Tm["b"][:], (rt["b"],), (rt["eb"],))
                ACT(p, AF.Exp, Tm["enb"][:], Tm["b"][:], (rt["b"],), (rt["enb"],), scale=-1.0)
                ACT(p, AF.Exp, Tm["eB"][:], Tm["Bg"][:], (rt["Bg"],), (rt["eB"],))
                b3 = Tm["b"][:].rearrange("q (c s) -> q c s", s=HG_C)
                p.op("pool", lambda g: g.tensor_tensor(out=Tm["d2"][:].rearrange("q (c s) -> q c s", s=HG_C),
                                                       in0=b3[:, :, HG_C - 1:HG_C].to_broadcast([128, 8, HG_C]),
                                                       in1=b3, op=ALU.subtract), (rt["b"],), (rt["d2"],))
                ACT(p, AF.Exp, Tm["d2"][:], Tm["d2"][:], (rt["d2"],), (rt["d2"],))
                p.op("dve", lambda g: g.tensor_copy(out=self.a_all[:, hd, :], in_=Tm["eb"][:, HG_C - 1::HG_C]),
                     (rt["eb"],), (self.r_a,))
                p.op("dve", lambda g: g.tensor_tensor(out=self.qt[:, hd, :], in0=pq[:], in1=Tm["eb"][:], op=ALU.mult),
                     (r_pq, rt["eb"]), (self.r_qt,))
                qd, r_qd = self.qd[hd % 2], self.r_qd[hd % 2]
                p.op("dve", lambda g: g.tensor_tensor(out=qd[:], in0=pq[:], in1=Tm["eB"][:], op=ALU.mult),
                     (r_pq, rt["eB"]), (r_qd,))
                p.dma("sp", qdec[hd * 128:(hd + 1) * 128, T0:T0 + TT], qd[:], r_qd, (r_qd,), (), is_output=True)
                p.op("dve", lambda g: g.tensor_tensor(out=self.kt[:, hd, :], in0=Tm["ksig"][:], in1=Tm["enb"][:], op=ALU.mult),
                     (rt["ksig"], rt["enb"]), (self.r_kt,))
                p.op("dve", lambda g: g.tensor_tensor(out=self.kh[:, hd, :], in0=Tm["ksig"][:], in1=Tm["d2"][:], op=ALU.mult),
                     (rt["ksig"], rt["d2"]), (self.r_kh,))
        for gb in range(2):
            w, r_w = self.ws.next()
            for hh in range(4):
                hd = gb * 4 + hh
                pg, r_pg = bk.b[4 + hd % 2], bk.r[4 + hd % 2]
                for k in range(KD):
                    MM(p, pg[:], w[:, k, hh * 128:(hh + 1) * 128], h[:, k, :], k == 0, k == KD - 1, (r_w, r_h), (r_pg,))
                sg, r_sg = self.sg[hd % 2], self.r_sg[hd % 2]
                ACT(p, AF.Silu, sg[:], pg[:], (r_pg,), (r_sg,))
                p.dma("sp", sgo[hd * 128:(hd + 1) * 128, T0:T0 + TT], sg[:], r_sg, (r_sg,), (), is_output=True)
        for vb in range(2):
            w, r_w = self.ws.next()
            for ci in range(8):
                pv, r_pv = bk.b[4 + ci % 2], bk.r[4 + ci % 2]
                for k in range(KD):
                    MM(p, pv[0:64, :], h[:, k, ci * HG_C:(ci + 1) * HG_C], w[:, k, :], k == 0, k == KD - 1,
                       (r_w, r_h), (r_pv,))
                ACT(p, AF.Copy, self.vtok[:, ci, vb * 512:(vb + 1) * 512], pv[0:64, :], (r_pv,), (self.r_vtok,))
        pa, r_pa = bk.b[4], bk.r[4]
        po, r_po = bk.b[5], bk.r[5]
        pU, r_pU = (bk.b[0], bk.b[1]), (bk.r[0], bk.r[1])
        for ci in range(8):
            cs = slice(ci * HG_C, (ci + 1) * HG_C)
            for hd in range(8):
                MM(p, pa[0:64, hd * 64:(hd + 1) * 64], self.kt[:, hd, cs], self.qt[:, hd, cs], True, True,
                   (self.r_kt, self.r_qt), (r_pa,))
            p.op("dve", lambda g: g.tensor_tensor(out=self.attn[:], in0=pa[0:64, :].rearrange("q (h s) -> q h s", s=64),
                                                  in1=self.cd["tri"][:].unsqueeze(1).to_broadcast([64, 8, 64]),
                                                  op=ALU.mult), (r_pa, self.cd["r"]), (self.r_attn,))
            for hd in range(8):
                p.op("pe", lambda g, hd=hd: g.transpose(bk.tb[0:64, hd * 128:(hd + 1) * 128], self.kh[:, hd, cs],
                                                        self.cd["ident"][:]),
                     (self.r_kh, self.cd["r"]), (bk.r_tb,))
            ACT(p, AF.Copy, self.khat[:].rearrange("q h d -> q (h d)"), bk.tb[0:64, :], (bk.r_tb,), (self.r_khat,))
            for hd in range(8):
                MM(p, pU[hd // 4][:, (hd % 4) * 128:(hd % 4 + 1) * 128], self.khat[:, hd, :],
                   self.vtok[:, ci, hd * 128:(hd + 1) * 128], True, True,
                   (self.r_khat, self.r_vtok), (r_pU[hd // 4],))
            for hd in range(8):
                MM(p, po[:, hd * 64:(hd + 1) * 64], self.vtok[:, ci, hd * 128:(hd + 1) * 128], self.attn[:, hd, :],
                   True, False, (self.r_vtok, self.r_attn), (r_po,))
                MM(p, po[:, hd * 64:(hd + 1) * 64], self.Sb[:, hd, :], self.qt[:, hd, cs],
                   False, True, (self.r_Sb, self.r_qt), (r_po,))
            ACT(p, AF.Copy, self.ost[:, :, cs], po[:].rearrange("q (h s) -> q h s", s=64), (r_po,), (self.r_ost,))
            for hd in range(8):
                p.op("dve", lambda g, hd=hd: g.scalar_tensor_tensor(
                    out=self.S[:, hd, :], in0=self.S[:, hd, :], scalar=self.a_all[:, hd, ci:ci + 1],
                    in1=pU[hd // 4][:, (hd % 4) * 128:(hd % 4 + 1) * 128], op0=ALU.mult, op1=ALU.add),
                    (self.r_S, self.r_a, r_pU[hd // 4]), (self.r_S,))
            ACT(p, AF.Copy, self.Sb[:], self.S[:], (self.r_S,), (self.r_Sb,))
        p.dma("sp", o_loc[:, T0:T0 + TT].rearrange("(h q) n -> q h n", q=128), self.ost[:], self.r_ost,
              (self.r_ost,), (), is_output=True)

    def finish(self, s_fin, dec):
        p = self.p
        p.dma("sp", s_fin, self.S[:], self.r_S, (self.r_S,), (), is_output=True)
        ACT(p, AF.Exp, self.lbt[:, 0, :], self.bgl[:], (self.r_bgl,), (self.r_lb,))
        p.dma("sp", dec, self.lbt[:, 0, :], self.r_lb, (self.r_lb,), (), is_output=True)


def load_consts_hg(p, reset_d, tri_d, ident_d):
    cd = {"r": p.res("cd")}
    cd["reset"] = p.sb("c_reset", [128, TT], F32)
    cd["tri"] = p.sb("c_tri", [64, 64], BF16)
    cd["ident"] = p.sb("c_ident", [128, 128], BF16)
    p.dma("sp", cd["reset"][:], reset_d, cd["r"], (), (cd["r"],))
    p.dma("sp", cd["tri"][:], tri_d, cd["r"], (), (cd["r"],))
    p.dma("sp", cd["ident"][:], ident_d, cd["r"], (), (cd["r"],))
    return cd


class HG2:
    def __init__(self, p, c, ws, bk):
        self.p, self.c, self.ws, self.bk = p, c, ws, bk
        self.Sin = p.sb("Sin", [128, 8, 128], F32)
        self.SinB = p.sb("SinB", [128, 8, 128], BF16)
        self.r_Sin, self.r_SinB = p.res("Sin"), p.res("SinB")
        self.Sf = [p.sb("Sf%d" % i, [128, 8, 128], F32) for i in range(2)]
        self.r_Sf = [p.res("Sf%d" % i) for i in range(2)]
        self.dcl = p.sb("dcl", [128, 8, 8], F32)
        self.selv = p.sb("selv", [128, 8], F32)
        self.al = p.sb("al", [128, 8], F32)
        self.r_sm = p.res("hg2small")
        self.gw = p.sb("gw", [128, 1], F32)
        self.r_gw = p.res("gw")
        self.xs = [p.sb("g2x%d" % i, [128, KD, TT], F32) for i in range(2)]
        self.r_xs = [p.res("g2x%d" % i) for i in range(2)]
        self.ot = p.sb("g2o", [128, 8, TT], F32)
        self.qd = p.sb("g2qd", [128, 8, TT], BF16)
        self.sgt = p.sb("g2sg", [128, 8, TT], BF16)
        self.on = p.sb("g2on", [128, 8, TT], BF16)
        self.tmp = p.sb("g2tmp", [128, TT], F32)
        self.r_ot, self.r_qd, self.r_sgt, self.r_on, self.r_tmp = (p.res("g2o"), p.res("g2qd"), p.res("g2sg"),
                                                                   p.res("g2on"), p.res("g2tmp"))
        self.eps128 = c.eps

    def prefix(self, S_all, dec_all, selv_d, gnorm_w):
        p = self.p
        R = (self.r_sm,)
        with p.nc.allow_non_contiguous_dma(reason="tiny"):
            p.dma("sp", self.dcl[:], dec_all.rearrange("c q h -> q c h"), self.r_sm, (), R)
            p.dma("sp", self.selv[:], selv_d, self.r_sm, (), R)
            p.dma("sp", self.gw[:], gnorm_w.rearrange("(q o) -> q o", o=1), self.r_gw, (), (self.r_gw,))
        p.op("dve", lambda g: g.memset(self.Sin[:], 0.0), (), (self.r_Sin,))
        for cp in range(8):
            sf, r_sf = self.Sf[cp % 2], self.r_Sf[cp % 2]
            p.dma("sp", sf[:], S_all[cp], r_sf, (), (r_sf,))
            sel_c = self.selv[:, cp:cp + 1]
            p.op("dve", lambda g: g.tensor_scalar(out=self.al[:], in0=self.dcl[:, cp, :], scalar1=-1.0, scalar2=sel_c,
                                                  op0=ALU.add, op1=ALU.mult), R, R)
            p.op("dve", lambda g: g.tensor_scalar(out=self.al[:], in0=self.al[:], scalar1=1.0, scalar2=None,
                                                  op0=ALU.add), R, R)
            p.op("dve", lambda g: g.tensor_scalar(out=sf[:], in0=sf[:], scalar1=sel_c, scalar2=None, op0=ALU.mult),
                 (r_sf, self.r_sm), (r_sf,))
            for hd in range(8):
                p.op("dve", lambda g, hd=hd: g.scalar_tensor_tensor(
                    out=self.Sin[:, hd, :], in0=self.Sin[:, hd, :], scalar=self.al[:, hd:hd + 1], in1=sf[:, hd, :],
                    op0=ALU.mult, op1=ALU.add), (self.r_Sin, self.r_sm, r_sf), (self.r_Sin,))
        ACT(p, AF.Copy, self.SinB[:], self.Sin[:], (self.r_Sin,), (self.r_SinB,))

    def plan_weights(self, w_out, ntiles):
        for t in range(ntiles):
            for blk in range(2):
                self.ws.plan_cols(w_out, blk * 512)

    def tile(self, t, x_in, x_out, o_loc, qdec, sgo):
        p, c, bk = self.p, self.c, self.bk
        T0 = t * TT
        xs, r_xs = self.xs[t % 2], self.r_xs[t % 2]
        p.dma("sp", xs[:], x_in[:, T0:T0 + TT].rearrange("(k q) n -> q k n", q=128), r_xs, (), (r_xs,))
        p.dma("sp", self.ot[:], o_loc[:, T0:T0 + TT].rearrange("(h q) n -> q h n", q=128), self.r_ot, (), (self.r_ot,))
        p.dma("sp", self.qd[:], qdec[:, T0:T0 + TT].rearrange("(h q) n -> q h n", q=128), self.r_qd, (), (self.r_qd,))
        p.dma("sp", self.sgt[:], sgo[:, T0:T0 + TT].rearrange("(h q) n -> q h n", q=128), self.r_sgt, (), (self.r_sgt,))
        for hd in range(8):
            pc, r_pc = bk.b[hd % 2], bk.r[hd % 2]
            MM(p, pc[:], self.SinB[:, hd, :], self.qd[:, hd, :], True, True, (self.r_SinB, self.r_qd), (r_pc,))
            p.op("dve", lambda g: g.tensor_tensor(out=self.ot[:, hd, :], in0=self.ot[:, hd, :], in1=pc[:], op=ALU.add),
                 (self.r_ot, r_pc), (self.r_ot,))
            ACT(p, AF.Square, c.sq[:, hd, :], self.ot[:, hd, :], (self.r_ot,), (c.r_sq,))
            pn, r_pn = bk.b[2 + hd % 2], bk.r[2 + hd % 2]
            MM(p, pn[:], c.ones_bf[:], c.sq[:, hd, :], True, True, (c.r_sq, c.r_ones), (r_pn,))
            ACT(p, AF.Sqrt, c.rstd[:], pn[:], (r_pn, c.r_eps), (c.r_rstd,), scale=1.0 / 128, bias=c.eps[:, 0:1])
            p.op("dve", lambda g: g.reciprocal(out=c.rstd[:], in_=c.rstd[:]), (c.r_rstd,), (c.r_rstd,))
            p.op("dve", lambda g: g.scalar_tensor_tensor(out=self.tmp[:], in0=self.ot[:, hd, :], scalar=self.gw[:, 0:1],
                                                         in1=c.rstd[:], op0=ALU.mult, op1=ALU.mult),
                 (self.r_ot, self.r_gw, c.r_rstd), (self.r_tmp,))
            p.op("pool", lambda g: g.tensor_tensor(out=self.on[:, hd, :], in0=self.tmp[:], in1=self.sgt[:, hd, :],
                                                   op=ALU.mult), (self.r_tmp, self.r_sgt), (self.r_on,))
        for blk in range(2):
            w, r_w = self.ws.next()
            for dt_ in range(4):
                po, r_po = bk.b[4 + dt_ % 2], bk.r[4 + dt_ % 2]
                for k in range(KD):
                    MM(p, po[:], w[:, k, dt_ * 128:(dt_ + 1) * 128], self.on[:, k, :], k == 0, k == KD - 1,
                       (r_w, self.r_on), (r_po,))
                kk = blk * 4 + dt_
                p.op("dve", lambda g: g.tensor_tensor(out=xs[:, kk, :], in0=xs[:, kk, :], in1=po[:], op=ALU.add),
                     (r_xs, r_po), (r_xs,))
        p.dma("sp", x_out[:, T0:T0 + TT].rearrange("(k q) n -> q k n", q=128), xs[:], r_xs, (r_xs,), (), is_output=True)


def host_consts_hg():
    import ml_dtypes
    reset = np.ones((128, TT), np.float32)
    reset[:, ::HG_C] = 0.0
    tri = (np.arange(64)[:, None] <= np.arange(64)[None, :]).astype(ml_dtypes.bfloat16)
    ident = np.eye(128).astype(ml_dtypes.bfloat16)
    return {"c_reset": reset, "c_tri": tri, "c_ident": ident}


NSA_SLOPES = [2.0 ** (-(h + 1) / 2.0) for h in range(16)]
QK_SCALE = 0.125
NAUG = 7
KROWS = 96


class KVPhase:
    def __init__(self, p, c, ws, bk):
        self.p, self.c, self.ws, self.bk = p, c, ws, bk
        self.xs = [p.sb("kvx%d" % i, [128, KD, TT], F32) for i in range(2)]
        self.r_xs = [p.res("kvx%d" % i) for i in range(2)]
        self.h = p.sb("kvh", [128, KD, TT], BF16)
        self.r_h = p.res("kvh")
        self.wn = p.sb("kvwn", [128, KD], F32)
        self.r_wn = p.res("kvwn")
        self.st = [p.sb("kvst%d" % i, [128, TT], BF16) for i in range(2)]
        self.r_st = [p.res("kvst%d" % i) for i in range(2)]
        self.sq32 = p.sb("kvsq", [128, TT], BF16)
        self.r_sq32 = p.res("kvsq")
        self.bd = p.sb("kvbd", [128, 128], BF16)
        self.r_bd = p.res("kvbd")
        self.kmx = p.sb("kvkmx", [128, 4], F32)
        self.mtmp = p.sb("kvmt", [128, 1], F32)
        self.r_kmx = p.res("kvkmx")
        self.vst = [p.sb("kvvst%d" % i, [128, 8, 65], BF16) for i in range(2)]
        self.r_vst = [p.res("kvvst%d" % i) for i in range(2)]

    def setup(self, norm_w):
        p = self.p
        load_cols(p, "sp", self.wn, self.r_wn, norm_w, KD)
        p.op("dve", lambda g: g.memset(self.bd[:], 0.0), (), (self.r_bd,))
        p.op("dve", lambda g: g.memset(self.bd[0:64, 0:64], 1.0), (), (self.r_bd,))
        p.op("dve", lambda g: g.memset(self.bd[64:128, 64:128], 1.0), (), (self.r_bd,))
        p.op("dve", lambda g: g.memset(self.kmx[:], 0.0), (), (self.r_kmx,))
        for i in range(2):
            p.op("dve", lambda g, i=i: g.memset(self.vst[i][:], 1.0), (), (self.r_vst[i],))

    def plan_weights(self, kv_w, ntiles):
        re = lambda a: a.rearrange("(k q) f -> q k f", q=128)
        for t in range(ntiles):
            self.ws.plan([(0, 512, re(kv_w[:, 0:512]))])
            self.ws.plan([(0, 256, re(kv_w[:, 512:768])), (256, 256, re(kv_w[:, 1024:1280]))])
            self.ws.plan([(0, 256, re(kv_w[:, 768:1024])), (256, 256, re(kv_w[:, 1280:1536]))])

    def tile(self, t, x_in, kT01, kT24, vaug):
        p, c, bk = self.p, self.c, self.bk
        T0 = t * TT
        xs, r_xs = self.xs[t % 2], self.r_xs[t % 2]
        p.dma("sp", xs[:], x_in[:, T0:T0 + TT].rearrange("(k q) n -> q k n", q=128), r_xs, (), (r_xs,))
        rmsnorm_fm(p, c, xs, r_xs, self.wn, self.r_wn, self.h, self.r_h, TT)
        h, r_h = self.h, self.r_h
        n = 0
        for blk, dst in ((0, kT01), (1, kT24)):
            w, r_w = self.ws.next()
            for ct in range(4):
                pp, r_pp = bk.b[n % 2], bk.r[n % 2]
                st, r_st = self.st[n % 2], self.r_st[n % 2]
                n += 1
                for k in range(KD):
                    MM(p, pp[:], w[:, k, ct * 128:(ct + 1) * 128], h[:, k, :], k == 0, k == KD - 1, (r_w, r_h), (r_pp,))
                ACT(p, AF.Copy, st[:], pp[:], (r_pp,), (r_st,))
                p.dma("sp", dst[ct * 128:(ct + 1) * 128, T0:T0 + TT], st[:], r_st, (r_st,), (), is_output=True)
                if blk == 1:
                    ACT(p, AF.Square, self.sq32[:], pp[:], (r_pp,), (self.r_sq32,))
                    pm, r_pm = bk.b[2], bk.r[2]
                    MM(p, pm[:], self.bd[:], self.sq32[:], True, True, (self.r_bd, self.r_sq32), (r_pm,))
                    p.op("dve", lambda g: g.reduce_max(out=self.mtmp[:], in_=pm[:], axis=AX.X), (r_pm,), (self.r_kmx,))
                    p.op("dve", lambda g, ct=ct: g.tensor_tensor(out=self.kmx[:, ct:ct + 1], in0=self.kmx[:, ct:ct + 1],
                                                                 in1=self.mtmp[:], op=ALU.max), (self.r_kmx,), (self.r_kmx,))
        w, r_w = self.ws.next()
        for sub in range(TT // 128):
            pp, r_pp = bk.b[3 + sub % 2], bk.r[3 + sub % 2]
            vst, r_vst = self.vst[sub % 2], self.r_vst[sub % 2]
            for k in range(KD):
                MM(p, pp[:], h[:, k, sub * 128:(sub + 1) * 128], w[:, k, :], k == 0, k == KD - 1, (r_w, r_h), (r_pp,))
            ACT(p, AF.Copy, vst[:, :, 0:64], pp[:].rearrange("q (s d) -> q s d", d=64), (r_pp,), (r_vst,))
            ch = t * (TT // 128) + sub
            p.dma("sp", vaug[ch], vst[:], r_vst, (r_vst,), (), is_output=True)

    def finish(self, kmx_out):
        p = self.p
        p.dma("sp", kmx_out, self.kmx[:], self.r_kmx, (self.r_kmx,), (), is_output=True)


class CMPPhase:
    def __init__(self, p, c, bk, NB):
        self.p, self.c, self.bk = p, c, bk
        self.NB = NB
        self.kin = p.sb("cmkin", [64, 4, 16 * NB + 16], BF16)
        self.r_kin = p.res("cmkin")
        self.w1 = p.sb("cmw1", [64, 32, 256], BF16)
        self.r_w1 = p.res("cmw1")
        self.w2 = p.sb("cmw2", [128, 2, 64], BF16)
        self.r_w2 = p.res("cmw2")
        self.peT = p.sb("cmpe", [64, 32], BF16)
        self.r_pe = p.res("cmpe")
        self.c1 = p.sb("cmc1", [128, 2], F32)
        self.r_c1 = p.res("cmc1")
        self.z = p.sb("cmz", [128, NB], F32)
        self.z2 = p.sb("cmz2", [128, NB], F32)
        self.r_z, self.r_z2 = p.res("cmz"), p.res("cmz2")
        self.gl = p.sb("cmgl", [128, 2, NB], BF16)
        self.r_gl = p.res("cmgl")
        self.ko = p.sb("cmko", [64, 4, NB], BF16)
        self.r_ko = p.res("cmko")
        self.vo = p.sb("cmvo", [NB, 4, 65], BF16)
        self.r_vo = p.res("cmvo")
        self.sq = p.sb("cmsq", [64, NB], BF16)
        self.r_sq = p.res("cmsq")
        self.kmx = p.sb("cmkmx", [64, 4], F32)
        self.r_kmx = p.res("cmkmx")

    def run(self, kin_d, vin_d, pe_k, w1_k, w2_k, pe_v, w1_v, w2_v, kc_out, vc_out, kmx_out):
        p, c, bk = self.p, self.c, self.bk
        NB = self.NB
        p.op("dve", lambda g: g.memset(self.vo[:], 1.0), (), (self.r_vo,))
        for si, (src, pe, w1, w2) in enumerate(((kin_d, pe_k, w1_k, w2_k), (vin_d, pe_v, w1_v, w2_v))):
            p.dma("sp", self.kin[:], src.rearrange("g d n -> d g n"), self.r_kin, (), (self.r_kin,))
            p.dma("pool", self.w1[:], w1.rearrange("(j d) m -> d j m", d=64), self.r_w1, (), (self.r_w1,))
            p.dma("pool", self.w2[:], w2.rearrange("(k q) d -> q k d", q=128), self.r_w2, (), (self.r_w2,))
            with p.nc.allow_non_contiguous_dma(reason="tiny pe"):
                p.dma("pool", self.peT[:], pe.rearrange("j d -> d j"), self.r_pe, (), (self.r_pe,))
            for mt in range(2):
                pb, r_pb = bk.b[0], bk.r[0]
                for j in range(32):
                    MM(p, pb[:, 0:1], self.w1[:, j, mt * 128:(mt + 1) * 128], self.peT[:, j:j + 1], j == 0, j == 31,
                       (self.r_w1, self.r_pe), (r_pb,))
                p.op("dve", lambda g, mt=mt: g.tensor_copy(out=self.c1[:, mt:mt + 1], in_=pb[:, 0:1]), (r_pb,), (self.r_c1,))
            for gi in range(4):
                for mt in range(2):
                    ph, r_ph = bk.b[1 + mt], bk.r[1 + mt]
                    for j in range(32):
                        MM(p, ph[:, 0:NB], self.w1[:, j, mt * 128:(mt + 1) * 128], self.kin[:, gi, j:j + 16 * (NB - 1) + 1:16],
                           j == 0, j == 31, (self.r_w1, self.r_kin), (r_ph,))
                    ACT(p, AF.Identity, self.z[:], ph[:, 0:NB], (r_ph, self.r_c1), (self.r_z,), bias=self.c1[:, mt:mt + 1])
                    p.op("dve", lambda g: g.tensor_tensor(out=self.z2[:], in0=self.z[:], in1=self.z[:], op=ALU.mult),
                         (self.r_z,), (self.r_z2,))
                    p.op("dve", lambda g: g.tensor_scalar(out=self.z2[:], in0=self.z2[:], scalar1=0.044715, scalar2=1.0,
                                                          op0=ALU.mult, op1=ALU.add), (self.r_z2,), (self.r_z2,))
                    p.op("dve", lambda g: g.tensor_tensor(out=self.z2[:], in0=self.z2[:], in1=self.z[:], op=ALU.mult),
                         (self.r_z2, self.r_z), (self.r_z2,))
                    ACT(p, AF.Sigmoid, self.z2[:], self.z2[:], (self.r_z2,), (self.r_z2,), scale=1.5957691216057308)
                    p.op("dve", lambda g, mt=mt: g.tensor_tensor(out=self.gl[:, mt, :], in0=self.z2[:], in1=self.z[:],
                                                                 op=ALU.mult), (self.r_z2, self.r_z), (self.r_gl,))
                if si == 0:
                    po, r_po = bk.b[3], bk.r[3]
                    for mt in range(2):
                        MM(p, po[0:64, 0:NB], self.w2[:, mt, :], self.gl[:, mt, :], mt == 0, mt == 1,
                           (self.r_w2, self.r_gl), (r_po,))
                    ACT(p, AF.Copy, self.ko[:, gi, :], po[0:64, 0:NB], (r_po,), (self.r_ko,))
                    ACT(p, AF.Square, self.sq[:], po[0:64, 0:NB], (r_po,), (self.r_sq,))
                    pm, r_pm = bk.b[4], bk.r[4]
                    MM(p, pm[0:64, 0:NB], c.ones_bf[0:64, 0:64], self.sq[:], True, True, (c.r_ones, self.r_sq), (r_pm,))
                    p.op("dve", lambda g, gi=gi: g.reduce_max(out=self.kmx[:, gi:gi + 1], in_=pm[0:64, 0:NB], axis=AX.X),
                         (r_pm,), (self.r_kmx,))
                else:
                    po, r_po = bk.b[3], bk.r[3]
                    for mt in range(2):
                        MM(p, po[0:NB, 0:64], self.gl[:, mt, :], self.w2[:, mt, :], mt == 0, mt == 1,
                           (self.r_w2, self.r_gl), (r_po,))
                    ACT(p, AF.Copy, self.vo[:, gi, 0:64], po[0:NB, 0:64], (r_po,), (self.r_vo,))
        p.dma("sp", kc_out.rearrange("g d n -> d g n"), self.ko[:], self.r_ko, (self.r_ko,), (), is_output=True)
        p.dma("sp", vc_out, self.vo[:], self.r_vo, (self.r_vo,), (), is_output=True)
        p.dma("sp", kmx_out, self.kmx[:], self.r_kmx, (self.r_kmx,), (), is_output=True)


class ATTPhase:
    PC = 16

    def __init__(self, p, c, bk, NS, T):
        self.p, self.c, self.bk, self.NS, self.T = p, c, bk, NS, T
        self.NSB = T // 64
        self.NCc = max(1, (T // 16) // 128)
        NSB, NCc, PC = self.NSB, self.NCc, self.PC
        sb, res = p.sb, p.res
        self.win = sb("at_win", [128, KD, 1072], BF16)
        self.r_win = res("at_win")
        self.wn = sb("at_wn", [128, KD], F32)
        self.r_wn = res("at_wn")
        self.x4 = sb("at_x4", [128, KD, TT], F32)
        self.r_x4 = res("at_x4")
        self.h4 = sb("at_h4", [128, KD, TT], BF16)
        self.r_h4 = res("at_h4")
        self.Qa = [sb("at_Qa%d" % i, [128, 16, KROWS], BF16) for i in range(2)]
        self.r_Qa = [res("at_Qa%d" % i) for i in range(2)]
        self.QT = [sb("at_QT%d" % i, [KROWS, 4, 512], BF16) for i in range(2)]
        self.r_QT = [res("at_QT%d" % i) for i in range(2)]
        self.gates = [sb("at_gt%d" % i, [128, 48], F32) for i in range(2)]
        self.r_gates = [res("at_gt%d" % i) for i in range(2)]
        self.sqt = sb("at_sqt", [128, 512], F32)
        self.r_sqt = res("at_sqt")
        self.sm = sb("at_sm", [128, 8, 16], F32)
        self.r_sm = res("at_sm")
        self.KMs = sb("at_KMs", [128, 16], F32)
        self.kmall = sb("at_kmall", [128, 4, 24], F32)
        self.r_KM = res("at_KM")
        self.cst = {}
        self.r_cst = res("at_cst")
        for nm, shp, dt in (("ident", [128, 128], BF16), ("ident32", [128, 128], F32), ("iota1", [128, 128], F32), ("iota2", [128, 128], F32),
                            ("negL", [128, 128], F32), ("negU", [128, 128], F32), ("apool", [128, NCc, NSB], BF16),
                            ("thr1", [128, NS * NCc], F32), ("thr2", [128, 8], F32), ("negst", [128, NS, 16], F32),
                            ("bonus", [128, NS, NSB], F32)):
            self.cst[nm] = sb("at_c_" + nm, shp, dt)
        self.KTc = sb("at_KTc", [KROWS, 4, NCc * 128], BF16)
        self.Vc = sb("at_Vc", [128, 4, NCc, 65], BF16)
        self.r_kvc = res("at_kvc")
        self.KTp = [sb("at_KTp%d" % i, [KROWS, PC * 128], BF16) for i in range(3)]
        self.Vp = [sb("at_Vp%d" % i, [128, PC, 65], BF16) for i in range(3)]
        self.r_kvp = [res("at_kvp%d" % i) for i in range(3)]
        self.ikv = 0
        self.KTw = [sb("at_KTw%d" % i, [KROWS, 640], BF16) for i in range(2)]
        self.Vw = [sb("at_Vw%d" % i, [128, 5, 65], BF16) for i in range(2)]
        self.r_kvw = [res("at_kvw%d" % i) for i in range(2)]
        self.iw = 0
        self.NE = 6
        self.e = [sb("at_e%d" % i, [128, 512], BF16) for i in range(self.NE)]
        self.r_e = [res("at_e%d" % i) for i in range(self.NE)]
        self.pp = [sb("at_p%d" % i, [128, 512], BF16) for i in range(self.NE)]
        self.r_pp = [res("at_p%d" % i) for i in range(self.NE)]
        self.ie = 0
        self.zt = [sb("at_zt%d" % i, [128, 512], F32) for i in range(2)]
        self.r_zt = [res("at_zt%d" % i) for i in range(2)]
        self.m2 = [sb("at_m2%d" % i, [128, 128], F32) for i in range(3)]
        self.r_m2 = [res("at_m2%d" % i) for i in range(3)]
        self.im2 = 0
        self.sc = sb("at_sc", [128, NSB], F32)
        self.sc2 = sb("at_sc2", [128, NSB], F32)
        self.m8 = sb("at_m8", [128, 16], F32)
        self.sel = sb("at_sel", [128, NSB], BF16)
        self.selx = [sb("at_selx%d" % i, [128, 1024], BF16) for i in range(2)]
        self.r_selx = [res("at_selx%d" % i) for i in range(2)]
        self.r_sc, self.r_sel = res("at_sc"), res("at_sel")
        self.rd = sb("at_rd", [128, 8], F32)
        self.r_rd = res("at_rd")
        self.oacc = sb("at_oacc", [128, 16, 64], F32)
        self.r_oacc = res("at_oacc")
        self.obf = [sb("at_obf%d" % i, [128, 1024], BF16) for i in range(2)]
        self.r_obf = [res("at_obf%d" % i) for i in range(2)]
        self.obT = [sb("at_obT%d" % i, [128, 8, 128], BF16) for i in range(2)]
        self.r_obT = [res("at_obT%d" % i) for i in range(2)]
        self.ps_s = (bk.b[0], bk.b[1])
        self.r_ps_s = (bk.r[0], bk.r[1])
        self.ps_o = (bk.b[3], bk.b[3])
        self.r_ps_o = (bk.r[3], bk.r[3])
        self.poT, self.r_poT = bk.b[2], bk.r[2]
        self.oTs = sb("at_oTs", [65, 512], F32)
        self.r_oTs = res("at_oTs")
        self.ps_r = (bk.b[4], bk.b[5])
        self.r_ps_r = (bk.r[4], bk.r[5])
        self.iss = 0
        self.ipo = 0
        self.r_tbh = (bk.r_tb, bk.r_tb)

    def setup(self, d):
        p = self.p
        load_cols(p, "sp", self.wn, self.r_wn, d["norm_w"], KD)
        re = lambda a: a.rearrange("(k q) f -> q k f", q=128)
        p.dma("pool", self.win[:, :, 0:512], re(d["w_in"][:, 0:512]), self.r_win, (), (self.r_win,))
        p.dma("pool", self.win[:, :, 512:1024], re(d["w_in"][:, 512:1024]), self.r_win, (), (self.r_win,))
        with p.nc.allow_non_contiguous_dma(reason="small gate cols"):
            p.dma("pool", self.win[:, :, 1024:1072], re(d["w_in"][:, 1024:1072]), self.r_win, (), (self.r_win,))
        for nm in self.cst:
            p.dma("sp", self.cst[nm][:], d["c_" + nm], self.r_cst, (), (self.r_cst,))
        for i in range(2):
            p.op("dve", lambda g, i=i: g.memset(self.Qa[i][:], 0.0), (), (self.r_Qa[i],))
            with p.nc.allow_non_contiguous_dma(reason="small const cols"):
                p.dma("sp", self.Qa[i][:, :, 67:71], d["c_qaug"], self.r_Qa[i], (), (self.r_Qa[i],))
        p.dma("sp", self.KTc[:], d["KTc"].rearrange("g r n -> r g n"), self.r_kvc, (), (self.r_kvc,))
        p.dma("sp", self.Vc[:], d["Vc"].rearrange("g q c e -> q g c e"), self.r_kvc, (), (self.r_kvc,))
        p.dma("sp", self.kmall[:], d["kmall"], self.r_KM, (), (self.r_KM,))
        R = (self.r_KM,)
        for g_ in range(4):
            p.op("dve", lambda g, g_=g_: g.reduce_max(out=self.KMs[:, g_ * 4:g_ * 4 + 1], in_=self.kmall[:, g_, :], axis=AX.X), R, R)
        ACT(p, AF.Sqrt, self.KMs[:, 0::4], self.KMs[:, 0::4], R, R)
        p.op("dve", lambda g: g.tensor_scalar(out=self.KMs[:, 0::4], in0=self.KMs[:, 0::4], scalar1=1.02, scalar2=None,
                                              op0=ALU.mult), R, R)
        for j in range(1, 4):
            p.op("dve", lambda g, j=j: g.tensor_copy(out=self.KMs[:, j::4], in_=self.KMs[:, 0::4]), R, R)

    def prep4(self, s4, d):
        p, c = self.p, self.c
        n = min(4, self.NS - s4) * 128
        p.dma("sp", self.x4[:, :, :n], d["xT"][:, s4 * 128:s4 * 128 + n].rearrange("(k q) n -> q k n", q=128),
              self.r_x4, (), (self.r_x4,))
        rmsnorm_fm(p, c, self.x4, self.r_x4, self.wn, self.r_wn, self.h4, self.r_h4, n)

    def prep_slot(self, s):
        p, bk = self.p, self.bk
        sub = s % 4
        tok = slice(sub * 128, (sub + 1) * 128)
        Qa, r_Qa = self.Qa[s % 2], self.r_Qa[s % 2]
        QT, r_QT = self.QT[s % 2], self.r_QT[s % 2]
        gates, r_gates = self.gates[s % 2], self.r_gates[s % 2]
        sm, R = self.sm, (self.r_sm,)
        for half in range(2):
            pq, r_pq = self.ps_r[half], self.r_ps_r[half]
            for k in range(KD):
                MM(p, pq[:], self.h4[:, k, tok], self.win[:, k, half * 512:(half + 1) * 512], k == 0, k == KD - 1,
                   (self.r_h4, self.r_win), (r_pq,))
            ACT(p, AF.Copy, Qa[:, half * 8:(half + 1) * 8, 0:64], pq[:].rearrange("q (h d) -> q h d", d=64),
                (r_pq,), (r_Qa,), scale=QK_SCALE)
            ACT(p, AF.Square, self.sqt[:], pq[:], (r_pq,), (self.r_sqt,), scale=QK_SCALE)
            p.op("dve", lambda g, half=half: g.tensor_reduce(out=sm[:, 0, half * 8:(half + 1) * 8],
                                                             in_=self.sqt[:].rearrange("q (h d) -> q h d", d=64),
                                                             axis=AX.X, op=ALU.add), (self.r_sqt,), R)
        pg, r_pg = self.ps_r[0], self.r_ps_r[0]
        for k in range(KD):
            MM(p, pg[:, 0:48], self.h4[:, k, tok], self.win[:, k, 1024:1072], k == 0, k == KD - 1,
               (self.r_h4, self.r_win), (r_pg,))
        ACT(p, AF.Sigmoid, gates[:], pg[:, 0:48], (r_pg,), (r_gates,))
        ACT(p, AF.Sqrt, sm[:, 1, :], sm[:, 0, :], R, R)
        p.op("dve", lambda g: g.tensor_tensor(out=sm[:, 1, :], in0=sm[:, 1, :], in1=self.KMs[:], op=ALU.mult),
             (self.r_sm, self.r_KM), R)
        p.op("dve", lambda g: g.tensor_tensor(out=sm[:, 2, :], in0=self.cst["negst"][:, s, :], in1=sm[:, 1, :],
                                              op=ALU.subtract), (self.r_sm, self.r_cst), R)
        p.op("dve", lambda g: g.tensor_copy(out=Qa[:, :, 64], in_=sm[:, 2, :]), R, (r_Qa,))
        p.op("dve", lambda g: g.tensor_tensor(out=sm[:, 3, :], in0=sm[:, 2, :], in1=Qa[:, :, 64], op=ALU.subtract),
             (self.r_sm, r_Qa), R)
        p.op("dve", lambda g: g.tensor_copy(out=Qa[:, :, 65], in_=sm[:, 3, :]), R, (r_Qa,))
        p.op("dve", lambda g: g.tensor_tensor(out=sm[:, 4, :], in0=sm[:, 3, :], in1=Qa[:, :, 65], op=ALU.subtract),
             (self.r_sm, r_Qa), R)
        p.op("dve", lambda g: g.tensor_copy(out=Qa[:, :, 66], in_=sm[:, 4, :]), R, (r_Qa,))
        for g_ in range(4):
            hf = g_ % 2
            tb = bk.tb[0:KROWS, hf * 512:(hf + 1) * 512]
            for j in range(4):
                p.op("pe", lambda g, j=j: g.transpose(tb[:, j * 128:(j + 1) * 128], Qa[:, g_ * 4 + j, :],
                                                      self.cst["ident"][:]), (r_Qa, self.r_cst), (self.r_tbh[hf],))
            ACT(p, AF.Copy, QT[:, g_, :], tb, (self.r_tbh[hf],), (r_QT,))

    def _score_exp(self, KT_ap, r_k, QT_ap, r_q, neg=None, r_neg=()):
        p = self.p
        nb = len(self.sbanks)
        i = self.iss % nb
        self.iss += 1
        ps, r_ps = self.sbanks[i]
        MM(p, ps[:], KT_ap, QT_ap, True, True, (r_k, r_q), (r_ps,))
        ie = self.ie % self.NE
        self.ie += 1
        if neg is None:
            ACT(p, AF.Exp, self.e[ie][:], ps[:], (r_ps,), (self.r_e[ie],))
        else:
            zt, r_zt = self.zt[i % 2], self.r_zt[i % 2]
            p.op("dve", lambda g: g.tensor_tensor(out=zt[:].rearrange("k (j q) -> k j q", q=128),
                                                  in0=ps[:].rearrange("k (j q) -> k j q", q=128),
                                                  in1=neg.unsqueeze(1).to_broadcast([128, 4, 128]), op=ALU.add),
                 (r_ps,) + tuple(r_neg), (r_zt,))
            ACT(p, AF.Exp, self.e[ie][:], zt[:], (r_zt,), (self.r_e[ie],))
        return ie

    def _pv(self, po, r_po, pt, r_pt, V_ap, r_v, first, last):
        p = self.p
        p.op("pe", lambda g: g.matmul(self.poT[0:65, :], V_ap, pt[:], start=first, stop=last),
             (r_pt, r_v), (self.r_poT,))
        if last:
            ACT(p, AF.Copy, self.oTs[:], self.poT[0:65, :], (self.r_poT,), (self.r_oTs,))
            for j in range(4):
                p.op("pe", lambda g, j=j: g.transpose(po[:, j * 65:(j + 1) * 65], self.oTs[:, j * 128:(j + 1) * 128],
                                                      self.cst["ident32"][0:65, 0:65]),
                     (self.r_oTs, self.r_cst), (r_po,))

    def _mask_mul(self, ie, mask_ap, r_mask):
        p = self.p
        pp, r_pp = self.pp[ie], self.r_pp[ie]
        p.op("dve", lambda g: g.tensor_tensor(out=pp[:].rearrange("k (j q) -> k j q", q=128),
                                              in0=self.e[ie][:].rearrange("k (j q) -> k j q", q=128),
                                              in1=mask_ap.unsqueeze(1).to_broadcast([128, 4, 128]), op=ALU.mult),
             (self.r_e[ie],) + tuple(r_mask), (r_pp,))
        return pp, r_pp

    def _finish_branch(self, po, r_po, b, g_, gates, r_gates, first_branch):
        p = self.p
        R = (self.r_rd,)
        den = po[:, 64:260:65]
        p.op("dve", lambda g: g.tensor_scalar(out=self.rd[:, 0:4], in0=den, scalar1=1e-30, scalar2=None, op0=ALU.max),
             (r_po,), R)
        p.op("dve", lambda g: g.reciprocal(out=self.rd[:, 0:4], in_=self.rd[:, 0:4]), R, R)
        gv = gates[:, g_ * 12 + b:g_ * 12 + 12:3]
        p.op("dve", lambda g: g.tensor_tensor(out=self.rd[:, 4:8], in0=self.rd[:, 0:4], in1=gv, op=ALU.mult),
             (self.r_rd, r_gates), R)
        for j in range(4):
            h = g_ * 4 + j
            if first_branch:
                p.op("dve", lambda g, j=j, h=h: g.tensor_scalar(out=self.oacc[:, h, :], in0=po[:, j * 65:j * 65 + 64],
                                                                scalar1=self.rd[:, 4 + j:5 + j], scalar2=None, op0=ALU.mult),
                     (r_po, self.r_rd), (self.r_oacc,))
            else:
                p.op("dve", lambda g, j=j, h=h: g.scalar_tensor_tensor(
                    out=self.oacc[:, h, :], in0=po[:, j * 65:j * 65 + 64], scalar=self.rd[:, 4 + j:5 + j],
                    in1=self.oacc[:, h, :], op0=ALU.mult, op1=ALU.add), (r_po, self.r_rd, self.r_oacc), (self.r_oacc,))

    def _next_po(self):
        i = self.ipo % 2
        self.ipo += 1
        return self.ps_o[i], self.r_ps_o[i]

    def _run_chunks(self, chunks, qt, r_QT, po, r_po, after_pv=None):
        n = len(chunks)
        ies = [None] * n

        def S(i):
            ch = chunks[i]
            if ch.get("pre") is not None:
                ch["pre"]()
            neg = ch.get("neg")
            if neg is None:
                ies[i] = self._score_exp(ch["KT"], ch["r_k"], qt, r_QT)
            else:
                ies[i] = self._score_exp(ch["KT"], ch["r_k"], qt, r_QT, neg[0], neg[1])

        la = len(self.sbanks) - 1
        for i0 in range(min(la, n)):
            S(i0)
        for i in range(n):
            if i + la < n:
                S(i + la)
            ch = chunks[i]
            ie = ies[i]
            mul = ch.get("mul")
            if mul is not None:
                pt, r_pt = self._mask_mul(ie, mul[0], mul[1])
            else:
                pt, r_pt = self.e[ie], self.r_e[ie]
            self._pv(po, r_po, pt, r_pt, ch["V"], ch["r_v"], i == 0, i == n - 1)
            if after_pv is not None:
                after_pv(i, pt, r_pt)

    def _new_m2(self, iota, thr_col):
        p = self.p
        m2, r_m2 = self.m2[self.im2 % 3], self.r_m2[self.im2 % 3]
        self.im2 += 1
        p.op("dve", lambda g: g.tensor_scalar(out=m2[:], in0=iota, scalar1=thr_col, scalar2=-30000.0,
                                              op0=ALU.is_lt, op1=ALU.mult), (self.r_cst,), (r_m2,))
        return m2, r_m2

    def slot_group(self, s, g_, d):
        p, bk, c = self.p, self.bk, self.c
        NSB, NCc, PC = self.NSB, self.NCc, self.PC
        QT, r_QT = self.QT[s % 2], self.r_QT[s % 2]
        gates, r_gates = self.gates[s % 2], self.r_gates[s % 2]
        qt = QT[:, g_, :]
        cst, r_cst = self.cst, self.r_cst
        ncmp = min(NCc, (8 * s + 7) // 16 + 1)
        nfull = (8 * s - 17) // 16 + 1 if 8 * s >= 17 else 0
        po, r_po = self._next_po()
        chunks = []
        for jc in range(ncmp):
            ch = dict(KT=self.KTc[:, g_, jc * 128:(jc + 1) * 128], r_k=self.r_kvc, V=self.Vc[:, g_, jc, :], r_v=self.r_kvc)
            if jc >= nfull:
                m2, r_m2 = self._new_m2(cst["iota1"][:], cst["thr1"][:, s * NCc + jc:s * NCc + jc + 1])
                ch["neg"] = (m2[:], (r_m2,))
            chunks.append(ch)

        def imp_mm(jc, pt, r_pt):
            for j in range(4):
                pr, r_pr = self.ps_r[j // 2], self.r_ps_r[j // 2]
                p.op("pe", lambda g, j=j: g.matmul(pr[:, (j % 2) * 256:(j % 2) * 256 + NSB], pt[:, j * 128:(j + 1) * 128],
                                                   cst["apool"][:, jc, :], start=(jc == 0 and j % 2 == 0),
                                                   stop=(jc == ncmp - 1), skip_group_check=True),
                     (r_pt, r_cst), (r_pr,))

        self.sbanks = [(self.ps_s[0], self.r_ps_s[0]), (self.ps_s[1], self.r_ps_s[1])]
        self._run_chunks(chunks, qt, r_QT, po, r_po, after_pv=imp_mm)
        self._finish_branch(po, r_po, 0, g_, gates, r_gates, True)
        RS = (self.r_sc,)
        for j in range(4):
            pr, r_pr = self.ps_r[j // 2], self.r_ps_r[j // 2]
            src = pr[:, (j % 2) * 256:(j % 2) * 256 + NSB]
            if j == 0:
                p.op("dve", lambda g: g.tensor_scalar(out=self.sc[:], in0=src, scalar1=self.rd[:, 0:1], scalar2=None,
                                                      op0=ALU.mult), (r_pr, self.r_rd), RS)
            else:
                p.op("dve", lambda g, j=j: g.scalar_tensor_tensor(out=self.sc[:], in0=src, scalar=self.rd[:, j:j + 1],
                                                                  in1=self.sc[:], op0=ALU.mult, op1=ALU.add),
                     (r_pr, self.r_rd, self.r_sc), RS)
        p.op("dve", lambda g: g.tensor_tensor(out=self.sc[:], in0=self.sc[:], in1=cst["bonus"][:, s, :], op=ALU.add),
             (self.r_sc, r_cst), RS)
        p.op("dve", lambda g: g.max(out=self.m8[:, 0:8], in_=self.sc[:]), RS, RS)
        p.op("dve", lambda g: g.match_replace(out=self.sc2[:], in_to_replace=self.m8[:, 0:8], in_values=self.sc[:],
                                              imm_value=-3.0e38), RS, RS)
        p.op("dve", lambda g: g.max(out=self.m8[:, 8:16], in_=self.sc2[:]), RS, RS)
        p.op("dve", lambda g: g.tensor_scalar(out=self.m8[:, 15:16], in0=self.m8[:, 15:16], scalar1=-1.0e29, scalar2=None,
                                              op0=ALU.max), RS, RS)
        p.op("dve", lambda g: g.tensor_scalar(out=self.sel[:], in0=self.sc[:], scalar1=self.m8[:, 15:16], scalar2=None,
                                              op0=ALU.is_ge), RS, (self.r_sel,))
        nk = 8 * s + 8
        po, r_po = self._next_po()
        mbanks = ((bk.tb, bk.r_tb), (c.ps_n[:].bitcast(BF16), c.r_psn))
        chunks = []
        for kc in range(nk):
            cl = kc % PC
            pi = kc // PC
            ch = {}

            def pre(kc=kc, cl=cl, pi=pi, ch=ch):
                if cl == 0:
                    ncz = min(PC, nk - kc)
                    ib = self.ikv % 3
                    self.ikv += 1
                    self.cur_kv = (self.KTp[ib], self.Vp[ib], self.r_kvp[ib])
                    KTp, Vp, r_kv = self.cur_kv
                    p.dma("sp", KTp[:, 0:ncz * 128], d["KTs"][g_][:, kc * 128:(kc + ncz) * 128], r_kv, (), (r_kv,))
                    p.dma("sp", Vp[:, 0:ncz, :], d["Vs"][g_][:, kc:kc + ncz, :], r_kv, (), (r_kv,))
                KTp, Vp, r_kv = self.cur_kv
                ch["KT"], ch["r_k"], ch["V"], ch["r_v"] = KTp[:, cl * 128:(cl + 1) * 128], r_kv, Vp[:, cl, :], r_kv
                if kc % 8 == 0:
                    mb, r_mb = mbanks[(kc // 8) % 2]
                    nm_ = min(8, nk - kc)
                    sx, r_sx = self.selx[(kc // 8) % 2], self.r_selx[(kc // 8) % 2]
                    p.op("pool", lambda g: g.tensor_copy(
                        out=sx[:, 0:nm_ * 128].rearrange("q (b k) -> q b k", k=64),
                        in_=self.sel[:, 2 * kc:2 * kc + 2 * nm_].unsqueeze(2).to_broadcast([128, 2 * nm_, 64])),
                        (self.r_sel,), (r_sx,))
                    for m in range(nm_):
                        p.op("pe", lambda g, m=m: g.transpose(mb[:, m * 128:(m + 1) * 128], sx[:, m * 128:(m + 1) * 128],
                                                              cst["ident"][:]), (r_sx, r_cst), (r_mb,))
                mb, r_mb = mbanks[(kc // 8) % 2]
                ch["mul"] = (mb[:, (kc % 8) * 128:(kc % 8 + 1) * 128], (r_mb,))
                if kc >= 8 * s:
                    r = kc - 8 * s
                    m2, r_m2 = self._new_m2(cst["iota2"][:], cst["thr2"][:, r:r + 1])
                    ch["neg"] = (m2[:], (r_m2,))

            ch["pre"] = pre
            chunks.append(ch)
        self.sbanks = [(self.ps_s[0], self.r_ps_s[0]), (self.ps_s[1], self.r_ps_s[1]),
                       (self.ps_r[0], self.r_ps_r[0]), (self.ps_r[1], self.r_ps_r[1])]
        self._run_chunks(chunks, qt, r_QT, po, r_po)
        self._finish_branch(po, r_po, 1, g_, gates, r_gates, False)
        iw = self.iw % 2
        self.iw += 1
        KTw, Vw, r_kw = self.KTw[iw], self.Vw[iw], self.r_kvw[iw]
        p.dma("sp", KTw[:], d["KTw"][g_][:, s * 640:(s + 1) * 640], r_kw, (), (r_kw,))
        p.dma("sp", Vw[:], d["Vw"][g_][:, s * 5:(s + 1) * 5, :], r_kw, (), (r_kw,))
        po, r_po = self._next_po()
        chunks = []
        for r in range(5):
            ch = dict(KT=KTw[:, r * 128:(r + 1) * 128], r_k=r_kw, V=Vw[:, r, :], r_v=r_kw)
            if r == 0:
                ch["neg"] = (cst["negU"][:], (r_cst,))
            elif r == 4:
                ch["neg"] = (cst["negL"][:], (r_cst,))
            chunks.append(ch)
        self._run_chunks(chunks, qt, r_QT, po, r_po)
        self._finish_branch(po, r_po, 2, g_, gates, r_gates, False)

    def run(self, d):
        p = self.p
        self.setup(d)
        for s in range(self.NS):
            if s % 4 == 0:
                self.prep4(s, d)
            self.prep_slot(s)
            for g_ in range(4):
                self.slot_group(s, g_, d)
            ob, r_ob = self.obf[s % 2], self.r_obf[s % 2]
            ACT(p, AF.Copy, ob[:], self.oacc[:].rearrange("q h d -> q (h d)"), (self.r_oacc,), (r_ob,))
            ot, r_ot = self.obT[s % 2], self.r_obT[s % 2]
            for hf in range(2):
                for kk in range(4):
                    k8 = hf * 4 + kk
                    p.op("pe", lambda g, k8=k8, kk=kk, hf=hf: g.transpose(
                        self.bk.tb[:, hf * 512 + kk * 128:hf * 512 + (kk + 1) * 128], ob[:, k8 * 128:(k8 + 1) * 128],
                        self.cst["ident"][:]), (r_ob, self.r_cst), (self.r_tbh[hf],))
                ACT(p, AF.Copy, ot[:, hf * 4:(hf + 1) * 4, :],
                    self.bk.tb[:, hf * 512:(hf + 1) * 512].rearrange("f (k q) -> f k q", q=128), (self.r_tbh[hf],), (r_ot,))
            with p.nc.allow_non_contiguous_dma(reason="256B runs"):
                p.dma("sp", d["oT"][:, s * 128:(s + 1) * 128].rearrange("(k f) n -> f k n", f=128), ot[:], r_ot,
                      (r_ot,), (), is_output=True)


class OPPhase:
    def __init__(self, p, c, ws, bk):
        self.p, self.c, self.ws, self.bk = p, c, ws, bk
        self.xs = [p.sb("opx%d" % i, [128, KD, TT], F32) for i in range(2)]
        self.r_xs = [p.res("opx%d" % i) for i in range(2)]
        self.on = [p.sb("opo%d" % i, [128, KD, TT], BF16) for i in range(2)]
        self.r_on = [p.res("opo%d" % i) for i in range(2)]

    def run(self, x_in, x_out, oT, w_out, ntiles):
        p, bk = self.p, self.bk
        for t in range(ntiles):
            for blk in range(2):
                self.ws.plan_cols(w_out, blk * 512)
        for t in range(ntiles):
            T0 = t * TT
            xs, r_xs = self.xs[t % 2], self.r_xs[t % 2]
            on, r_on = self.on[t % 2], self.r_on[t % 2]
            p.dma("sp", xs[:], x_in[:, T0:T0 + TT].rearrange("(k q) n -> q k n", q=128), r_xs, (), (r_xs,))
            p.dma("sp", on[:], oT[:, T0:T0 + TT].rearrange("(k q) n -> q k n", q=128), r_on, (), (r_on,))
            for blk in range(2):
                w, r_w = self.ws.next()
                for dt_ in range(4):
                    po, r_po = bk.b[dt_ % 2], bk.r[dt_ % 2]
                    for k in range(KD):
                        MM(p, po[:], w[:, k, dt_ * 128:(dt_ + 1) * 128], on[:, k, :], k == 0, k == KD - 1,
                           (r_w, r_on), (r_po,))
                    kk = blk * 4 + dt_
                    p.op("dve", lambda g: g.tensor_tensor(out=xs[:, kk, :], in0=xs[:, kk, :], in1=po[:], op=ALU.add),
                         (r_xs, r_po), (r_xs,))
            p.dma("sp", x_out[:, T0:T0 + TT].rearrange("(k q) n -> q k n", q=128), xs[:], r_xs, (r_xs,), (),
                  is_output=True)


from contextlib import ExitStack
import ml_dtypes

NPBF = ml_dtypes.bfloat16
NCORES = 8


class _IO:
    def __init__(self, nc):
        self.nc = nc

    def i(self, n, s, d=F32):
        return self.nc.dram_tensor(n, list(s), d, kind="ExternalInput").ap()

    def o(self, n, s, d=F32):
        return self.nc.dram_tensor(n, list(s), d, kind="ExternalOutput").ap()

    def t(self, n, s, d=F32):
        return self.nc.dram_tensor(n, list(s), d, kind="Internal").ap()


def _hg1_io(io, NT, sfx, out=True):
    mk = io.o if out else io.i
    return dict(o_loc=mk("o_loc" + sfx, [D, NT]), qdec=mk("qdec" + sfx, [D, NT], BF16), sg=mk("sg" + sfx, [D, NT], BF16),
                s_fin=mk("s_fin" + sfx, [128, 8, 128]) if out else None, dec=mk("dec" + sfx, [128, 8]) if out else None)


def _phase_hg1(p, c, io, x_ap, layer, NT, outs, cd_in):
    p.begin_phase()
    ws = WStream(p)
    bk = Banks(p)
    cd = load_consts_hg(p, cd_in["reset"], cd_in["tri"], cd_in["ident"])
    g1 = HG1(p, c, ws, bk, cd)
    g1.setup(cd_in["a_norm_w"], cd_in["alb"], layer)
    g1.plan_weights(cd_in["a_w_in"], NT // TT)
    for t in range(NT // TT):
        g1.tile(t, x_ap, outs["o_loc"], outs["qdec"], outs["sg"])
    g1.finish(outs["s_fin"], outs["dec"])
    p.end_phase()


def _phase_hg2(p, c, x_in, x_out, ins, NT):
    p.begin_phase()
    ws = WStream(p)
    bk = Banks(p)
    g2 = HG2(p, c, ws, bk)
    g2.prefix(ins["S_all"], ins["dec_all"], ins["selv"], ins["gnorm_w"])
    g2.plan_weights(ins["a_w_out"], NT // TT)
    for t in range(NT // TT):
        g2.tile(t, x_in, x_out, ins["o_loc"], ins["qdec"], ins["sg"])
    p.end_phase()


def _phase_mlp(p, c, x_in, x_out, wn, wu, wd, NT, final_w=None):
    p.begin_phase()
    m = MLP(p, c)
    m.run(x_in, x_out, NT // TT, wn, wu, wd, final_w=final_w)
    p.end_phase()


def build_L1(NT):
    nc = bass.Bass("TRN2", target_bir_lowering=False)
    io = _IO(nc)
    x = io.i("xT", [D, NT])
    cd_in = dict(reset=io.i("c_reset", [128, TT]), tri=io.i("c_tri", [64, 64], BF16), ident=io.i("c_ident", [128, 128], BF16),
                 a_norm_w=io.i("a_norm_w", [D]), alb=io.i("alb", [2, D]), a_w_in=io.i("a_w_in", [D, 4096]))
    outs = _hg1_io(io, NT, "")
    with ExitStack() as st:
        p = Prog(nc, st)
        c = Common(p)
        _phase_hg1(p, c, io, x, 0, NT, outs, cd_in)
        p.finish()
        nc._ninstr = p.ninstr
    return nc


def build_L2(NT):
    nc = bass.Bass("TRN2", target_bir_lowering=False)
    io = _IO(nc)
    x = io.i("xT", [D, NT])
    ins = dict(S_all=io.i("S_all", [8, 128, 8, 128]), dec_all=io.i("dec_all", [8, 128, 8]), selv=io.i("selv", [128, 8]),
               gnorm_w=io.i("gnorm_w", [128]), a_w_out=io.i("a_w_out", [D, D]),
               o_loc=io.i("o_loc_in", [D, NT]), qdec=io.i("qdec_in", [D, NT], BF16), sg=io.i("sg_in", [D, NT], BF16))
    wn, wu, wd = io.i("mlp_norm_w", [D]), io.i("mlp_w_up", [D, DFF]), io.i("mlp_w_down", [DFF, D])
    cd_in = dict(reset=io.i("c_reset", [128, TT]), tri=io.i("c_tri", [64, 64], BF16), ident=io.i("c_ident", [128, 128], BF16),
                 a_norm_w=io.i("a_norm_w", [D]), alb=io.i("alb", [2, D]), a_w_in=io.i("a_w_in", [D, 4096]))
    x1 = io.t("x1", [D, NT])
    x2 = io.o("x2", [D, NT])
    outs = _hg1_io(io, NT, "")
    with ExitStack() as st:
        p = Prog(nc, st)
        c = Common(p)
        _phase_hg2(p, c, x, x1, ins, NT)
        _phase_mlp(p, c, x1, x2, wn, wu, wd, NT)
        _phase_hg1(p, c, io, x2, 1, NT, outs, cd_in)
        p.finish()
        nc._ninstr = p.ninstr
    return nc


def build_L3(NT):
    nc = bass.Bass("TRN2", target_bir_lowering=False)
    io = _IO(nc)
    x = io.i("xT", [D, NT])
    ins = dict(S_all=io.i("S_all", [8, 128, 8, 128]), dec_all=io.i("dec_all", [8, 128, 8]), selv=io.i("selv", [128, 8]),
               gnorm_w=io.i("gnorm_w", [128]), a_w_out=io.i("a_w_out", [D, D]),
               o_loc=io.i("o_loc_in", [D, NT]), qdec=io.i("qdec_in", [D, NT], BF16), sg=io.i("sg_in", [D, NT], BF16))
    wn, wu, wd = io.i("mlp_norm_w", [D]), io.i("mlp_w_up", [D, DFF]), io.i("mlp_w_down", [DFF, D])
    kvn, kvw = io.i("kv_norm_w", [D]), io.i("kv_w", [D, 1536])
    x1 = io.t("x1", [D, NT])
    x2 = io.o("x2", [D, NT])
    kT01, kT24 = io.o("kT01", [512, NT], BF16), io.o("kT24", [512, NT], BF16)
    vaug = io.o("vaug", [NT // 128, 128, 8, 65], BF16)
    kmx = io.o("kmx", [128, 4])
    with ExitStack() as st:
        p = Prog(nc, st)
        c = Common(p)
        _phase_hg2(p, c, x, x1, ins, NT)
        _phase_mlp(p, c, x1, x2, wn, wu, wd, NT)
        p.begin_phase()
        ws = WStream(p)
        bk = Banks(p)
        kv = KVPhase(p, c, ws, bk)
        kv.setup(kvn)
        kv.plan_weights(kvw, NT // TT)
        for t in range(NT // TT):
            kv.tile(t, x2, kT01, kT24, vaug)
        kv.finish(kmx)
        p.end_phase()
        p.finish()
        nc._ninstr = p.ninstr
    return nc


def build_L4(NB):
    nc = bass.Bass("TRN2", target_bir_lowering=False)
    io = _IO(nc)
    kin, vin = io.i("kin", [4, 64, 16 * NB + 16], BF16), io.i("vin", [4, 64, 16 * NB + 16], BF16)
    pk, w1k, w2k = io.i("cmp_pe_k", [32, 64]), io.i("cmp_w1_k", [2048, 256]), io.i("cmp_w2_k", [256, 64])
    pv, w1v, w2v = io.i("cmp_pe_v", [32, 64]), io.i("cmp_w1_v", [2048, 256]), io.i("cmp_w2_v", [256, 64])
    kc, vc, kmxc = io.o("kc", [4, 64, NB], BF16), io.o("vc", [NB, 4, 65], BF16), io.o("kmxc", [64, 4])
    with ExitStack() as st:
        p = Prog(nc, st)
        c = Common(p)
        p.begin_phase()
        bk = Banks(p)
        cm = CMPPhase(p, c, bk, NB)
        cm.run(kin, vin, pk, w1k, w2k, pv, w1v, w2v, kc, vc, kmxc)
        p.end_phase()
        p.finish()
        nc._ninstr = p.ninstr
    return nc


def build_L5(NT, T, nlayers=2):
    nc = bass.Bass("TRN2", target_bir_lowering=False)
    io = _IO(nc)
    NS = NT // 128
    NSB = T // 64
    NCc = max(1, (T // 16) // 128)
    x = io.i("xT", [D, NT])
    shared = dict(KTs=io.i("KTs", [4, KROWS, T], BF16), Vs=io.i("Vs", [4, 128, T // 128, 65], BF16),
                  KTw=io.i("KTw", [4, KROWS, NS * 640], BF16), Vw=io.i("Vw", [4, 128, NS * 5, 65], BF16),
                  KTc=io.i("KTc", [4, KROWS, NCc * 128], BF16), Vc=io.i("Vc", [4, 128, NCc, 65], BF16),
                  kmall=io.i("kmall", [128, 4, 24]),
                  c_ident=io.i("c_ident", [128, 128], BF16), c_ident32=io.i("c_ident32", [128, 128]), c_iota1=io.i("c_iota1", [128, 128]),
                  c_iota2=io.i("c_iota2", [128, 128]), c_negL=io.i("c_negL", [128, 128]), c_negU=io.i("c_negU", [128, 128]),
                  c_apool=io.i("c_apool", [128, NCc, NSB], BF16), c_thr1=io.i("c_thr1", [128, NS * NCc]),
                  c_thr2=io.i("c_thr2", [128, 8]), c_negst=io.i("c_negst", [128, NS, 16]),
                  c_bonus=io.i("c_bonus", [128, NS, NSB]), c_qaug=io.i("c_qaug", [128, 16, 4], BF16))
    lw = []
    for b in range(nlayers):
        lw.append(dict(norm_w=io.i("b_norm_w%d" % b, [D]), w_in=io.i("b_w_in%d" % b, [D, 1072]),
                       w_out=io.i("b_w_out%d" % b, [D, D]), wn=io.i("mlp_norm_w%d" % b, [D]),
                       wu=io.i("mlp_w_up%d" % b, [D, DFF]), wd=io.i("mlp_w_down%d" % b, [DFF, D])))
    fw = io.i("final_norm_w", [D])
    out = io.o("outT", [D, NT])
    with ExitStack() as st:
        p = Prog(nc, st)
        c = Common(p)
        xcur = x
        for b in range(nlayers):
            oT = io.t("oT%d" % b, [D, NT], BF16)
            xa = io.t("xa%d" % b, [D, NT])
            xb = out if b == nlayers - 1 else io.t("xb%d" % b, [D, NT])
            p.begin_phase()
            bk = Banks(p)
            at = ATTPhase(p, c, bk, NS, T)
            d = dict(shared)
            d.update(xT=xcur, norm_w=lw[b]["norm_w"], w_in=lw[b]["w_in"], oT=oT)
            at.run(d)
            p.end_phase()
            p.begin_phase()
            ws = WStream(p)
            bk = Banks(p)
            op = OPPhase(p, c, ws, bk)
            op.run(xcur, xa, oT, lw[b]["w_out"], NT // TT)
            p.end_phase()
            _phase_mlp(p, c, xa, xb, lw[b]["wn"], lw[b]["wu"], lw[b]["wd"], NT,
                       final_w=fw if b == nlayers - 1 else None)
            xcur = xb
        p.finish()
        nc._ninstr = p.ninstr
    return nc


def _bf16_split(v):
    hi = np.asarray(v, np.float64).astype(NPBF)
    lo = (np.asarray(v, np.float64) - hi.astype(np.float64)).astype(NPBF)
    return hi, lo


def _aug_rows(u):
    u = np.asarray(u, np.int64)
    a = (u // 128).astype(np.float32)
    b = (u % 128).astype(np.float32)
    one = np.ones_like(a)
    return np.stack([one, one, one, a, a, b, b], 0).astype(NPBF)


def att_consts(core, NS, T):
    NSB = T // 64
    NCc = max(1, (T // 16) // 128)
    pp = np.arange(128)[:, None]
    qq = np.arange(128)[None, :]
    cst = {}
    cst["c_ident"] = np.eye(128).astype(NPBF)
    cst["c_ident32"] = np.eye(128).astype(np.float32)
    cst["c_iota1"] = (qq - 16 * pp).astype(np.float32)
    cst["c_iota2"] = (qq - pp).astype(np.float32) * np.ones((128, 128), np.float32)
    cst["c_negL"] = np.where(pp > qq, -30000.0, 0.0).astype(np.float32)
    cst["c_negU"] = np.where(qq >= pp, -30000.0, 0.0).astype(np.float32)
    cg = (np.arange(NCc)[None, :, None] * 128 + np.arange(128)[:, None, None])
    nn = np.arange(NSB)[None, None, :]
    cst["c_apool"] = ((cg >= 4 * nn - 1) & (cg <= 4 * nn + 3)).astype(NPBF)
    qb = 8 * np.arange(NS) + core
    thr1 = (2048 * np.arange(NCc)[None, :] + 31 - 128 * qb[:, None]).reshape(-1).astype(np.float32)
    cst["c_thr1"] = np.broadcast_to(thr1[None, :], (128, NS * NCc)).copy()
    thr2 = (128 * (np.arange(8) - core)).astype(np.float32)
    cst["c_thr2"] = np.broadcast_to(thr2[None, :], (128, 8)).copy()
    bhi, blo = _bf16_split(NSA_SLOPES)
    seff = bhi.astype(np.float64) + blo.astype(np.float64)
    tq = (128 * qb[None, :] + np.arange(128)[:, None]).astype(np.float64)
    cst["c_negst"] = (-tq[:, :, None] * seff[None, None, :]).astype(np.float32)
    cur = (tq // 64).astype(np.int64)[:, :, None]
    nb = np.arange(NSB)[None, None, :]
    forced = (nb == 0) | (nb == cur) | (nb == cur - 1)
    cst["c_bonus"] = np.where(nb <= cur, 1.0e4 * forced, -1.0e30).astype(np.float32)
    qa = np.stack([(128.0 * bhi.astype(np.float64)).astype(NPBF), (128.0 * blo.astype(np.float64)).astype(NPBF), bhi, blo], -1)
    cst["c_qaug"] = np.broadcast_to(qa[None], (128, 16, 4)).copy()
    return cst


_PROGS = {}


def _prog(key, fn):
    if key not in _PROGS:
        _PROGS[key] = fn()
    return _PROGS[key]


def _run(nc, in_maps):
    return run_bass_kernel_spmd(nc, in_maps, core_ids=list(range(NCORES))).results


def kernel(x, a_norm_w, a_w_in, a_gnorm_w, a_w_out, a_lower_bounds, kv_norm_w, kv_w,
           cmp_pe_k, cmp_w1_k, cmp_w2_k, cmp_pe_v, cmp_w1_v, cmp_w2_v,
           b_norm_w, b_w_in, b_w_out, mlp_norm_w, mlp_w_up, mlp_w_down, final_norm_w, _debug=None):
    f32 = lambda a: np.ascontiguousarray(np.asarray(a, dtype=np.float32))
    x = f32(x)
    T = x.shape[1]
    NT = T // NCORES
    NS = NT // 128
    xs = x[0]
    hc = host_consts_hg()
    a_norm_w, a_w_in, a_gnorm_w, a_w_out, alb = map(f32, (a_norm_w, a_w_in, a_gnorm_w, a_w_out, a_lower_bounds))
    mlp_norm_w, mlp_w_up, mlp_w_down = map(f32, (mlp_norm_w, mlp_w_up, mlp_w_down))
    b_norm_w, b_w_in, b_w_out = map(f32, (b_norm_w, b_w_in, b_w_out))
    xT = [np.ascontiguousarray(xs[c * NT:(c + 1) * NT].T) for c in range(NCORES)]

    def selv(c):
        s = np.zeros((128, 8), np.float32)
        s[:, :c] = 1.0
        return s

    r1 = _run(_prog(("L1", NT), lambda: build_L1(NT)),
              [dict(xT=xT[c], a_norm_w=a_norm_w[0], alb=alb, a_w_in=a_w_in[0], **hc) for c in range(NCORES)])
    S_all = np.stack([r["s_fin"] for r in r1])
    dec_all = np.stack([r["dec"] for r in r1])
    r2 = _run(_prog(("L2", NT), lambda: build_L2(NT)),
              [dict(xT=xT[c], S_all=S_all, dec_all=dec_all, selv=selv(c), gnorm_w=a_gnorm_w[0], a_w_out=a_w_out[0],
                    o_loc_in=r1[c]["o_loc"], qdec_in=r1[c]["qdec"], sg_in=r1[c]["sg"],
                    mlp_norm_w=mlp_norm_w[0], mlp_w_up=mlp_w_up[0], mlp_w_down=mlp_w_down[0],
                    a_norm_w=a_norm_w[1], alb=alb, a_w_in=a_w_in[1], **hc) for c in range(NCORES)])
    S_all = np.stack([r["s_fin"] for r in r2])
    dec_all = np.stack([r["dec"] for r in r2])
    r3 = _run(_prog(("L3", NT), lambda: build_L3(NT)),
              [dict(xT=r2[c]["x2"], S_all=S_all, dec_all=dec_all, selv=selv(c), gnorm_w=a_gnorm_w[1], a_w_out=a_w_out[1],
                    o_loc_in=r2[c]["o_loc"], qdec_in=r2[c]["qdec"], sg_in=r2[c]["sg"],
                    mlp_norm_w=mlp_norm_w[1], mlp_w_up=mlp_w_up[1], mlp_w_down=mlp_w_down[1],
                    kv_norm_w=f32(kv_norm_w), kv_w=f32(kv_w)) for c in range(NCORES)])
    if _debug is not None:
        _debug["x_l1"] = np.concatenate([r["x2"].T for r in r3], 0)
    kT01 = np.concatenate([r["kT01"] for r in r3], 1)
    kT24 = np.concatenate([r["kT24"] for r in r3], 1)
    vaug = np.concatenate([r["vaug"] for r in r3], 0)
    n_cmp = T // 16
    NB = n_cmp // NCORES
    pad = np.zeros((512, 16), NPBF)
    kpad = np.concatenate([kT01, pad], 1)
    in4 = []
    for c in range(NCORES):
        sl = kpad[:, 16 * NB * c:16 * NB * (c + 1) + 16]
        in4.append(dict(kin=np.ascontiguousarray(sl[0:256].reshape(4, 64, -1)),
                        vin=np.ascontiguousarray(sl[256:512].reshape(4, 64, -1)),
                        cmp_pe_k=f32(cmp_pe_k), cmp_w1_k=f32(cmp_w1_k), cmp_w2_k=f32(cmp_w2_k),
                        cmp_pe_v=f32(cmp_pe_v), cmp_w1_v=f32(cmp_w1_v), cmp_w2_v=f32(cmp_w2_v)))
    r4 = _run(_prog(("L4", NB), lambda: build_L4(NB)), in4)
    NCc = max(1, n_cmp // 128)
    aug_tok = _aug_rows(np.arange(T))
    zpad = np.zeros((KROWS - 64 - NAUG, T), NPBF)
    KTs = np.stack([np.concatenate([kT24[g * 64:(g + 1) * 64], aug_tok, zpad], 0) for g in range(4)])
    KTwin_full = np.stack([np.concatenate([kT24[256 + g * 64:256 + (g + 1) * 64], aug_tok, zpad], 0) for g in range(4)])
    Vs = np.ascontiguousarray(np.transpose(vaug[:, :, 0:4, :], (2, 1, 0, 3)))
    Vwin_full = np.transpose(vaug[:, :, 4:8, :], (2, 1, 0, 3))
    kc_all = np.concatenate([r["kc"] for r in r4], 2)
    ncp = NCc * 128
    KTc = np.zeros((4, KROWS, ncp), NPBF)
    KTc[:, 0:64, :n_cmp] = kc_all
    KTc[:, 64:64 + NAUG, :n_cmp] = _aug_rows(16 * np.arange(n_cmp) + 31)[None]
    vc_all = np.concatenate([r["vc"] for r in r4], 0)
    vcp = np.zeros((ncp, 4, 65), NPBF)
    vcp[:n_cmp] = vc_all
    Vc = np.ascontiguousarray(np.transpose(vcp.reshape(NCc, 128, 4, 65), (2, 1, 0, 3)))
    km = np.zeros((4, 24), np.float32)
    for g in range(4):
        row = (g % 2) * 64
        vals = [r3[c]["kmx"][row, g // 2] for c in range(NCORES)] + [r3[c]["kmx"][row, 2 + g // 2] for c in range(NCORES)] \
            + [r4[c]["kmxc"][0, g] for c in range(NCORES)]
        km[g] = np.asarray(vals, np.float32)
    kmall = np.broadcast_to(km[None], (128, 4, 24)).copy()
    x_l1 = np.concatenate([r["x2"] for r in r3], 1)
    in5 = []
    for c in range(NCORES):
        qbs = [8 * s + c for s in range(NS)]
        xc = np.concatenate([x_l1[:, 128 * qb:128 * qb + 128] for qb in qbs], 1)
        KTw = np.zeros((4, KROWS, NS * 640), NPBF)
        Vw = np.zeros((4, 128, NS * 5, 65), NPBF)
        for s, qb in enumerate(qbs):
            for r in range(5):
                ch = qb - 4 + r
                if ch >= 0:
                    KTw[:, :, s * 640 + r * 128:s * 640 + (r + 1) * 128] = KTwin_full[:, :, ch * 128:(ch + 1) * 128]
                    Vw[:, :, s * 5 + r, :] = Vwin_full[:, :, ch, :]
        dd = dict(xT=np.ascontiguousarray(xc), KTs=KTs, Vs=Vs, KTw=KTw, Vw=Vw, KTc=KTc, Vc=Vc, kmall=kmall,
                  final_norm_w=f32(final_norm_w))
        dd.update(att_consts(c, NS, T))
        for b in range(2):
            dd.update({"b_norm_w%d" % b: b_norm_w[b], "b_w_in%d" % b: b_w_in[b], "b_w_out%d" % b: b_w_out[b],
                       "mlp_norm_w%d" % b: mlp_norm_w[2 + b], "mlp_w_up%d" % b: mlp_w_up[2 + b],
                       "mlp_w_down%d" % b: mlp_w_down[2 + b]})
        in5.append(dd)
    r5 = _run(_prog(("L5", NT, T), lambda: build_L5(NT, T)), in5)
    out = np.zeros((T, D), np.float32)
    for c in range(NCORES):
        oc = r5[c]["outT"]
        for s in range(NS):
            qb = 8 * s + c
            out[128 * qb:128 * qb + 128] = oc[:, s * 128:(s + 1) * 128].T
    return out[None]
```

```python
import numpy as np
import concourse.bass as bass
import concourse.mybir as mybir
from concourse.bass_utils import run_bass_kernel_spmd

F32 = mybir.dt.float32
BF16 = mybir.dt.bfloat16
AF = mybir.ActivationFunctionType
ALU = mybir.AluOpType
AX = mybir.AxisListType


class Res:
    __slots__ = ("name", "lw", "rd", "dsem", "dcnt", "dq")

    def __init__(self, name):
        self.name = name
        self.lw = None
        self.rd = []
        self.dsem = None
        self.dq = None
        self.dcnt = 0


class Prog:
    def __init__(self, nc, stack):
        self.nc = nc
        self.stack = stack
        self.gstack = stack
        self.free_sems = []
        self.phase_res = []
        self.eng = {"pe": nc.tensor, "act": nc.scalar, "dve": nc.vector,
                    "pool": nc.gpsimd, "sp": nc.sync}
        self.sem = {}
        self.cnt = {}
        for k in ("pe", "act", "dve", "pool"):
            self.sem[k] = stack.enter_context(nc.semaphore("s_" + k))
            self.cnt[k] = 0
        self.waited = {k: {} for k in self.eng}
        self.nres = 0
        self.out_tokens = []
        self.ninstr = 0

    def sb(self, name, shape, dt):
        self.nalloc = getattr(self, "nalloc", 0) + 1
        t = self.stack.enter_context(self.nc.sbuf_tensor("s%d_%s" % (self.nalloc, name), list(shape), dt))
        return t

    def ps(self, name, shape, dt=F32):
        self.nalloc = getattr(self, "nalloc", 0) + 1
        t = self.stack.enter_context(self.nc.psum_tensor("p%d_%s" % (self.nalloc, name), list(shape), dt))
        return t

    def res(self, name=None):
        self.nres += 1
        r = Res(name or ("r%d" % self.nres))
        self.phase_res.append(r)
        return r

    def barrier(self):
        toks = [(k, self.cnt[k]) for k in ("pe", "act", "dve", "pool") if self.cnt[k] > 0]
        for r in self.phase_res:
            if r.dsem is not None:
                toks.append((r.dsem, r.dcnt))
        for e in ("sp", "pe", "act", "dve", "pool"):
            self._emit_waits_all(e, toks)

    def _emit_waits_all(self, e, toks):
        wd = self.waited[e]
        for (k, v) in toks:
            if wd.get(k, 0) >= v:
                continue
            self.eng[e].wait_ge(self.sem[k], v)
            wd[k] = v
            self.ninstr += 1

    def begin_phase(self):
        from contextlib import ExitStack as _ES
        self.stack = _ES()
        self.phase_res = []
        return self.stack

    def end_phase(self):
        self.barrier()
        for r in self.phase_res:
            if r.dsem is not None:
                self.free_sems.append((r.dsem, r.dcnt, r.dq))
                r.dsem = None
        self.phase_res = []
        self.stack.close()
        self.stack = self.gstack

    def _deps(self, reads, writes):
        toks = []
        for r in reads:
            if r.lw is not None:
                toks.append(r.lw)
        for w in writes:
            if w.lw is not None:
                toks.append(w.lw)
            toks.extend(w.rd)
        return toks

    def _emit_waits(self, e, toks):
        wd = self.waited[e]
        need = {}
        for (k, v) in toks:
            if e == "pe" and k == "pe":
                continue
            if k == e and v <= self.cnt[e] - 2:
                continue
            if wd.get(k, 0) >= v:
                continue
            if need.get(k, 0) < v:
                need[k] = v
        for k, v in need.items():
            self.eng[e].wait_ge(self.sem[k], v)
            wd[k] = v
            self.ninstr += 1

    def _commit(self, tok, reads, writes):
        for r in reads:
            r.rd.append(tok)
        for w in writes:
            w.lw = tok
            w.rd = []

    def op(self, e, fn, reads=(), writes=()):
        self._emit_waits(e, self._deps(reads, writes))
        ins = fn(self.eng[e])
        self.cnt[e] += 1
        ins.then_inc(self.sem[e], 1)
        self.ninstr += 1
        tok = (e, self.cnt[e])
        self._commit(tok, reads, writes)
        return tok

    def dma(self, q, out, in_, sres, reads=(), writes=(), is_output=False, **kw):
        self._emit_waits(q, self._deps(reads, writes))
        qt = "sw" if q == "pool" else "hw"
        if sres.dsem is not None:
            assert sres.dq == qt, "mixed SW/HW DMA on one semaphore: " + sres.name
        if sres.dsem is None:
            sres.dq = qt
            fl = [i for i, f in enumerate(self.free_sems) if f[2] == qt]
            if fl:
                key, cnt0, _ = self.free_sems.pop(fl[0])
                sres.dsem = key
                sres.dcnt = cnt0
            else:
                key = "d%d" % self.nres
                self.nres += 1
                self.sem[key] = self.gstack.enter_context(self.nc.semaphore(key))
                sres.dsem = key
        ins = self.eng[q].dma_start(out=out, in_=in_, **kw)
        sres.dcnt += 16
        ins.then_inc(self.sem[sres.dsem], 16)
        self.ninstr += 1
        tok = (sres.dsem, sres.dcnt)
        self._commit(tok, reads, writes)
        if is_output:
            self.out_tokens.append(tok)
        return tok

    def finish(self):
        self._emit_waits("sp", self.out_tokens)


D = 1024
KD = D // 128
DFF = 4096
EPS = 1e-6
TT = 512


def ACT(p, func, out, in_, reads, writes, **kw):
    return p.op("act", lambda g: g.activation(out=out, in_=in_, func=func, **kw), reads, writes)


def MM(p, out, lhsT, rhs, start, stop, reads, writes):
    return p.op("pe", lambda g: g.matmul(out, lhsT, rhs, start=start, stop=stop), reads, writes)


class Common:
    def __init__(self, p):
        self.p = p
        self.ones_bf = p.sb("ones_bf", [128, 128], BF16)
        self.r_ones = p.res("ones")
        p.op("dve", lambda g: g.memset(self.ones_bf[:], 1.0), (), (self.r_ones,))
        self.eps = p.sb("eps_col", [128, 1], F32)
        self.r_eps = p.res("eps")
        p.op("dve", lambda g: g.memset(self.eps[:], EPS), (), (self.r_eps,))
        self.sq = p.sb("sq", [128, KD, TT], BF16)
        self.r_sq = p.res("sq")
        self.rstd = p.sb("rstd", [128, TT], F32)
        self.r_rstd = p.res("rstd")
        self.ps_n = p.ps("ps_n", [128, TT])
        self.r_psn = p.res("psn")


def load_cols(p, q, dst, r_dst, src_vec, n):
    with p.nc.allow_non_contiguous_dma(reason="tiny param vectors"):
        p.dma(q, dst[:, :n], src_vec.rearrange("(k p) -> p k", p=128), r_dst, (), (r_dst,))


def rmsnorm_fm(p, c, x_ap, r_x, w_col, r_w, h_ap, r_h, n):
    for k in range(KD):
        ACT(p, AF.Square, c.sq[:, k, :n], x_ap[:, k, :n], (r_x,), (c.r_sq,))
    for k in range(KD):
        MM(p, c.ps_n[:, :n], c.ones_bf[:], c.sq[:, k, :n], k == 0, k == KD - 1,
           (c.r_sq, c.r_ones), (c.r_psn,))
    ACT(p, AF.Sqrt, c.rstd[:, :n], c.ps_n[:, :n], (c.r_psn, c.r_eps), (c.r_rstd,),
        scale=1.0 / D, bias=c.eps[:, 0:1])
    p.op("dve", lambda g: g.reciprocal(out=c.rstd[:, :n], in_=c.rstd[:, :n]), (c.r_rstd,), (c.r_rstd,))
    for k in range(KD):
        p.op("dve", lambda g, k=k: g.scalar_tensor_tensor(
            out=h_ap[:, k, :n], in0=x_ap[:, k, :n], scalar=w_col[:, k:k + 1],
            in1=c.rstd[:, :n], op0=ALU.mult, op1=ALU.mult), (r_x, r_w, c.r_rstd), (r_h,))


class MLP:
    def __init__(self, p, c):
        self.p, self.c = p, c
        self.NS = 3
        self.wu = [p.sb("wu%d" % i, [128, KD, 512], BF16) for i in range(self.NS)]
        self.r_wu = [p.res("wu%d" % i) for i in range(self.NS)]
        self.wd = [p.sb("wd%d" % i, [128, 8, 512], BF16) for i in range(self.NS)]
        self.r_wd = [p.res("wd%d" % i) for i in range(self.NS)]
        self.h = [p.sb("mh%d" % i, [128, KD, TT], BF16) for i in range(2)]
        self.r_h = [p.res("mh%d" % i) for i in range(2)]
        self.hid = [p.sb("hid%d" % i, [128, 32, TT], BF16) for i in range(2)]
        self.r_hid = [p.res("hid%d" % i) for i in range(2)]
        self.rl = [p.sb("rl%d" % i, [128, TT], BF16) for i in range(2)]
        self.r_rl = [p.res("rl%d" % i) for i in range(2)]
        self.ps_u = [p.ps("ps_u%d" % i, [128, TT]) for i in range(2)]
        self.r_psu = [p.res("psu%d" % i) for i in range(2)]
        self.ps_d = [p.ps("ps_d%d" % i, [128, TT]) for i in range(4)]
        self.r_psd = [p.res("psd%d" % i) for i in range(4)]
        self.wn = p.sb("mlp_wn", [128, KD], F32)
        self.r_wn = p.res("mlp_wn")
        self.iu = 0
        self.id = 0
        self.ih = 0
        self.ipu = 0

    def load_up(self, w_up, fb):
        s = self.iu % self.NS
        self.iu += 1
        self.p.dma("pool", self.wu[s][:], w_up[:, fb * 512:(fb + 1) * 512].rearrange("(k p) f -> p k f", p=128),
                   self.r_wu[s], (), (self.r_wu[s],))
        return s

    def load_down(self, w_down, half, g):
        s = self.id % self.NS
        self.id += 1
        src = w_down[g * 1024:(g + 1) * 1024, half * 512:(half + 1) * 512].rearrange("(k p) d -> p k d", p=128)
        self.p.dma("pool", self.wd[s][:], src, self.r_wd[s], (), (self.r_wd[s],))
        return s

    def run(self, x_in, x_out, ntiles, wn_vec, w_up, w_down, final_w=None):
        p, c = self.p, self.c
        load_cols(p, "sp", self.wn, self.r_wn, wn_vec, KD)
        if final_w is not None:
            self.fw = p.sb("mlp_fw", [128, KD], F32)
            self.r_fw = p.res("mlp_fw")
            load_cols(p, "sp", self.fw, self.r_fw, final_w, KD)
            self.fo = p.sb("mlp_fo", [128, KD, TT], F32)
            self.r_fo = p.res("mlp_fo")
        xs = [p.sb("mlpx%d" % i, [128, KD, TT], F32) for i in range(2)]
        r_xs = [p.res("mlpx%d" % i) for i in range(2)]
        sched = []
        for t in range(ntiles):
            for fb in range(8):
                sched.append(("u", fb))
            for half in range(2):
                for g in range(4):
                    sched.append(("d", half, g))
        slots = {}
        nxt = [0]

        def prefetch(upto):
            while nxt[0] < len(sched) and nxt[0] <= upto:
                it = sched[nxt[0]]
                if it[0] == "u":
                    slots[nxt[0]] = self.load_up(w_up, it[1])
                else:
                    slots[nxt[0]] = self.load_down(w_down, it[1], it[2])
                nxt[0] += 1

        def load_x(t):
            p.dma("sp", xs[t % 2][:], x_in[:, t * TT:(t + 1) * TT].rearrange("(k q) n -> q k n", q=128),
                  r_xs[t % 2], (), (r_xs[t % 2],))

        def norm(t):
            hs = self.ih % 2
            self.ih += 1
            rmsnorm_fm(p, c, xs[t % 2], r_xs[t % 2], self.wn, self.r_wn, self.h[hs], self.r_h[hs], TT)
            return hs

        prefetch(1)
        load_x(0)
        hs = norm(0)
        si = 0
        for t in range(ntiles):
            hb = t % 2
            xt, r_xt = xs[t % 2], r_xs[t % 2]
            for fb in range(8):
                prefetch(si + self.NS - 1)
                s = slots[si]
                si += 1
                for ft in range(4):
                    pu = self.ipu % 2
                    self.ipu += 1
                    for k in range(KD):
                        MM(p, self.ps_u[pu][:], self.wu[s][:, k, ft * 128:(ft + 1) * 128], self.h[hs][:, k, :],
                           k == 0, k == KD - 1, (self.r_wu[s], self.r_h[hs]), (self.r_psu[pu],))
                    ACT(p, AF.Relu, self.rl[pu][:], self.ps_u[pu][:], (self.r_psu[pu],), (self.r_rl[pu],))
                    f = fb * 4 + ft
                    p.op("dve", lambda g, f=f, pu=pu: g.tensor_tensor(
                        out=self.hid[hb][:, f, :], in0=self.rl[pu][:], in1=self.rl[pu][:], op=ALU.mult),
                        (self.r_rl[pu],), (self.r_hid[hb],))
            if t + 1 < ntiles:
                load_x(t + 1)
                hs_next = norm(t + 1)
            else:
                hs_next = None
            for half in range(2):
                for g in range(4):
                    prefetch(si + self.NS - 1)
                    s = slots[si]
                    si += 1
                    for dt_ in range(4):
                        for kk in range(8):
                            fc = g * 8 + kk
                            MM(p, self.ps_d[dt_][:], self.wd[s][:, kk, dt_ * 128:(dt_ + 1) * 128],
                               self.hid[hb][:, fc, :], fc == 0, fc == 31,
                               (self.r_wd[s], self.r_hid[hb]), (self.r_psd[dt_],))
                for dt_ in range(4):
                    k = half * 4 + dt_
                    p.op("dve", lambda g, k=k, dt_=dt_: g.tensor_tensor(
                        out=xt[:, k, :], in0=xt[:, k, :], in1=self.ps_d[dt_][:], op=ALU.add),
                        (self.r_psd[dt_], r_xt), (r_xt,))
            if final_w is None:
                p.dma("sp", x_out[:, t * TT:(t + 1) * TT].rearrange("(k q) n -> q k n", q=128), xt[:], r_xt,
                      (r_xt,), (), is_output=True)
            else:
                for k in range(KD):
                    ACT(p, AF.Square, c.sq[:, k, :], xt[:, k, :], (r_xt,), (c.r_sq,))
                for k in range(KD):
                    MM(p, c.ps_n[:], c.ones_bf[:], c.sq[:, k, :], k == 0, k == KD - 1, (c.r_sq, c.r_ones), (c.r_psn,))
                ACT(p, AF.Sqrt, c.rstd[:], c.ps_n[:], (c.r_psn, c.r_eps), (c.r_rstd,), scale=1.0 / D, bias=c.eps[:, 0:1])
                p.op("dve", lambda g: g.reciprocal(out=c.rstd[:], in_=c.rstd[:]), (c.r_rstd,), (c.r_rstd,))
                for k in range(KD):
                    p.op("dve", lambda g, k=k: g.scalar_tensor_tensor(
                        out=self.fo[:, k, :], in0=xt[:, k, :], scalar=self.fw[:, k:k + 1], in1=c.rstd[:],
                        op0=ALU.mult, op1=ALU.mult), (r_xt, self.r_fw, c.r_rstd), (self.r_fo,))
                p.dma("sp", x_out[:, t * TT:(t + 1) * TT].rearrange("(k q) n -> q k n", q=128), self.fo[:], self.r_fo,
                      (self.r_fo,), (), is_output=True)
            hs = hs_next


class WStream:
    def __init__(self, p, ns=3):
        self.p = p
        self.ns = ns
        self.t = [p.sb("ws%d" % i, [128, KD, 512], BF16) for i in range(ns)]
        self.r = [p.res("ws%d" % i) for i in range(ns)]
        self.sched = []
        self.issued = 0
        self.used = 0

    def plan(self, item):
        self.sched.append(item)

    def plan_cols(self, w, c0, n=512):
        self.plan([(0, n, w[:, c0:c0 + n].rearrange("(k p) f -> p k f", p=128))])

    def plan_rows(self, w, r0, c0, n=512):
        self.plan([(0, n, w[r0:r0 + 1024, c0:c0 + n].rearrange("(k p) f -> p k f", p=128))])

    def _issue(self, upto):
        while self.issued < len(self.sched) and self.issued <= upto:
            s = self.issued % self.ns
            for (c0, n, src) in self.sched[self.issued]:
                self.p.dma("pool", self.t[s][:, :, c0:c0 + n], src, self.r[s], (), (self.r[s],))
            self.issued += 1

    def next(self):
        i = self.used
        self._issue(i + self.ns - 1)
        self.used += 1
        return self.t[i % self.ns], self.r[i % self.ns]


class Banks:
    def __init__(self, p, n=6):
        self.b = [p.ps("bk%d" % i, [128, 512]) for i in range(n)]
        self.r = [p.res("bk%d" % i) for i in range(n)]
        self.tb = p.ps("bkT", [128, 1024], BF16)
        self.r_tb = p.res("bkT")


HG_C = 64


class HG1:
    def __init__(self, p, c, ws, bk, cd):
        self.p, self.c, self.ws, self.bk = p, c, ws, bk
        f32t = lambda n: p.sb(n, [128, TT], F32)
        self.xs = [p.sb("g1x%d" % i, [128, KD, TT], F32) for i in range(2)]
        self.r_xs = [p.res("g1x%d" % i) for i in range(2)]
        self.h = [p.sb("g1h%d" % i, [128, KD, TT], BF16) for i in range(1)]
        self.r_h = [p.res("g1h%d" % i) for i in range(1)]
        self.vtok = p.sb("vtok", [64, 8, 1024], BF16)
        self.r_vtok = p.res("vtok")
        self.qt = p.sb("qt", [128, 8, TT], BF16)
        self.kt = p.sb("kt", [128, 8, TT], BF16)
        self.kh = p.sb("kh", [128, 8, TT], BF16)
        self.r_qt, self.r_kt, self.r_kh = p.res("qt"), p.res("kt"), p.res("kh")
        names = ("sig", "ksig", "lf", "b", "Bg", "eb", "enb", "eB", "d2")
        self.Tm = [{n: f32t(n + str(i)) for n in names} for i in range(2)]
        self.Rt = [{n: p.res(n + str(i)) for n in names} for i in range(2)]
        self.a_all = p.sb("a_all", [128, 8, 8], F32)
        self.r_a = p.res("a_all")
        self.bgl = p.sb("bgl", [128, 8], F32)
        self.r_bgl = p.res("bgl")
        self.S = p.sb("S32", [128, 8, 128], F32)
        self.Sb = p.sb("Sbf", [128, 8, 128], BF16)
        self.r_S, self.r_Sb = p.res("S32"), p.res("Sbf")
        self.attn = p.sb("attn", [64, 8, 64], BF16)
        self.r_attn = p.res("attn")
        self.khat = p.sb("khat", [64, 8, 128], BF16)
        self.r_khat = p.res("khat")
        self.ost = p.sb("ost", [128, 8, TT], F32)
        self.r_ost = p.res("ost")
        self.qd = [p.sb("qdst%d" % i, [128, TT], BF16) for i in range(2)]
        self.r_qd = [p.res("qdst%d" % i) for i in range(2)]
        self.sg = [p.sb("sgst%d" % i, [128, TT], BF16) for i in range(2)]
        self.r_sg = [p.res("sgst%d" % i) for i in range(2)]
        self.wn = p.sb("g1wn", [128, KD], F32)
        self.r_wn = p.res("g1wn")
        self.alb = p.sb("alb", [128, 2, 8], F32)
        self.lbt = p.sb("lbt", [128, 8, 8], F32)
        self.r_lb = p.res("lb")
        self.ones32 = p.sb("ones32", [128, TT], F32)
        self.r_ones32 = p.res("ones32")
        self.cd = cd

    def setup(self, norm_w, alb_dram, layer):
        p = self.p
        load_cols(p, "sp", self.wn, self.r_wn, norm_w, KD)
        with p.nc.allow_non_contiguous_dma(reason="tiny"):
            p.dma("sp", self.alb[:], alb_dram.rearrange("l (h q) -> q l h", q=128), self.r_lb, (), (self.r_lb,))
        L = self.lbt
        R = (self.r_lb,)
        a0, a1 = self.alb[:, 0, :], self.alb[:, 1, :]
        p.op("dve", lambda g: g.tensor_tensor(out=L[:, 0, :], in0=a0, in1=a1, op=ALU.max), R, R)
        p.op("dve", lambda g: g.tensor_tensor(out=L[:, 1, :], in0=a0, in1=L[:, 0, :], op=ALU.subtract), R, R)
        p.op("dve", lambda g: g.tensor_tensor(out=L[:, 2, :], in0=a1, in1=L[:, 0, :], op=ALU.subtract), R, R)
        ACT(p, AF.Exp, L[:, 1:3, :], L[:, 1:3, :], R, R)
        p.op("dve", lambda g: g.tensor_tensor(out=L[:, 3, :], in0=L[:, 1, :], in1=L[:, 2, :], op=ALU.add), R, R)
        p.op("dve", lambda g: g.reciprocal(out=L[:, 3, :], in_=L[:, 3, :]), R, R)
        p.op("dve", lambda g: g.tensor_tensor(out=L[:, 4, :], in0=L[:, 1, :], in1=L[:, 3, :], op=ALU.mult), R, R)
        if layer == 0:
            p.op("dve", lambda g: g.tensor_copy(out=L[:, 5, :], in_=L[:, 4, :]), R, R)
        else:
            p.op("dve", lambda g: g.tensor_tensor(out=L[:, 5, :], in0=L[:, 2, :], in1=L[:, 3, :], op=ALU.mult), R, R)
            p.op("dve", lambda g: g.tensor_tensor(out=L[:, 5, :], in0=L[:, 5, :], in1=L[:, 4, :], op=ALU.add), R, R)
        p.op("dve", lambda g: g.tensor_tensor(out=L[:, 6, :], in0=L[:, 5, :], in1=L[:, 4, :], op=ALU.subtract), R, R)
        p.op("dve", lambda g: g.tensor_scalar(out=L[:, 7, :], in0=L[:, 6, :], scalar1=-1.0, scalar2=1.0,
                                              op0=ALU.mult, op1=ALU.add), R, R)
        p.op("dve", lambda g: g.memset(self.ones32[:], 1.0), (), (self.r_ones32,))
        p.op("dve", lambda g: g.memset(self.S[:], 0.0), (), (self.r_S,))
        p.op("dve", lambda g: g.memset(self.Sb[:], 0.0), (), (self.r_Sb,))
        p.op("dve", lambda g: g.memset(self.bgl[:], 0.0), (), (self.r_bgl,))

    def plan_weights(self, w_in, ntiles):
        for t in range(ntiles):
            for hb in range(4):
                self.ws.plan([(0, 256, w_in[:, hb * 256:(hb + 1) * 256].rearrange("(k p) f -> p k f", p=128)),
                              (256, 256, w_in[:, 1024 + hb * 256:1024 + (hb + 1) * 256].rearrange("(k p) f -> p k f", p=128))])
            for gb in range(2):
                self.ws.plan_cols(w_in, 3072 + gb * 512)
            for vb in range(2):
                self.ws.plan_cols(w_in, 2048 + vb * 512)

    def tile(self, t, xT_dram, o_loc, qdec, sgo):
        p, c, bk = self.p, self.c, self.bk
        T0 = t * TT
        xs, r_xs = self.xs[t % 2], self.r_xs[t % 2]
        p.dma("sp", xs[:], xT_dram[:, T0:T0 + TT].rearrange("(k q) n -> q k n", q=128), r_xs, (), (r_xs,))
        h, r_h = self.h[0], self.r_h[0]
        rmsnorm_fm(p, c, xs, r_xs, self.wn, self.r_wn, h, r_h, TT)
        L = self.lbt
        for hb in range(4):
            w, r_w = self.ws.next()
            for hh in range(2):
                hd = hb * 2 + hh
                pq, r_pq = bk.b[hd % 2], bk.r[hd % 2]
                pf, r_pf = bk.b[2 + hd % 2], bk.r[2 + hd % 2]
                for k in range(KD):
                    MM(p, pq[:], w[:, k, hh * 128:(hh + 1) * 128], h[:, k, :], k == 0, k == KD - 1, (r_w, r_h), (r_pq,))
                for k in range(KD):
                    MM(p, pf[:], w[:, k, 256 + hh * 128:256 + (hh + 1) * 128], h[:, k, :], k == 0, k == KD - 1,
                       (r_w, r_h), (r_pf,))
                lb_c, oml_c = L[:, 6, hd:hd + 1], L[:, 7, hd:hd + 1]
                Tm, rt = self.Tm[hd % 2], self.Rt[hd % 2]
                ACT(p, AF.Sigmoid, Tm["sig"][:], pf[:], (r_pf,), (rt["sig"],))
                ACT(p, AF.Sigmoid, Tm["ksig"][:], pf[:], (r_pf,), (rt["ksig"],), scale=-1.0)
                p.op("dve", lambda g: g.tensor_scalar(out=Tm["lf"][:], in0=Tm["sig"][:], scalar1=oml_c, scalar2=lb_c,
                                                      op0=ALU.mult, op1=ALU.add), (rt["sig"], self.r_lb), (rt["lf"],))
                p.op("dve", lambda g: g.tensor_scalar(out=Tm["lf"][:], in0=Tm["lf"][:], scalar1=1e-30, scalar2=None, op0=ALU.max),
                     (rt["lf"],), (rt["lf"],))
                ACT(p, AF.Ln, Tm["lf"][:], Tm["lf"][:], (rt["lf"],), (rt["lf"],))
                p.op("dve", lambda g: g.tensor_scalar(out=Tm["ksig"][:], in0=Tm["ksig"][:], scalar1=oml_c, scalar2=None, op0=ALU.mult),
                     (rt["ksig"], self.r_lb), (rt["ksig"],))
                p.op("dve", lambda g: g.tensor_tensor_scan(out=Tm["b"][:], data0=self.cd["reset"][:], data1=Tm["lf"][:],
                                                           initial=0.0, op0=ALU.mult, op1=ALU.add),
                     (rt["lf"], self.cd["r"]), (rt["b"],))
                p.op("dve", lambda g: g.tensor_tensor_scan(out=Tm["Bg"][:], data0=self.ones32[:], data1=Tm["lf"][:],
                                                           initial=self.bgl[:, hd:hd + 1], op0=ALU.mult, op1=ALU.add),
                     (rt["lf"], self.r_ones32, self.r_bgl), (rt["Bg"],))
                p.op("dve", lambda g: g.tensor_copy(out=self.bgl[:, hd:hd + 1], in_=Tm["Bg"][:, TT - 1:TT]),
                     (rt["Bg"],), (self.r_bgl,))
                ACT(p, AF.Exp, Tm["eb"][:], Tm["b"][:], (rt["b"],), (rt["eb"],))
                ACT(p, AF.Exp, Tm["enb"][:], Tm["b"][:], (rt["b"],), (rt["enb"],), scale=-1.0)
                ACT(p, AF.Exp, Tm["eB"][:], Tm["Bg"][:], (rt["Bg"],), (rt["eB"],))
                b3 = Tm["b"][:].rearrange("q (c s) -> q c s", s=HG_C)
                p.op("dve", lambda g: g.tensor_tensor(out=Tm["d2"][:].rearrange("q (c s) -> q c s", s=HG_C),
                                                       in0=b3[:, :, HG_C - 1:HG_C].to_broadcast([128, 8, HG_C]),
                                                       in1=b3, op=ALU.subtract), (rt["b"],), (rt["d2"],))
                ACT(p, AF.Exp, Tm["d2"][:], Tm["d2"][:], (rt["d2"],), (rt["d2"],))
                p.op("dve", lambda g: g.tensor_copy(out=self.a_all[:, hd, :], in_=Tm["eb"][:, HG_C - 1::HG_C]),
                     (rt["eb"],), (self.r_a,))
                p.op("dve", lambda g: g.tensor_tensor(out=self.qt[:, hd, :], in0=pq[:], in1=Tm["eb"][:], op=ALU.mult),
                     (r_pq, rt["eb"]), (self.r_qt,))
                qd, r_qd = self.qd[hd % 2], self.r_qd[hd % 2]
                p.op("dve", lambda g: g.tensor_tensor(out=qd[:], in0=pq[:], in1=Tm["eB"][:], op=ALU.mult),
                     (r_pq, rt["eB"]), (r_qd,))
                p.dma("sp", qdec[hd * 128:(hd + 1) * 128, T0:T0 + TT], qd[:], r_qd, (r_qd,), (), is_output=True)
                p.op("dve", lambda g: g.tensor_tensor(out=self.kt[:, hd, :], in0=Tm["ksig"][:], in1=Tm["enb"][:], op=ALU.mult),
                     (rt["ksig"], rt["enb"]), (self.r_kt,))
                p.op("dve", lambda g: g.tensor_tensor(out=self.kh[:, hd, :], in0=Tm["ksig"][:], in1=Tm["d2"][:], op=ALU.mult),
                     (rt["ksig"], rt["d2"]), (self.r_kh,))
        for gb in range(2):
            w, r_w = self.ws.next()
            for hh in range(4):
                hd = gb * 4 + hh
                pg, r_pg = bk.b[4 + hd % 2], bk.r[4 + hd % 2]
                for k in range(KD):
                    MM(p, pg[:], w[:, k, hh * 128:(hh + 1) * 128], h[:, k, :], k == 0, k == KD - 1, (r_w, r_h), (r_pg,))
                sg, r_sg = self.sg[hd % 2], self.r_sg[hd % 2]
                ACT(p, AF.Silu, sg[:], pg[:], (r_pg,), (r_sg,))
                p.dma("sp", sgo[hd * 128:(hd + 1) * 128, T0:T0 + TT], sg[:], r_sg, (r_sg,), (), is_output=True)
        for vb in range(2):
            w, r_w = self.ws.next()
            for ci in range(8):
                pv, r_pv = bk.b[4 + ci % 2], bk.r[4 + ci % 2]
                for k in range(KD):
                    MM(p, pv[0:64, :], h[:, k, ci * HG_C:(ci + 1) * HG_C], w[:, k, :], k == 0, k == KD - 1,
                       (r_w, r_h), (r_pv,))
                ACT(p, AF.Copy, self.vtok[:, ci, vb * 512:(vb + 1) * 512], pv[0:64, :], (r_pv,), (self.r_vtok,))
        pa, r_pa = bk.b[4], bk.r[4]
        po, r_po = bk.b[5], bk.r[5]
        pU, r_pU = (bk.b[0], bk.b[1]), (bk.r[0], bk.r[1])
        for ci in range(8):
            cs = slice(ci * HG_C, (ci + 1) * HG_C)
            for hd in range(8):
                MM(p, pa[0:64, hd * 64:(hd + 1) * 64], self.kt[:, hd, cs], self.qt[:, hd, cs], True, True,
                   (self.r_kt, self.r_qt), (r_pa,))
            p.op("dve", lambda g: g.tensor_tensor(out=self.attn[:], in0=pa[0:64, :].rearrange("q (h s) -> q h s", s=64),
                                                  in1=self.cd["tri"][:].unsqueeze(1).to_broadcast([64, 8, 64]),
                                                  op=ALU.mult), (r_pa, self.cd["r"]), (self.r_attn,))
            for hd in range(8):
                p.op("pe", lambda g, hd=hd: g.transpose(bk.tb[0:64, hd * 128:(hd + 1) * 128], self.kh[:, hd, cs],
                                                        self.cd["ident"][:]),
                     (self.r_kh, self.cd["r"]), (bk.r_tb,))
            ACT(p, AF.Copy, self.khat[:].rearrange("q h d -> q (h d)"), bk.tb[0:64, :], (bk.r_tb,), (self.r_khat,))
            for hd in range(8):
                MM(p, pU[hd // 4][:, (hd % 4) * 128:(hd % 4 + 1) * 128], self.khat[:, hd, :],
                   self.vtok[:, ci, hd * 128:(hd + 1) * 128], True, True,
                   (self.r_khat, self.r_vtok), (r_pU[hd // 4],))
            for hd in range(8):
                MM(p, po[:, hd * 64:(hd + 1) * 64], self.vtok[:, ci, hd * 128:(hd + 1) * 128], self.attn[:, hd, :],
                   True, False, (self.r_vtok, self.r_attn), (r_po,))
                MM(p, po[:, hd * 64:(hd + 1) * 64], self.Sb[:, hd, :], self.qt[:, hd, cs],
                   False, True, (self.r_Sb, self.r_qt), (r_po,))
            ACT(p, AF.Copy, self.ost[:, :, cs], po[:].rearrange("q (h s) -> q h s", s=64), (r_po,), (self.r_ost,))
            for hd in range(8):
                p.op("dve", lambda g, hd=hd: g.scalar_tensor_tensor(
                    out=self.S[:, hd, :], in0=self.S[:, hd, :], scalar=self.a_all[:, hd, ci:ci + 1],
                    in1=pU[hd // 4][:, (hd % 4) * 128:(hd % 4 + 1) * 128], op0=ALU.mult, op1=ALU.add),
                    (self.r_S, self.r_a, r_pU[hd // 4]), (self.r_S,))
            ACT(p, AF.Copy, self.Sb[:], self.S[:], (self.r_S,), (self.r_Sb,))
        p.dma("sp", o_loc[:, T0:T0 + TT].rearrange("(h q) n -> q h n", q=128), self.ost[:], self.r_ost,
              (self.r_ost,), (), is_output=True)

    def finish(self, s_fin, dec):
        p = self.p
        p.dma("sp", s_fin, self.S[:], self.r_S, (self.r_S,), (), is_output=True)
        ACT(p, AF.Exp, self.lbt[:, 0, :], self.bgl[:], (self.r_bgl,), (self.r_lb,))
        p.dma("sp", dec, self.lbt[:, 0, :], self.r_lb, (self.r_lb,), (), is_output=True)


def load_consts_hg(p, reset_d, tri_d, ident_d):
    cd = {"r": p.res("cd")}
    cd["reset"] = p.sb("c_reset", [128, TT], F32)
    cd["tri"] = p.sb("c_tri", [64, 64], BF16)
    cd["ident"] = p.sb("c_ident", [128, 128], BF16)
    p.dma("sp", cd["reset"][:], reset_d, cd["r"], (), (cd["r"],))
    p.dma("sp", cd["tri"][:], tri_d, cd["r"], (), (cd["r"],))
    p.dma("sp", cd["ident"][:], ident_d, cd["r"], (), (cd["r"],))
    return cd


class HG2:
    def __init__(self, p, c, ws, bk):
        self.p, self.c, self.ws, self.bk = p, c, ws, bk
        self.Sin = p.sb("Sin", [128, 8, 128], F32)
        self.SinB = p.sb("SinB", [128, 8, 128], BF16)
        self.r_Sin, self.r_SinB = p.res("Sin"), p.res("SinB")
        self.Sf = [p.sb("Sf%d" % i, [128, 8, 128], F32) for i in range(2)]
        self.r_Sf = [p.res("Sf%d" % i) for i in range(2)]
        self.dcl = p.sb("dcl", [128, 8, 8], F32)
        self.selv = p.sb("selv", [128, 8], F32)
        self.al = p.sb("al", [128, 8], F32)
        self.r_sm = p.res("hg2small")
        self.gw = p.sb("gw", [128, 1], F32)
        self.r_gw = p.res("gw")
        self.xs = [p.sb("g2x%d" % i, [128, KD, TT], F32) for i in range(2)]
        self.r_xs = [p.res("g2x%d" % i) for i in range(2)]
        self.ot = p.sb("g2o", [128, 8, TT], F32)
        self.qd = p.sb("g2qd", [128, 8, TT], BF16)
        self.sgt = p.sb("g2sg", [128, 8, TT], BF16)
        self.on = p.sb("g2on", [128, 8, TT], BF16)
        self.tmp = p.sb("g2tmp", [128, TT], F32)
        self.r_ot, self.r_qd, self.r_sgt, self.r_on, self.r_tmp = (p.res("g2o"), p.res("g2qd"), p.res("g2sg"),
                                                                   p.res("g2on"), p.res("g2tmp"))
        self.eps128 = c.eps

    def prefix(self, S_all, dec_all, selv_d, gnorm_w):
        p = self.p
        R = (self.r_sm,)
        with p.nc.allow_non_contiguous_dma(reason="tiny"):
            p.dma("sp", self.dcl[:], dec_all.rearrange("c q h -> q c h"), self.r_sm, (), R)
            p.dma("sp", self.selv[:], selv_d, self.r_sm, (), R)
            p.dma("sp", self.gw[:], gnorm_w.rearrange("(q o) -> q o", o=1), self.r_gw, (), (self.r_gw,))
        p.op("dve", lambda g: g.memset(self.Sin[:], 0.0), (), (self.r_Sin,))
        for cp in range(8):
            sf, r_sf = self.Sf[cp % 2], self.r_Sf[cp % 2]
            p.dma("sp", sf[:], S_all[cp], r_sf, (), (r_sf,))
            sel_c = self.selv[:, cp:cp + 1]
            p.op("dve", lambda g: g.tensor_scalar(out=self.al[:], in0=self.dcl[:, cp, :], scalar1=-1.0, scalar2=sel_c,
                                                  op0=ALU.add, op1=ALU.mult), R, R)
            p.op("dve", lambda g: g.tensor_scalar(out=self.al[:], in0=self.al[:], scalar1=1.0, scalar2=None,
                                                  op0=ALU.add), R, R)
            p.op("dve", lambda g: g.tensor_scalar(out=sf[:], in0=sf[:], scalar1=sel_c, scalar2=None, op0=ALU.mult),
                 (r_sf, self.r_sm), (r_sf,))
            for hd in range(8):
                p.op("dve", lambda g, hd=hd: g.scalar_tensor_tensor(
                    out=self.Sin[:, hd, :], in0=self.Sin[:, hd, :], scalar=self.al[:, hd:hd + 1], in1=sf[:, hd, :],
                    op0=ALU.mult, op1=ALU.add), (self.r_Sin, self.r_sm, r_sf), (self.r_Sin,))
        ACT(p, AF.Copy, self.SinB[:], self.Sin[:], (self.r_Sin,), (self.r_SinB,))

    def plan_weights(self, w_out, ntiles):
        for t in range(ntiles):
            for blk in range(2):
                self.ws.plan_cols(w_out, blk * 512)

    def tile(self, t, x_in, x_out, o_loc, qdec, sgo):
        p, c, bk = self.p, self.c, self.bk
        T0 = t * TT
        xs, r_xs = self.xs[t % 2], self.r_xs[t % 2]
        p.dma("sp", xs[:], x_in[:, T0:T0 + TT].rearrange("(k q) n -> q k n", q=128), r_xs, (), (r_xs,))
        p.dma("sp", self.ot[:], o_loc[:, T0:T0 + TT].rearrange("(h q) n -> q h n", q=128), self.r_ot, (), (self.r_ot,))
        p.dma("sp", self.qd[:], qdec[:, T0:T0 + TT].rearrange("(h q) n -> q h n", q=128), self.r_qd, (), (self.r_qd,))
        p.dma("sp", self.sgt[:], sgo[:, T0:T0 + TT].rearrange("(h q) n -> q h n", q=128), self.r_sgt, (), (self.r_sgt,))
        for hd in range(8):
            pc, r_pc = bk.b[hd % 2], bk.r[hd % 2]
            MM(p, pc[:], self.SinB[:, hd, :], self.qd[:, hd, :], True, True, (self.r_SinB, self.r_qd), (r_pc,))
            p.op("dve", lambda g: g.tensor_tensor(out=self.ot[:, hd, :], in0=self.ot[:, hd, :], in1=pc[:], op=ALU.add),
                 (self.r_ot, r_pc), (self.r_ot,))
            ACT(p, AF.Square, c.sq[:, hd, :], self.ot[:, hd, :], (self.r_ot,), (c.r_sq,))
            pn, r_pn = bk.b[2 + hd % 2], bk.r[2 + hd % 2]
            MM(p, pn[:], c.ones_bf[:], c.sq[:, hd, :], True, True, (c.r_sq, c.r_ones), (r_pn,))
            ACT(p, AF.Sqrt, c.rstd[:], pn[:], (r_pn, c.r_eps), (c.r_rstd,), scale=1.0 / 128, bias=c.eps[:, 0:1])
            p.op("dve", lambda g: g.reciprocal(out=c.rstd[:], in_=c.rstd[:]), (c.r_rstd,), (c.r_rstd,))
            p.op("dve", lambda g: g.scalar_tensor_tensor(out=self.tmp[:], in0=self.ot[:, hd, :], scalar=self.gw[:, 0:1],
                                                         in1=c.rstd[:], op0=ALU.mult, op1=ALU.mult),
                 (self.r_ot, self.r_gw, c.r_rstd), (self.r_tmp,))
            p.op("dve", lambda g: g.tensor_tensor(out=self.on[:, hd, :], in0=self.tmp[:], in1=self.sgt[:, hd, :],
                                                   op=ALU.mult), (self.r_tmp, self.r_sgt), (self.r_on,))
        for blk in range(2):
            w, r_w = self.ws.next()
            for dt_ in range(4):
                po, r_po = bk.b[4 + dt_ % 2], bk.r[4 + dt_ % 2]
                for k in range(KD):
                    MM(p, po[:], w[:, k, dt_ * 128:(dt_ + 1) * 128], self.on[:, k, :], k == 0, k == KD - 1,
                       (r_w, self.r_on), (r_po,))
                kk = blk * 4 + dt_
                p.op("dve", lambda g: g.tensor_tensor(out=xs[:, kk, :], in0=xs[:, kk, :], in1=po[:], op=ALU.add),
                     (r_xs, r_po), (r_xs,))
        p.dma("sp", x_out[:, T0:T0 + TT].rearrange("(k q) n -> q k n", q=128), xs[:], r_xs, (r_xs,), (), is_output=True)


def host_consts_hg():
    import ml_dtypes
    reset = np.ones((128, TT), np.float32)
    reset[:, ::HG_C] = 0.0
    tri = (np.arange(64)[:, None] <= np.arange(64)[None, :]).astype(ml_dtypes.bfloat16)
    ident = np.eye(128).astype(ml_dtypes.bfloat16)
    return {"c_reset": reset, "c_tri": tri, "c_ident": ident}


ATT_DUMMY_N = 0
NSA_SLOPES = [2.0 ** (-(h + 1) / 2.0) for h in range(16)]
QK_SCALE = 0.125
NAUG = 7
KROWS = 96


class KVPhase:
    def __init__(self, p, c, ws, bk):
        self.p, self.c, self.ws, self.bk = p, c, ws, bk
        self.xs = [p.sb("kvx%d" % i, [128, KD, TT], F32) for i in range(2)]
        self.r_xs = [p.res("kvx%d" % i) for i in range(2)]
        self.h = p.sb("kvh", [128, KD, TT], BF16)
        self.r_h = p.res("kvh")
        self.wn = p.sb("kvwn", [128, KD], F32)
        self.r_wn = p.res("kvwn")
        self.st = [p.sb("kvst%d" % i, [128, TT], BF16) for i in range(2)]
        self.r_st = [p.res("kvst%d" % i) for i in range(2)]
        self.sq32 = p.sb("kvsq", [128, TT], BF16)
        self.r_sq32 = p.res("kvsq")
        self.bd = p.sb("kvbd", [128, 128], BF16)
        self.r_bd = p.res("kvbd")
        self.kmx = p.sb("kvkmx", [128, 4], F32)
        self.mtmp = p.sb("kvmt", [128, 1], F32)
        self.r_kmx = p.res("kvkmx")
        self.vst = [p.sb("kvvst%d" % i, [128, 8, 65], BF16) for i in range(2)]
        self.r_vst = [p.res("kvvst%d" % i) for i in range(2)]

    def setup(self, norm_w):
        p = self.p
        load_cols(p, "sp", self.wn, self.r_wn, norm_w, KD)
        p.op("dve", lambda g: g.memset(self.bd[:], 0.0), (), (self.r_bd,))
        p.op("dve", lambda g: g.memset(self.bd[0:64, 0:64], 1.0), (), (self.r_bd,))
        p.op("dve", lambda g: g.memset(self.bd[64:128, 64:128], 1.0), (), (self.r_bd,))
        p.op("dve", lambda g: g.memset(self.kmx[:], 0.0), (), (self.r_kmx,))
        for i in range(2):
            p.op("dve", lambda g, i=i: g.memset(self.vst[i][:], 1.0), (), (self.r_vst[i],))

    def plan_weights(self, kv_w, ntiles):
        re = lambda a: a.rearrange("(k q) f -> q k f", q=128)
        for t in range(ntiles):
            self.ws.plan([(0, 512, re(kv_w[:, 0:512]))])
            self.ws.plan([(0, 256, re(kv_w[:, 512:768])), (256, 256, re(kv_w[:, 1024:1280]))])
            self.ws.plan([(0, 256, re(kv_w[:, 768:1024])), (256, 256, re(kv_w[:, 1280:1536]))])

    def tile(self, t, x_in, kT01, kT24, vaug):
        p, c, bk = self.p, self.c, self.bk
        T0 = t * TT
        xs, r_xs = self.xs[t % 2], self.r_xs[t % 2]
        p.dma("sp", xs[:], x_in[:, T0:T0 + TT].rearrange("(k q) n -> q k n", q=128), r_xs, (), (r_xs,))
        rmsnorm_fm(p, c, xs, r_xs, self.wn, self.r_wn, self.h, self.r_h, TT)
        h, r_h = self.h, self.r_h
        n = 0
        for blk, dst in ((0, kT01), (1, kT24)):
            w, r_w = self.ws.next()
            for ct in range(4):
                pp, r_pp = bk.b[n % 2], bk.r[n % 2]
                st, r_st = self.st[n % 2], self.r_st[n % 2]
                n += 1
                for k in range(KD):
                    MM(p, pp[:], w[:, k, ct * 128:(ct + 1) * 128], h[:, k, :], k == 0, k == KD - 1, (r_w, r_h), (r_pp,))
                ACT(p, AF.Copy, st[:], pp[:], (r_pp,), (r_st,))
                p.dma("sp", dst[ct * 128:(ct + 1) * 128, T0:T0 + TT], st[:], r_st, (r_st,), (), is_output=True)
                if blk == 1:
                    ACT(p, AF.Square, self.sq32[:], pp[:], (r_pp,), (self.r_sq32,))
                    pm, r_pm = bk.b[2], bk.r[2]
                    MM(p, pm[:], self.bd[:], self.sq32[:], True, True, (self.r_bd, self.r_sq32), (r_pm,))
                    p.op("dve", lambda g: g.reduce_max(out=self.mtmp[:], in_=pm[:], axis=AX.X), (r_pm,), (self.r_kmx,))
                    p.op("dve", lambda g, ct=ct: g.tensor_tensor(out=self.kmx[:, ct:ct + 1], in0=self.kmx[:, ct:ct + 1],
                                                                 in1=self.mtmp[:], op=ALU.max), (self.r_kmx,), (self.r_kmx,))
        w, r_w = self.ws.next()
        for sub in range(TT // 128):
            pp, r_pp = bk.b[3 + sub % 2], bk.r[3 + sub % 2]
            vst, r_vst = self.vst[sub % 2], self.r_vst[sub % 2]
            for k in range(KD):
                MM(p, pp[:], h[:, k, sub * 128:(sub + 1) * 128], w[:, k, :], k == 0, k == KD - 1, (r_w, r_h), (r_pp,))
            ACT(p, AF.Copy, vst[:, :, 0:64], pp[:].rearrange("q (s d) -> q s d", d=64), (r_pp,), (r_vst,))
            ch = t * (TT // 128) + sub
            p.dma("sp", vaug[ch], vst[:], r_vst, (r_vst,), (), is_output=True)

    def finish(self, kmx_out):
        p = self.p
        p.dma("sp", kmx_out, self.kmx[:], self.r_kmx, (self.r_kmx,), (), is_output=True)


class CMPPhase:
    def __init__(self, p, c, bk, NB):
        self.p, self.c, self.bk = p, c, bk
        self.NB = NB
        self.kin = p.sb("cmkin", [64, 4, 16 * NB + 16], BF16)
        self.r_kin = p.res("cmkin")
        self.w1 = p.sb("cmw1", [64, 32, 256], BF16)
        self.r_w1 = p.res("cmw1")
        self.w2 = p.sb("cmw2", [128, 2, 64], BF16)
        self.r_w2 = p.res("cmw2")
        self.peT = p.sb("cmpe", [64, 32], BF16)
        self.r_pe = p.res("cmpe")
        self.c1 = p.sb("cmc1", [128, 2], F32)
        self.r_c1 = p.res("cmc1")
        self.z = p.sb("cmz", [128, NB], F32)
        self.z2 = p.sb("cmz2", [128, NB], F32)
        self.r_z, self.r_z2 = p.res("cmz"), p.res("cmz2")
        self.gl = p.sb("cmgl", [128, 2, NB], BF16)
        self.r_gl = p.res("cmgl")
        self.ko = p.sb("cmko", [64, 4, NB], BF16)
        self.r_ko = p.res("cmko")
        self.vo = p.sb("cmvo", [NB, 4, 65], BF16)
        self.r_vo = p.res("cmvo")
        self.sq = p.sb("cmsq", [64, NB], BF16)
        self.r_sq = p.res("cmsq")
        self.kmx = p.sb("cmkmx", [64, 4], F32)
        self.r_kmx = p.res("cmkmx")

    def run(self, kin_d, vin_d, pe_k, w1_k, w2_k, pe_v, w1_v, w2_v, kc_out, vc_out, kmx_out):
        p, c, bk = self.p, self.c, self.bk
        NB = self.NB
        p.op("dve", lambda g: g.memset(self.vo[:], 1.0), (), (self.r_vo,))
        for si, (src, pe, w1, w2) in enumerate(((kin_d, pe_k, w1_k, w2_k), (vin_d, pe_v, w1_v, w2_v))):
            p.dma("sp", self.kin[:], src.rearrange("g d n -> d g n"), self.r_kin, (), (self.r_kin,))
            p.dma("pool", self.w1[:], w1.rearrange("(j d) m -> d j m", d=64), self.r_w1, (), (self.r_w1,))
            p.dma("pool", self.w2[:], w2.rearrange("(k q) d -> q k d", q=128), self.r_w2, (), (self.r_w2,))
            with p.nc.allow_non_contiguous_dma(reason="tiny pe"):
                p.dma("pool", self.peT[:], pe.rearrange("j d -> d j"), self.r_pe, (), (self.r_pe,))
            for mt in range(2):
                pb, r_pb = bk.b[0], bk.r[0]
                for j in range(32):
                    MM(p, pb[:, 0:1], self.w1[:, j, mt * 128:(mt + 1) * 128], self.peT[:, j:j + 1], j == 0, j == 31,
                       (self.r_w1, self.r_pe), (r_pb,))
                p.op("dve", lambda g, mt=mt: g.tensor_copy(out=self.c1[:, mt:mt + 1], in_=pb[:, 0:1]), (r_pb,), (self.r_c1,))
            for gi in range(4):
                for mt in range(2):
                    ph, r_ph = bk.b[1 + mt], bk.r[1 + mt]
                    for j in range(32):
                        MM(p, ph[:, 0:NB], self.w1[:, j, mt * 128:(mt + 1) * 128], self.kin[:, gi, j:j + 16 * (NB - 1) + 1:16],
                           j == 0, j == 31, (self.r_w1, self.r_kin), (r_ph,))
                    ACT(p, AF.Identity, self.z[:], ph[:, 0:NB], (r_ph, self.r_c1), (self.r_z,), bias=self.c1[:, mt:mt + 1])
                    p.op("dve", lambda g: g.tensor_tensor(out=self.z2[:], in0=self.z[:], in1=self.z[:], op=ALU.mult),
                         (self.r_z,), (self.r_z2,))
                    p.op("dve", lambda g: g.tensor_scalar(out=self.z2[:], in0=self.z2[:], scalar1=0.044715, scalar2=1.0,
                                                          op0=ALU.mult, op1=ALU.add), (self.r_z2,), (self.r_z2,))
                    p.op("dve", lambda g: g.tensor_tensor(out=self.z2[:], in0=self.z2[:], in1=self.z[:], op=ALU.mult),
                         (self.r_z2, self.r_z), (self.r_z2,))
                    ACT(p, AF.Sigmoid, self.z2[:], self.z2[:], (self.r_z2,), (self.r_z2,), scale=1.5957691216057308)
                    p.op("dve", lambda g, mt=mt: g.tensor_tensor(out=self.gl[:, mt, :], in0=self.z2[:], in1=self.z[:],
                                                                 op=ALU.mult), (self.r_z2, self.r_z), (self.r_gl,))
                if si == 0:
                    po, r_po = bk.b[3], bk.r[3]
                    for mt in range(2):
                        MM(p, po[0:64, 0:NB], self.w2[:, mt, :], self.gl[:, mt, :], mt == 0, mt == 1,
                           (self.r_w2, self.r_gl), (r_po,))
                    ACT(p, AF.Copy, self.ko[:, gi, :], po[0:64, 0:NB], (r_po,), (self.r_ko,))
                    ACT(p, AF.Square, self.sq[:], po[0:64, 0:NB], (r_po,), (self.r_sq,))
                    pm, r_pm = bk.b[4], bk.r[4]
                    MM(p, pm[0:64, 0:NB], c.ones_bf[0:64, 0:64], self.sq[:], True, True, (c.r_ones, self.r_sq), (r_pm,))
                    p.op("dve", lambda g, gi=gi: g.reduce_max(out=self.kmx[:, gi:gi + 1], in_=pm[0:64, 0:NB], axis=AX.X),
                         (r_pm,), (self.r_kmx,))
                else:
                    po, r_po = bk.b[3], bk.r[3]
                    for mt in range(2):
                        MM(p, po[0:NB, 0:64], self.gl[:, mt, :], self.w2[:, mt, :], mt == 0, mt == 1,
                           (self.r_w2, self.r_gl), (r_po,))
                    ACT(p, AF.Copy, self.vo[:, gi, 0:64], po[0:NB, 0:64], (r_po,), (self.r_vo,))
        p.dma("sp", kc_out.rearrange("g d n -> d g n"), self.ko[:], self.r_ko, (self.r_ko,), (), is_output=True)
        p.dma("sp", vc_out, self.vo[:], self.r_vo, (self.r_vo,), (), is_output=True)
        p.dma("sp", kmx_out, self.kmx[:], self.r_kmx, (self.r_kmx,), (), is_output=True)


class ATTPhase:
    PC = 16

    def __init__(self, p, c, bk, NS, T):
        self.p, self.c, self.bk, self.NS, self.T = p, c, bk, NS, T
        self.NSB = T // 64
        self.NCc = max(1, (T // 16) // 128)
        NSB, NCc, PC = self.NSB, self.NCc, self.PC
        sb, res = p.sb, p.res
        self.win = sb("at_win", [128, KD, 1072], BF16)
        self.r_win = res("at_win")
        self.wn = sb("at_wn", [128, KD], F32)
        self.r_wn = res("at_wn")
        self.x4 = sb("at_x4", [128, KD, TT], F32)
        self.r_x4 = res("at_x4")
        self.h4 = sb("at_h4", [128, KD, TT], BF16)
        self.r_h4 = res("at_h4")
        self.Qa = [sb("at_Qa%d" % i, [128, 16, KROWS], BF16) for i in range(2)]
        self.r_Qa = [res("at_Qa%d" % i) for i in range(2)]
        self.QT = [sb("at_QT%d" % i, [KROWS, 4, 512], BF16) for i in range(2)]
        self.r_QT = [res("at_QT%d" % i) for i in range(2)]
        self.gates = [sb("at_gt%d" % i, [128, 48], F32) for i in range(2)]
        self.r_gates = [res("at_gt%d" % i) for i in range(2)]
        self.sqt = sb("at_sqt", [128, 512], F32)
        self.r_sqt = res("at_sqt")
        self.sm = sb("at_sm", [128, 8, 16], F32)
        self.r_sm = res("at_sm")
        self.KMs = sb("at_KMs", [128, 16], F32)
        self.kmall = sb("at_kmall", [128, 4, 24], F32)
        self.r_KM = res("at_KM")
        self.cst = {}
        self.r_cst = res("at_cst")
        for nm, shp, dt in (("ident", [128, 128], BF16), ("ident32", [128, 128], F32), ("iota1", [128, 128], F32), ("iota2", [128, 128], F32),
                            ("negL", [128, 128], F32), ("negU", [128, 128], F32), ("apool", [128, NCc, NSB], BF16),
                            ("thr1", [128, NS * NCc], F32), ("thr2", [128, 8], F32), ("negst", [128, NS, 16], F32),
                            ("bonus", [128, NS, NSB], F32)):
            self.cst[nm] = sb("at_c_" + nm, shp, dt)
        self.KTc = sb("at_KTc", [KROWS, 4, NCc * 128], BF16)
        self.Vc = sb("at_Vc", [128, 4, NCc, 65], BF16)
        self.r_kvc = res("at_kvc")
        self.KTp = [sb("at_KTp%d" % i, [KROWS, PC * 128], BF16) for i in range(3)]
        self.Vp = [sb("at_Vp%d" % i, [128, PC, 65], BF16) for i in range(3)]
        self.r_kvp = [res("at_kvp%d" % i) for i in range(3)]
        self.ikv = 0
        self.KTw = [sb("at_KTw%d" % i, [KROWS, 640], BF16) for i in range(2)]
        self.Vw = [sb("at_Vw%d" % i, [128, 5, 65], BF16) for i in range(2)]
        self.r_kvw = [res("at_kvw%d" % i) for i in range(2)]
        self.iw = 0
        self.NE = 6
        self.e = [sb("at_e%d" % i, [128, 512], BF16) for i in range(self.NE)]
        self.r_e = [res("at_e%d" % i) for i in range(self.NE)]
        self.pp = [sb("at_p%d" % i, [128, 512], BF16) for i in range(self.NE)]
        self.r_pp = [res("at_p%d" % i) for i in range(self.NE)]
        self.ie = 0
        self.zt = [sb("at_zt%d" % i, [128, 512], F32) for i in range(2)]
        self.r_zt = [res("at_zt%d" % i) for i in range(2)]
        self.m2 = [sb("at_m2%d" % i, [128, 128], F32) for i in range(3)]
        self.r_m2 = [res("at_m2%d" % i) for i in range(3)]
        self.im2 = 0
        self.sc = sb("at_sc", [128, NSB], F32)
        self.sc2 = sb("at_sc2", [128, NSB], F32)
        self.m8 = sb("at_m8", [128, 16], F32)
        self.sel = sb("at_sel", [128, NSB], BF16)
        self.selx = [sb("at_selx%d" % i, [128, 1024], BF16) for i in range(2)]
        self.r_selx = [res("at_selx%d" % i) for i in range(2)]
        self.r_sc, self.r_sel = res("at_sc"), res("at_sel")
        self.rd = sb("at_rd", [128, 8], F32)
        self.r_rd = res("at_rd")
        self.oacc = sb("at_oacc", [128, 16, 64], F32)
        self.r_oacc = res("at_oacc")
        self.obf = [sb("at_obf%d" % i, [128, 1024], BF16) for i in range(2)]
        self.r_obf = [res("at_obf%d" % i) for i in range(2)]
        self.obT = [sb("at_obT%d" % i, [128, 8, 128], BF16) for i in range(2)]
        self.r_obT = [res("at_obT%d" % i) for i in range(2)]
        self.ps_s = (bk.b[0], bk.b[1])
        self.r_ps_s = (bk.r[0], bk.r[1])
        self.ps_o = (bk.b[3], bk.b[3])
        self.r_ps_o = (bk.r[3], bk.r[3])
        self.poT, self.r_poT = bk.b[2], bk.r[2]
        self.oTs = sb("at_oTs", [65, 512], F32)
        self.r_oTs = res("at_oTs")
        self.ps_r = (bk.b[4], bk.b[5])
        self.r_ps_r = (bk.r[4], bk.r[5])
        self.iss = 0
        self.ipo = 0
        self.r_tbh = (bk.r_tb, bk.r_tb)

    def setup(self, d):
        p = self.p
        load_cols(p, "sp", self.wn, self.r_wn, d["norm_w"], KD)
        re = lambda a: a.rearrange("(k q) f -> q k f", q=128)
        p.dma("pool", self.win[:, :, 0:512], re(d["w_in"][:, 0:512]), self.r_win, (), (self.r_win,))
        p.dma("pool", self.win[:, :, 512:1024], re(d["w_in"][:, 512:1024]), self.r_win, (), (self.r_win,))
        with p.nc.allow_non_contiguous_dma(reason="small gate cols"):
            p.dma("pool", self.win[:, :, 1024:1072], re(d["w_in"][:, 1024:1072]), self.r_win, (), (self.r_win,))
        for nm in self.cst:
            p.dma("sp", self.cst[nm][:], d["c_" + nm], self.r_cst, (), (self.r_cst,))
        for i in range(2):
            p.op("dve", lambda g, i=i: g.memset(self.Qa[i][:], 0.0), (), (self.r_Qa[i],))
            with p.nc.allow_non_contiguous_dma(reason="small const cols"):
                p.dma("sp", self.Qa[i][:, :, 67:71], d["c_qaug"], self.r_Qa[i], (), (self.r_Qa[i],))
        p.dma("sp", self.KTc[:], d["KTc"].rearrange("g r n -> r g n"), self.r_kvc, (), (self.r_kvc,))
        p.dma("sp", self.Vc[:], d["Vc"].rearrange("g q c e -> q g c e"), self.r_kvc, (), (self.r_kvc,))
        p.dma("sp", self.kmall[:], d["kmall"], self.r_KM, (), (self.r_KM,))
        R = (self.r_KM,)
        for g_ in range(4):
            p.op("dve", lambda g, g_=g_: g.reduce_max(out=self.KMs[:, g_ * 4:g_ * 4 + 1], in_=self.kmall[:, g_, :], axis=AX.X), R, R)
        ACT(p, AF.Sqrt, self.KMs[:, 0::4], self.KMs[:, 0::4], R, R)
        p.op("dve", lambda g: g.tensor_scalar(out=self.KMs[:, 0::4], in0=self.KMs[:, 0::4], scalar1=1.02, scalar2=None,
                                              op0=ALU.mult), R, R)
        for j in range(1, 4):
            p.op("dve", lambda g, j=j: g.tensor_copy(out=self.KMs[:, j::4], in_=self.KMs[:, 0::4]), R, R)

    def prep4(self, s4, d):
        p, c = self.p, self.c
        n = min(4, self.NS - s4) * 128
        p.dma("sp", self.x4[:, :, :n], d["xT"][:, s4 * 128:s4 * 128 + n].rearrange("(k q) n -> q k n", q=128),
              self.r_x4, (), (self.r_x4,))
        rmsnorm_fm(p, c, self.x4, self.r_x4, self.wn, self.r_wn, self.h4, self.r_h4, n)

    def prep_slot(self, s):
        p, bk = self.p, self.bk
        sub = s % 4
        tok = slice(sub * 128, (sub + 1) * 128)
        Qa, r_Qa = self.Qa[s % 2], self.r_Qa[s % 2]
        QT, r_QT = self.QT[s % 2], self.r_QT[s % 2]
        gates, r_gates = self.gates[s % 2], self.r_gates[s % 2]
        sm, R = self.sm, (self.r_sm,)
        for half in range(2):
            pq, r_pq = self.ps_r[half], self.r_ps_r[half]
            for k in range(KD):
                MM(p, pq[:], self.h4[:, k, tok], self.win[:, k, half * 512:(half + 1) * 512], k == 0, k == KD - 1,
                   (self.r_h4, self.r_win), (r_pq,))
            ACT(p, AF.Copy, Qa[:, half * 8:(half + 1) * 8, 0:64], pq[:].rearrange("q (h d) -> q h d", d=64),
                (r_pq,), (r_Qa,), scale=QK_SCALE)
            ACT(p, AF.Square, self.sqt[:], pq[:], (r_pq,), (self.r_sqt,), scale=QK_SCALE)
            p.op("dve", lambda g, half=half: g.tensor_reduce(out=sm[:, 0, half * 8:(half + 1) * 8],
                                                             in_=self.sqt[:].rearrange("q (h d) -> q h d", d=64),
                                                             axis=AX.X, op=ALU.add), (self.r_sqt,), R)
        pg, r_pg = self.ps_r[0], self.r_ps_r[0]
        for k in range(KD):
            MM(p, pg[:, 0:48], self.h4[:, k, tok], self.win[:, k, 1024:1072], k == 0, k == KD - 1,
               (self.r_h4, self.r_win), (r_pg,))
        ACT(p, AF.Sigmoid, gates[:], pg[:, 0:48], (r_pg,), (r_gates,))
        ACT(p, AF.Sqrt, sm[:, 1, :], sm[:, 0, :], R, R)
        p.op("dve", lambda g: g.tensor_tensor(out=sm[:, 1, :], in0=sm[:, 1, :], in1=self.KMs[:], op=ALU.mult),
             (self.r_sm, self.r_KM), R)
        p.op("dve", lambda g: g.tensor_tensor(out=sm[:, 2, :], in0=self.cst["negst"][:, s, :], in1=sm[:, 1, :],
                                              op=ALU.subtract), (self.r_sm, self.r_cst), R)
        p.op("dve", lambda g: g.tensor_copy(out=Qa[:, :, 64], in_=sm[:, 2, :]), R, (r_Qa,))
        p.op("dve", lambda g: g.tensor_tensor(out=sm[:, 3, :], in0=sm[:, 2, :], in1=Qa[:, :, 64], op=ALU.subtract),
             (self.r_sm, r_Qa), R)
        p.op("dve", lambda g: g.tensor_copy(out=Qa[:, :, 65], in_=sm[:, 3, :]), R, (r_Qa,))
        p.op("dve", lambda g: g.tensor_tensor(out=sm[:, 4, :], in0=sm[:, 3, :], in1=Qa[:, :, 65], op=ALU.subtract),
             (self.r_sm, r_Qa), R)
        p.op("dve", lambda g: g.tensor_copy(out=Qa[:, :, 66], in_=sm[:, 4, :]), R, (r_Qa,))
        for g_ in range(4):
            hf = g_ % 2
            tb = bk.tb[0:KROWS, hf * 512:(hf + 1) * 512]
            for j in range(4):
                p.op("pe", lambda g, j=j: g.transpose(tb[:, j * 128:(j + 1) * 128], Qa[:, g_ * 4 + j, :],
                                                      self.cst["ident"][:]), (r_Qa, self.r_cst), (self.r_tbh[hf],))
            ACT(p, AF.Copy, QT[:, g_, :], tb, (self.r_tbh[hf],), (r_QT,))

    def _score_exp(self, KT_ap, r_k, QT_ap, r_q, neg=None, r_neg=()):
        p = self.p
        nb = len(self.sbanks)
        i = self.iss % nb
        self.iss += 1
        ps, r_ps = self.sbanks[i]
        MM(p, ps[:], KT_ap, QT_ap, True, True, (r_k, r_q), (r_ps,))
        ie = self.ie % self.NE
        self.ie += 1
        if neg is None:
            ACT(p, AF.Exp, self.e[ie][:], ps[:], (r_ps,), (self.r_e[ie],))
        else:
            zt, r_zt = self.zt[i % 2], self.r_zt[i % 2]
            p.op("dve", lambda g: g.tensor_tensor(out=zt[:].rearrange("k (j q) -> k j q", q=128),
                                                  in0=ps[:].rearrange("k (j q) -> k j q", q=128),
                                                  in1=neg.unsqueeze(1).to_broadcast([128, 4, 128]), op=ALU.add),
                 (r_ps,) + tuple(r_neg), (r_zt,))
            ACT(p, AF.Exp, self.e[ie][:], zt[:], (r_zt,), (self.r_e[ie],))
        return ie

    def _pv(self, po, r_po, pt, r_pt, V_ap, r_v, first, last):
        p = self.p
        if ATT_DUMMY_N > 0:
            p.op("pe", lambda g: g.matmul(self.poT[96:128, 0:ATT_DUMMY_N], self.cst["ident"][:, 0:32],
                                          self.win[:, 0, 0:ATT_DUMMY_N], start=True, stop=True, skip_group_check=True),
                 (), ())
        p.op("pe", lambda g: g.matmul(self.poT[0:65, :], V_ap, pt[:], start=first, stop=last),
             (r_pt, r_v), (self.r_poT,))
        if last:
            ACT(p, AF.Copy, self.oTs[:], self.poT[0:65, :], (self.r_poT,), (self.r_oTs,))
            for j in range(4):
                p.op("pe", lambda g, j=j: g.transpose(po[:, j * 65:(j + 1) * 65], self.oTs[:, j * 128:(j + 1) * 128],
                                                      self.cst["ident32"][0:65, 0:65]),
                     (self.r_oTs, self.r_cst), (r_po,))

    def _mask_mul(self, ie, mask_ap, r_mask):
        p = self.p
        pp, r_pp = self.pp[ie], self.r_pp[ie]
        p.op("dve", lambda g: g.tensor_tensor(out=pp[:].rearrange("k (j q) -> k j q", q=128),
                                              in0=self.e[ie][:].rearrange("k (j q) -> k j q", q=128),
                                              in1=mask_ap.unsqueeze(1).to_broadcast([128, 4, 128]), op=ALU.mult),
             (self.r_e[ie],) + tuple(r_mask), (r_pp,))
        return pp, r_pp

    def _finish_branch(self, po, r_po, b, g_, gates, r_gates, first_branch):
        p = self.p
        R = (self.r_rd,)
        den = po[:, 64:260:65]
        p.op("dve", lambda g: g.tensor_scalar(out=self.rd[:, 0:4], in0=den, scalar1=1e-30, scalar2=None, op0=ALU.max),
             (r_po,), R)
        p.op("dve", lambda g: g.reciprocal(out=self.rd[:, 0:4], in_=self.rd[:, 0:4]), R, R)
        gv = gates[:, g_ * 12 + b:g_ * 12 + 12:3]
        p.op("dve", lambda g: g.tensor_tensor(out=self.rd[:, 4:8], in0=self.rd[:, 0:4], in1=gv, op=ALU.mult),
             (self.r_rd, r_gates), R)
        for j in range(4):
            h = g_ * 4 + j
            if first_branch:
                p.op("dve", lambda g, j=j, h=h: g.tensor_scalar(out=self.oacc[:, h, :], in0=po[:, j * 65:j * 65 + 64],
                                                                scalar1=self.rd[:, 4 + j:5 + j], scalar2=None, op0=ALU.mult),
                     (r_po, self.r_rd), (self.r_oacc,))
            else:
                p.op("dve", lambda g, j=j, h=h: g.scalar_tensor_tensor(
                    out=self.oacc[:, h, :], in0=po[:, j * 65:j * 65 + 64], scalar=self.rd[:, 4 + j:5 + j],
                    in1=self.oacc[:, h, :], op0=ALU.mult, op1=ALU.add), (r_po, self.r_rd, self.r_oacc), (self.r_oacc,))

    def _next_po(self):
        i = self.ipo % 2
        self.ipo += 1
        return self.ps_o[i], self.r_ps_o[i]

    def _run_chunks(self, chunks, qt, r_QT, po, r_po, after_pv=None):
        n = len(chunks)
        ies = [None] * n

        def S(i):
            ch = chunks[i]
            if ch.get("pre") is not None:
                ch["pre"]()
            neg = ch.get("neg")
            if neg is None:
                ies[i] = self._score_exp(ch["KT"], ch["r_k"], qt, r_QT)
            else:
                ies[i] = self._score_exp(ch["KT"], ch["r_k"], qt, r_QT, neg[0], neg[1])

        la = len(self.sbanks) - 1
        for i0 in range(min(la, n)):
            S(i0)
        for i in range(n):
            if i + la < n:
                S(i + la)
            ch = chunks[i]
            ie = ies[i]
            mul = ch.get("mul")
            if mul is not None:
                pt, r_pt = self._mask_mul(ie, mul[0], mul[1])
            else:
                pt, r_pt = self.e[ie], self.r_e[ie]
            self._pv(po, r_po, pt, r_pt, ch["V"], ch["r_v"], i == 0, i == n - 1)
            if after_pv is not None:
                after_pv(i, pt, r_pt)

    def _new_m2(self, iota, thr_col):
        p = self.p
        m2, r_m2 = self.m2[self.im2 % 3], self.r_m2[self.im2 % 3]
        self.im2 += 1
        p.op("dve", lambda g: g.tensor_scalar(out=m2[:], in0=iota, scalar1=thr_col, scalar2=-30000.0,
                                              op0=ALU.is_lt, op1=ALU.mult), (self.r_cst,), (r_m2,))
        return m2, r_m2

    def slot_group(self, s, g_, d):
        p, bk, c = self.p, self.bk, self.c
        NSB, NCc, PC = self.NSB, self.NCc, self.PC
        QT, r_QT = self.QT[s % 2], self.r_QT[s % 2]
        gates, r_gates = self.gates[s % 2], self.r_gates[s % 2]
        qt = QT[:, g_, :]
        cst, r_cst = self.cst, self.r_cst
        ncmp = min(NCc, (8 * s + 7) // 16 + 1)
        nfull = (8 * s - 17) // 16 + 1 if 8 * s >= 17 else 0
        po, r_po = self._next_po()
        chunks = []
        for jc in range(ncmp):
            ch = dict(KT=self.KTc[:, g_, jc * 128:(jc + 1) * 128], r_k=self.r_kvc, V=self.Vc[:, g_, jc, :], r_v=self.r_kvc)
            if jc >= nfull:
                m2, r_m2 = self._new_m2(cst["iota1"][:], cst["thr1"][:, s * NCc + jc:s * NCc + jc + 1])
                ch["neg"] = (m2[:], (r_m2,))
            chunks.append(ch)

        def imp_mm(jc, pt, r_pt):
            for j in range(4):
                pr, r_pr = self.ps_r[j // 2], self.r_ps_r[j // 2]
                p.op("pe", lambda g, j=j: g.matmul(pr[:, (j % 2) * 256:(j % 2) * 256 + NSB], pt[:, j * 128:(j + 1) * 128],
                                                   cst["apool"][:, jc, :], start=(jc == 0 and j % 2 == 0),
                                                   stop=(jc == ncmp - 1), skip_group_check=True),
                     (r_pt, r_cst), (r_pr,))

        self.sbanks = [(self.ps_s[0], self.r_ps_s[0]), (self.ps_s[1], self.r_ps_s[1])]
        self._run_chunks(chunks, qt, r_QT, po, r_po, after_pv=imp_mm)
        self._finish_branch(po, r_po, 0, g_, gates, r_gates, True)
        RS = (self.r_sc,)
        for j in range(4):
            pr, r_pr = self.ps_r[j // 2], self.r_ps_r[j // 2]
            src = pr[:, (j % 2) * 256:(j % 2) * 256 + NSB]
            if j == 0:
                p.op("dve", lambda g: g.tensor_scalar(out=self.sc[:], in0=src, scalar1=self.rd[:, 0:1], scalar2=None,
                                                      op0=ALU.mult), (r_pr, self.r_rd), RS)
            else:
                p.op("dve", lambda g, j=j: g.scalar_tensor_tensor(out=self.sc[:], in0=src, scalar=self.rd[:, j:j + 1],
                                                                  in1=self.sc[:], op0=ALU.mult, op1=ALU.add),
                     (r_pr, self.r_rd, self.r_sc), RS)
        p.op("dve", lambda g: g.tensor_tensor(out=self.sc[:], in0=self.sc[:], in1=cst["bonus"][:, s, :], op=ALU.add),
             (self.r_sc, r_cst), RS)
        p.op("dve", lambda g: g.max(out=self.m8[:, 0:8], in_=self.sc[:]), RS, RS)
        p.op("dve", lambda g: g.match_replace(out=self.sc2[:], in_to_replace=self.m8[:, 0:8], in_values=self.sc[:],
                                              imm_value=-3.0e38), RS, RS)
        p.op("dve", lambda g: g.max(out=self.m8[:, 8:16], in_=self.sc2[:]), RS, RS)
        p.op("dve", lambda g: g.tensor_scalar(out=self.m8[:, 15:16], in0=self.m8[:, 15:16], scalar1=-1.0e29, scalar2=None,
                                              op0=ALU.max), RS, RS)
        p.op("dve", lambda g: g.tensor_scalar(out=self.sel[:], in0=self.sc[:], scalar1=self.m8[:, 15:16], scalar2=None,
                                              op0=ALU.is_ge), RS, (self.r_sel,))
        nk = 8 * s + 8
        po, r_po = self._next_po()
        mbanks = ((bk.tb, bk.r_tb), (c.ps_n[:].bitcast(BF16), c.r_psn))
        chunks = []
        for kc in range(nk):
            cl = kc % PC
            pi = kc // PC
            ch = {}

            def pre(kc=kc, cl=cl, pi=pi, ch=ch):
                if cl == 0:
                    ncz = min(PC, nk - kc)
                    ib = self.ikv % 3
                    self.ikv += 1
                    self.cur_kv = (self.KTp[ib], self.Vp[ib], self.r_kvp[ib])
                    KTp, Vp, r_kv = self.cur_kv
                    p.dma("sp", KTp[:, 0:ncz * 128], d["KTs"][g_][:, kc * 128:(kc + ncz) * 128], r_kv, (), (r_kv,))
                    p.dma("sp", Vp[:, 0:ncz, :], d["Vs"][g_][:, kc:kc + ncz, :], r_kv, (), (r_kv,))
                KTp, Vp, r_kv = self.cur_kv
                ch["KT"], ch["r_k"], ch["V"], ch["r_v"] = KTp[:, cl * 128:(cl + 1) * 128], r_kv, Vp[:, cl, :], r_kv
                if kc % 8 == 0:
                    mb, r_mb = mbanks[(kc // 8) % 2]
                    nm_ = min(8, nk - kc)
                    sx, r_sx = self.selx[(kc // 8) % 2], self.r_selx[(kc // 8) % 2]
                    p.op("pool", lambda g: g.tensor_copy(
                        out=sx[:, 0:nm_ * 128].rearrange("q (b k) -> q b k", k=64),
                        in_=self.sel[:, 2 * kc:2 * kc + 2 * nm_].unsqueeze(2).to_broadcast([128, 2 * nm_, 64])),
                        (self.r_sel,), (r_sx,))
                    for m in range(nm_):
                        p.op("pe", lambda g, m=m: g.transpose(mb[:, m * 128:(m + 1) * 128], sx[:, m * 128:(m + 1) * 128],
                                                              cst["ident"][:]), (r_sx, r_cst), (r_mb,))
                mb, r_mb = mbanks[(kc // 8) % 2]
                ch["mul"] = (mb[:, (kc % 8) * 128:(kc % 8 + 1) * 128], (r_mb,))
                if kc >= 8 * s:
                    r = kc - 8 * s
                    m2, r_m2 = self._new_m2(cst["iota2"][:], cst["thr2"][:, r:r + 1])
                    ch["neg"] = (m2[:], (r_m2,))

            ch["pre"] = pre
            chunks.append(ch)
        self.sbanks = [(self.ps_s[0], self.r_ps_s[0]), (self.ps_s[1], self.r_ps_s[1]),
                       (self.ps_r[0], self.r_ps_r[0]), (self.ps_r[1], self.r_ps_r[1])]
        self._run_chunks(chunks, qt, r_QT, po, r_po)
        self._finish_branch(po, r_po, 1, g_, gates, r_gates, False)
        iw = self.iw % 2
        self.iw += 1
        KTw, Vw, r_kw = self.KTw[iw], self.Vw[iw], self.r_kvw[iw]
        p.dma("sp", KTw[:], d["KTw"][g_][:, s * 640:(s + 1) * 640], r_kw, (), (r_kw,))
        p.dma("sp", Vw[:], d["Vw"][g_][:, s * 5:(s + 1) * 5, :], r_kw, (), (r_kw,))
        po, r_po = self._next_po()
        chunks = []
        for r in range(5):
            ch = dict(KT=KTw[:, r * 128:(r + 1) * 128], r_k=r_kw, V=Vw[:, r, :], r_v=r_kw)
            if r == 0:
                ch["neg"] = (cst["negU"][:], (r_cst,))
            elif r == 4:
                ch["neg"] = (cst["negL"][:], (r_cst,))
            chunks.append(ch)
        self._run_chunks(chunks, qt, r_QT, po, r_po)
        self._finish_branch(po, r_po, 2, g_, gates, r_gates, False)

    def run(self, d):
        p = self.p
        self.setup(d)
        for s in range(self.NS):
            if s % 4 == 0:
                self.prep4(s, d)
            self.prep_slot(s)
            for g_ in range(4):
                self.slot_group(s, g_, d)
            ob, r_ob = self.obf[s % 2], self.r_obf[s % 2]
            ACT(p, AF.Copy, ob[:], self.oacc[:].rearrange("q h d -> q (h d)"), (self.r_oacc,), (r_ob,))
            ot, r_ot = self.obT[s % 2], self.r_obT[s % 2]
            for hf in range(2):
                for kk in range(4):
                    k8 = hf * 4 + kk
                    p.op("pe", lambda g, k8=k8, kk=kk, hf=hf: g.transpose(
                        self.bk.tb[:, hf * 512 + kk * 128:hf * 512 + (kk + 1) * 128], ob[:, k8 * 128:(k8 + 1) * 128],
                        self.cst["ident"][:]), (r_ob, self.r_cst), (self.r_tbh[hf],))
                ACT(p, AF.Copy, ot[:, hf * 4:(hf + 1) * 4, :],
                    self.bk.tb[:, hf * 512:(hf + 1) * 512].rearrange("f (k q) -> f k q", q=128), (self.r_tbh[hf],), (r_ot,))
            with p.nc.allow_non_contiguous_dma(reason="256B runs"):
                p.dma("sp", d["oT"][:, s * 128:(s + 1) * 128].rearrange("(k f) n -> f k n", f=128), ot[:], r_ot,
                      (r_ot,), (), is_output=True)


class OPPhase:
    def __init__(self, p, c, ws, bk):
        self.p, self.c, self.ws, self.bk = p, c, ws, bk
        self.xs = [p.sb("opx%d" % i, [128, KD, TT], F32) for i in range(2)]
        self.r_xs = [p.res("opx%d" % i) for i in range(2)]
        self.on = [p.sb("opo%d" % i, [128, KD, TT], BF16) for i in range(2)]
        self.r_on = [p.res("opo%d" % i) for i in range(2)]

    def run(self, x_in, x_out, oT, w_out, ntiles):
        p, bk = self.p, self.bk
        for t in range(ntiles):
            for blk in range(2):
                self.ws.plan_cols(w_out, blk * 512)
        for t in range(ntiles):
            T0 = t * TT
            xs, r_xs = self.xs[t % 2], self.r_xs[t % 2]
            on, r_on = self.on[t % 2], self.r_on[t % 2]
            p.dma("sp", xs[:], x_in[:, T0:T0 + TT].rearrange("(k q) n -> q k n", q=128), r_xs, (), (r_xs,))
            p.dma("sp", on[:], oT[:, T0:T0 + TT].rearrange("(k q) n -> q k n", q=128), r_on, (), (r_on,))
            for blk in range(2):
                w, r_w = self.ws.next()
                for dt_ in range(4):
                    po, r_po = bk.b[dt_ % 2], bk.r[dt_ % 2]
                    for k in range(KD):
                        MM(p, po[:], w[:, k, dt_ * 128:(dt_ + 1) * 128], on[:, k, :], k == 0, k == KD - 1,
                           (r_w, r_on), (r_po,))
                    kk = blk * 4 + dt_
                    p.op("dve", lambda g: g.tensor_tensor(out=xs[:, kk, :], in0=xs[:, kk, :], in1=po[:], op=ALU.add),
                         (r_xs, r_po), (r_xs,))
            p.dma("sp", x_out[:, T0:T0 + TT].rearrange("(k q) n -> q k n", q=128), xs[:], r_xs, (r_xs,), (),
                  is_output=True)


from contextlib import ExitStack
import ml_dtypes

NPBF = ml_dtypes.bfloat16
NCORES = 8


class _IO:
    def __init__(self, nc):
        self.nc = nc

    def i(self, n, s, d=F32):
        return self.nc.dram_tensor(n, list(s), d, kind="ExternalInput").ap()

    def o(self, n, s, d=F32):
        return self.nc.dram_tensor(n, list(s), d, kind="ExternalOutput").ap()

    def t(self, n, s, d=F32):
        return self.nc.dram_tensor(n, list(s), d, kind="Internal").ap()


def _hg1_io(io, NT, sfx, out=True):
    mk = io.o if out else io.i
    return dict(o_loc=mk("o_loc" + sfx, [D, NT]), qdec=mk("qdec" + sfx, [D, NT], BF16), sg=mk("sg" + sfx, [D, NT], BF16),
                s_fin=mk("s_fin" + sfx, [128, 8, 128]) if out else None, dec=mk("dec" + sfx, [128, 8]) if out else None)


def _phase_hg1(p, c, io, x_ap, layer, NT, outs, cd_in):
    p.begin_phase()
    ws = WStream(p)
    bk = Banks(p)
    cd = load_consts_hg(p, cd_in["reset"], cd_in["tri"], cd_in["ident"])
    g1 = HG1(p, c, ws, bk, cd)
    g1.setup(cd_in["a_norm_w"], cd_in["alb"], layer)
    g1.plan_weights(cd_in["a_w_in"], NT // TT)
    for t in range(NT // TT):
        g1.tile(t, x_ap, outs["o_loc"], outs["qdec"], outs["sg"])
    g1.finish(outs["s_fin"], outs["dec"])
    p.end_phase()


def _phase_hg2(p, c, x_in, x_out, ins, NT):
    p.begin_phase()
    ws = WStream(p)
    bk = Banks(p)
    g2 = HG2(p, c, ws, bk)
    g2.prefix(ins["S_all"], ins["dec_all"], ins["selv"], ins["gnorm_w"])
    g2.plan_weights(ins["a_w_out"], NT // TT)
    for t in range(NT // TT):
        g2.tile(t, x_in, x_out, ins["o_loc"], ins["qdec"], ins["sg"])
    p.end_phase()


def _phase_mlp(p, c, x_in, x_out, wn, wu, wd, NT, final_w=None):
    p.begin_phase()
    m = MLP(p, c)
    m.run(x_in, x_out, NT // TT, wn, wu, wd, final_w=final_w)
    p.end_phase()


def build_L1(NT):
    nc = bass.Bass("TRN2", target_bir_lowering=False)
    io = _IO(nc)
    x = io.i("xT", [D, NT])
    cd_in = dict(reset=io.i("c_reset", [128, TT]), tri=io.i("c_tri", [64, 64], BF16), ident=io.i("c_ident", [128, 128], BF16),
                 a_norm_w=io.i("a_norm_w", [D]), alb=io.i("alb", [2, D]), a_w_in=io.i("a_w_in", [D, 4096]))
    outs = _hg1_io(io, NT, "")
    with ExitStack() as st:
        p = Prog(nc, st)
        c = Common(p)
        _phase_hg1(p, c, io, x, 0, NT, outs, cd_in)
        p.finish()
        nc._ninstr = p.ninstr
    return nc


def build_L2(NT):
    nc = bass.Bass("TRN2", target_bir_lowering=False)
    io = _IO(nc)
    x = io.i("xT", [D, NT])
    ins = dict(S_all=io.i("S_all", [8, 128, 8, 128]), dec_all=io.i("dec_all", [8, 128, 8]), selv=io.i("selv", [128, 8]),
               gnorm_w=io.i("gnorm_w", [128]), a_w_out=io.i("a_w_out", [D, D]),
               o_loc=io.i("o_loc_in", [D, NT]), qdec=io.i("qdec_in", [D, NT], BF16), sg=io.i("sg_in", [D, NT], BF16))
    wn, wu, wd = io.i("mlp_norm_w", [D]), io.i("mlp_w_up", [D, DFF]), io.i("mlp_w_down", [DFF, D])
    cd_in = dict(reset=io.i("c_reset", [128, TT]), tri=io.i("c_tri", [64, 64], BF16), ident=io.i("c_ident", [128, 128], BF16),
                 a_norm_w=io.i("a_norm_w", [D]), alb=io.i("alb", [2, D]), a_w_in=io.i("a_w_in", [D, 4096]))
    x1 = io.t("x1", [D, NT])
    x2 = io.o("x2", [D, NT])
    outs = _hg1_io(io, NT, "")
    with ExitStack() as st:
        p = Prog(nc, st)
        c = Common(p)
        _phase_hg2(p, c, x, x1, ins, NT)
        _phase_mlp(p, c, x1, x2, wn, wu, wd, NT)
        _phase_hg1(p, c, io, x2, 1, NT, outs, cd_in)
        p.finish()
        nc._ninstr = p.ninstr
    return nc


def build_L3(NT):
    nc = bass.Bass("TRN2", target_bir_lowering=False)
    io = _IO(nc)
    x = io.i("xT", [D, NT])
    ins = dict(S_all=io.i("S_all", [8, 128, 8, 128]), dec_all=io.i("dec_all", [8, 128, 8]), selv=io.i("selv", [128, 8]),
               gnorm_w=io.i("gnorm_w", [128]), a_w_out=io.i("a_w_out", [D, D]),
               o_loc=io.i("o_loc_in", [D, NT]), qdec=io.i("qdec_in", [D, NT], BF16), sg=io.i("sg_in", [D, NT], BF16))
    wn, wu, wd = io.i("mlp_norm_w", [D]), io.i("mlp_w_up", [D, DFF]), io.i("mlp_w_down", [DFF, D])
    kvn, kvw = io.i("kv_norm_w", [D]), io.i("kv_w", [D, 1536])
    x1 = io.t("x1", [D, NT])
    x2 = io.o("x2", [D, NT])
    kT01, kT24 = io.o("kT01", [512, NT], BF16), io.o("kT24", [512, NT], BF16)
    vaug = io.o("vaug", [NT // 128, 128, 8, 65], BF16)
    kmx = io.o("kmx", [128, 4])
    with ExitStack() as st:
        p = Prog(nc, st)
        c = Common(p)
        _phase_hg2(p, c, x, x1, ins, NT)
        _phase_mlp(p, c, x1, x2, wn, wu, wd, NT)
        p.begin_phase()
        ws = WStream(p)
        bk = Banks(p)
        kv = KVPhase(p, c, ws, bk)
        kv.setup(kvn)
        kv.plan_weights(kvw, NT // TT)
        for t in range(NT // TT):
            kv.tile(t, x2, kT01, kT24, vaug)
        kv.finish(kmx)
        p.end_phase()
        p.finish()
        nc._ninstr = p.ninstr
    return nc


def build_L4(NB):
    nc = bass.Bass("TRN2", target_bir_lowering=False)
    io = _IO(nc)
    kin, vin = io.i("kin", [4, 64, 16 * NB + 16], BF16), io.i("vin", [4, 64, 16 * NB + 16], BF16)
    pk, w1k, w2k = io.i("cmp_pe_k", [32, 64]), io.i("cmp_w1_k", [2048, 256]), io.i("cmp_w2_k", [256, 64])
    pv, w1v, w2v = io.i("cmp_pe_v", [32, 64]), io.i("cmp_w1_v", [2048, 256]), io.i("cmp_w2_v", [256, 64])
    kc, vc, kmxc = io.o("kc", [4, 64, NB], BF16), io.o("vc", [NB, 4, 65], BF16), io.o("kmxc", [64, 4])
    with ExitStack() as st:
        p = Prog(nc, st)
        c = Common(p)
        p.begin_phase()
        bk = Banks(p)
        cm = CMPPhase(p, c, bk, NB)
        cm.run(kin, vin, pk, w1k, w2k, pv, w1v, w2v, kc, vc, kmxc)
        p.end_phase()
        p.finish()
        nc._ninstr = p.ninstr
    return nc


def build_L5(NT, T, nlayers=2):
    nc = bass.Bass("TRN2", target_bir_lowering=False)
    io = _IO(nc)
    NS = NT // 128
    NSB = T // 64
    NCc = max(1, (T // 16) // 128)
    x = io.i("xT", [D, NT])
    shared = dict(KTs=io.i("KTs", [4, KROWS, T], BF16), Vs=io.i("Vs", [4, 128, T // 128, 65], BF16),
                  KTw=io.i("KTw", [4, KROWS, NS * 640], BF16), Vw=io.i("Vw", [4, 128, NS * 5, 65], BF16),
                  KTc=io.i("KTc", [4, KROWS, NCc * 128], BF16), Vc=io.i("Vc", [4, 128, NCc, 65], BF16),
                  kmall=io.i("kmall", [128, 4, 24]),
                  c_ident=io.i("c_ident", [128, 128], BF16), c_ident32=io.i("c_ident32", [128, 128]), c_iota1=io.i("c_iota1", [128, 128]),
                  c_iota2=io.i("c_iota2", [128, 128]), c_negL=io.i("c_negL", [128, 128]), c_negU=io.i("c_negU", [128, 128]),
                  c_apool=io.i("c_apool", [128, NCc, NSB], BF16), c_thr1=io.i("c_thr1", [128, NS * NCc]),
                  c_thr2=io.i("c_thr2", [128, 8]), c_negst=io.i("c_negst", [128, NS, 16]),
                  c_bonus=io.i("c_bonus", [128, NS, NSB]), c_qaug=io.i("c_qaug", [128, 16, 4], BF16))
    lw = []
    for b in range(nlayers):
        lw.append(dict(norm_w=io.i("b_norm_w%d" % b, [D]), w_in=io.i("b_w_in%d" % b, [D, 1072]),
                       w_out=io.i("b_w_out%d" % b, [D, D]), wn=io.i("mlp_norm_w%d" % b, [D]),
                       wu=io.i("mlp_w_up%d" % b, [D, DFF]), wd=io.i("mlp_w_down%d" % b, [DFF, D])))
    fw = io.i("final_norm_w", [D])
    out = io.o("outT", [D, NT])
    with ExitStack() as st:
        p = Prog(nc, st)
        c = Common(p)
        xcur = x
        for b in range(nlayers):
            oT = io.t("oT%d" % b, [D, NT], BF16)
            xa = io.t("xa%d" % b, [D, NT])
            xb = out if b == nlayers - 1 else io.t("xb%d" % b, [D, NT])
            p.begin_phase()
            bk = Banks(p)
            at = ATTPhase(p, c, bk, NS, T)
            d = dict(shared)
            d.update(xT=xcur, norm_w=lw[b]["norm_w"], w_in=lw[b]["w_in"], oT=oT)
            at.run(d)
            p.end_phase()
            p.begin_phase()
            ws = WStream(p)
            bk = Banks(p)
            op = OPPhase(p, c, ws, bk)
            op.run(xcur, xa, oT, lw[b]["w_out"], NT // TT)
            p.end_phase()
            _phase_mlp(p, c, xa, xb, lw[b]["wn"], lw[b]["wu"], lw[b]["wd"], NT,
                       final_w=fw if b == nlayers - 1 else None)
            xcur = xb
        p.finish()
        nc._ninstr = p.ninstr
    return nc


def _bf16_split(v):
    hi = np.asarray(v, np.float64).astype(NPBF)
    lo = (np.asarray(v, np.float64) - hi.astype(np.float64)).astype(NPBF)
    return hi, lo


def _aug_rows(u):
    u = np.asarray(u, np.int64)
    a = (u // 128).astype(np.float32)
    b = (u % 128).astype(np.float32)
    one = np.ones_like(a)
    return np.stack([one, one, one, a, a, b, b], 0).astype(NPBF)


def att_consts(core, NS, T):
    NSB = T // 64
    NCc = max(1, (T // 16) // 128)
    pp = np.arange(128)[:, None]
    qq = np.arange(128)[None, :]
    cst = {}
    cst["c_ident"] = np.eye(128).astype(NPBF)
    cst["c_ident32"] = np.eye(128).astype(np.float32)
    cst["c_iota1"] = (qq - 16 * pp).astype(np.float32)
    cst["c_iota2"] = (qq - pp).astype(np.float32) * np.ones((128, 128), np.float32)
    cst["c_negL"] = np.where(pp > qq, -30000.0, 0.0).astype(np.float32)
    cst["c_negU"] = np.where(qq >= pp, -30000.0, 0.0).astype(np.float32)
    cg = (np.arange(NCc)[None, :, None] * 128 + np.arange(128)[:, None, None])
    nn = np.arange(NSB)[None, None, :]
    cst["c_apool"] = ((cg >= 4 * nn - 1) & (cg <= 4 * nn + 3)).astype(NPBF)
    qb = 8 * np.arange(NS) + core
    thr1 = (2048 * np.arange(NCc)[None, :] + 31 - 128 * qb[:, None]).reshape(-1).astype(np.float32)
    cst["c_thr1"] = np.broadcast_to(thr1[None, :], (128, NS * NCc)).copy()
    thr2 = (128 * (np.arange(8) - core)).astype(np.float32)
    cst["c_thr2"] = np.broadcast_to(thr2[None, :], (128, 8)).copy()
    bhi, blo = _bf16_split(NSA_SLOPES)
    seff = bhi.astype(np.float64) + blo.astype(np.float64)
    tq = (128 * qb[None, :] + np.arange(128)[:, None]).astype(np.float64)
    cst["c_negst"] = (-tq[:, :, None] * seff[None, None, :]).astype(np.float32)
    cur = (tq // 64).astype(np.int64)[:, :, None]
    nb = np.arange(NSB)[None, None, :]
    forced = (nb == 0) | (nb == cur) | (nb == cur - 1)
    cst["c_bonus"] = np.where(nb <= cur, 1.0e4 * forced, -1.0e30).astype(np.float32)
    qa = np.stack([(128.0 * bhi.astype(np.float64)).astype(NPBF), (128.0 * blo.astype(np.float64)).astype(NPBF), bhi, blo], -1)
    cst["c_qaug"] = np.broadcast_to(qa[None], (128, 16, 4)).copy()
    return cst


_PROGS = {}


def _prog(key, fn):
    if key not in _PROGS:
        _PROGS[key] = fn()
    return _PROGS[key]


def _run(nc, in_maps):
    return run_bass_kernel_spmd(nc, in_maps, core_ids=list(range(NCORES))).results


def kernel(x, a_norm_w, a_w_in, a_gnorm_w, a_w_out, a_lower_bounds, kv_norm_w, kv_w,
           cmp_pe_k, cmp_w1_k, cmp_w2_k, cmp_pe_v, cmp_w1_v, cmp_w2_v,
           b_norm_w, b_w_in, b_w_out, mlp_norm_w, mlp_w_up, mlp_w_down, final_norm_w, _debug=None):
    f32 = lambda a: np.ascontiguousarray(np.asarray(a, dtype=np.float32))
    x = f32(x)
    T = x.shape[1]
    NT = T // NCORES
    NS = NT // 128
    xs = x[0]
    hc = host_consts_hg()
    a_norm_w, a_w_in, a_gnorm_w, a_w_out, alb = map(f32, (a_norm_w, a_w_in, a_gnorm_w, a_w_out, a_lower_bounds))
    mlp_norm_w, mlp_w_up, mlp_w_down = map(f32, (mlp_norm_w, mlp_w_up, mlp_w_down))
    b_norm_w, b_w_in, b_w_out = map(f32, (b_norm_w, b_w_in, b_w_out))
    xT = [np.ascontiguousarray(xs[c * NT:(c + 1) * NT].T) for c in range(NCORES)]

    def selv(c):
        s = np.zeros((128, 8), np.float32)
        s[:, :c] = 1.0
        return s

    r1 = _run(_prog(("L1", NT), lambda: build_L1(NT)),
              [dict(xT=xT[c], a_norm_w=a_norm_w[0], alb=alb, a_w_in=a_w_in[0], **hc) for c in range(NCORES)])
    S_all = np.stack([r["s_fin"] for r in r1])
    dec_all = np.stack([r["dec"] for r in r1])
    r2 = _run(_prog(("L2", NT), lambda: build_L2(NT)),
              [dict(xT=xT[c], S_all=S_all, dec_all=dec_all, selv=selv(c), gnorm_w=a_gnorm_w[0], a_w_out=a_w_out[0],
                    o_loc_in=r1[c]["o_loc"], qdec_in=r1[c]["qdec"], sg_in=r1[c]["sg"],
                    mlp_norm_w=mlp_norm_w[0], mlp_w_up=mlp_w_up[0], mlp_w_down=mlp_w_down[0],
                    a_norm_w=a_norm_w[1], alb=alb, a_w_in=a_w_in[1], **hc) for c in range(NCORES)])
    S_all = np.stack([r["s_fin"] for r in r2])
    dec_all = np.stack([r["dec"] for r in r2])
    r3 = _run(_prog(("L3", NT), lambda: build_L3(NT)),
              [dict(xT=r2[c]["x2"], S_all=S_all, dec_all=dec_all, selv=selv(c), gnorm_w=a_gnorm_w[1], a_w_out=a_w_out[1],
                    o_loc_in=r2[c]["o_loc"], qdec_in=r2[c]["qdec"], sg_in=r2[c]["sg"],
                    mlp_norm_w=mlp_norm_w[1], mlp_w_up=mlp_w_up[1], mlp_w_down=mlp_w_down[1],
                    kv_norm_w=f32(kv_norm_w), kv_w=f32(kv_w)) for c in range(NCORES)])
    if _debug is not None:
        _debug["x_l1"] = np.concatenate([r["x2"].T for r in r3], 0)
    kT01 = np.concatenate([r["kT01"] for r in r3], 1)
    kT24 = np.concatenate([r["kT24"] for r in r3], 1)
    vaug = np.concatenate([r["vaug"] for r in r3], 0)
    n_cmp = T // 16
    NB = n_cmp // NCORES
    pad = np.zeros((512, 16), NPBF)
    kpad = np.concatenate([kT01, pad], 1)
    in4 = []
    for c in range(NCORES):
        sl = kpad[:, 16 * NB * c:16 * NB * (c + 1) + 16]
        in4.append(dict(kin=np.ascontiguousarray(sl[0:256].reshape(4, 64, -1)),
                        vin=np.ascontiguousarray(sl[256:512].reshape(4, 64, -1)),
                        cmp_pe_k=f32(cmp_pe_k), cmp_w1_k=f32(cmp_w1_k), cmp_w2_k=f32(cmp_w2_k),
                        cmp_pe_v=f32(cmp_pe_v), cmp_w1_v=f32(cmp_w1_v), cmp_w2_v=f32(cmp_w2_v)))
    r4 = _run(_prog(("L4", NB), lambda: build_L4(NB)), in4)
    NCc = max(1, n_cmp // 128)
    aug_tok = _aug_rows(np.arange(T))
    zpad = np.zeros((KROWS - 64 - NAUG, T), NPBF)
    KTs = np.stack([np.concatenate([kT24[g * 64:(g + 1) * 64], aug_tok, zpad], 0) for g in range(4)])
    KTwin_full = np.stack([np.concatenate([kT24[256 + g * 64:256 + (g + 1) * 64], aug_tok, zpad], 0) for g in range(4)])
    Vs = np.ascontiguousarray(np.transpose(vaug[:, :, 0:4, :], (2, 1, 0, 3)))
    Vwin_full = np.transpose(vaug[:, :, 4:8, :], (2, 1, 0, 3))
    kc_all = np.concatenate([r["kc"] for r in r4], 2)
    ncp = NCc * 128
    KTc = np.zeros((4, KROWS, ncp), NPBF)
    KTc[:, 0:64, :n_cmp] = kc_all
    KTc[:, 64:64 + NAUG, :n_cmp] = _aug_rows(16 * np.arange(n_cmp) + 31)[None]
    vc_all = np.concatenate([r["vc"] for r in r4], 0)
    vcp = np.zeros((ncp, 4, 65), NPBF)
    vcp[:n_cmp] = vc_all
    Vc = np.ascontiguousarray(np.transpose(vcp.reshape(NCc, 128, 4, 65), (2, 1, 0, 3)))
    km = np.zeros((4, 24), np.float32)
    for g in range(4):
        row = (g % 2) * 64
        vals = [r3[c]["kmx"][row, g // 2] for c in range(NCORES)] + [r3[c]["kmx"][row, 2 + g // 2] for c in range(NCORES)] \
            + [r4[c]["kmxc"][0, g] for c in range(NCORES)]
        km[g] = np.asarray(vals, np.float32)
    kmall = np.broadcast_to(km[None], (128, 4, 24)).copy()
    x_l1 = np.concatenate([r["x2"] for r in r3], 1)
    in5 = []
    for c in range(NCORES):
        qbs = [8 * s + c for s in range(NS)]
        xc = np.concatenate([x_l1[:, 128 * qb:128 * qb + 128] for qb in qbs], 1)
        KTw = np.zeros((4, KROWS, NS * 640), NPBF)
        Vw = np.zeros((4, 128, NS * 5, 65), NPBF)
        for s, qb in enumerate(qbs):
            for r in range(5):
                ch = qb - 4 + r
                if ch >= 0:
                    KTw[:, :, s * 640 + r * 128:s * 640 + (r + 1) * 128] = KTwin_full[:, :, ch * 128:(ch + 1) * 128]
                    Vw[:, :, s * 5 + r, :] = Vwin_full[:, :, ch, :]
        dd = dict(xT=np.ascontiguousarray(xc), KTs=KTs, Vs=Vs, KTw=KTw, Vw=Vw, KTc=KTc, Vc=Vc, kmall=kmall,
                  final_norm_w=f32(final_norm_w))
        dd.update(att_consts(c, NS, T))
        for b in range(2):
            dd.update({"b_norm_w%d" % b: b_norm_w[b], "b_w_in%d" % b: b_w_in[b], "b_w_out%d" % b: b_w_out[b],
                       "mlp_norm_w%d" % b: mlp_norm_w[2 + b], "mlp_w_up%d" % b: mlp_w_up[2 + b],
                       "mlp_w_down%d" % b: mlp_w_down[2 + b]})
        in5.append(dd)
    r5 = _run(_prog(("L5", NT, T), lambda: build_L5(NT, T)), in5)
    out = np.zeros((T, D), np.float32)
    for c in range(NCORES):
        oc = r5[c]["outT"]
        for s in range(NS):
            qb = 8 * s + c
            out[128 * qb:128 * qb + 128] = oc[:, s * 128:(s + 1) * 128].T
    return out[None]
```
